# Optimizing a Trainium2 kernel written in Bass

```python
import math
import jax, jax.numpy as jnp
from jax import lax
import numpy as np

D_MODEL = 1024
BATCH = 4
SEQ = 4096
DEPTH = 4

ROPE_THETA = 500000.0
Q_BLOCK = 128
NEG_INF = -1e30
FORCE_SCORE = 1e9
N_BRANCH = 4

DA_HEADS = 4
DA_DIM = 64
DA_ROT = DA_DIM // 4

NSA_HEADS = 4
NSA_DK = 128
NSA_DV = 128
NSA_ROT = NSA_DK // 4
CMP_LEN = 32
CMP_STRIDE = 16
SEL_LEN = 64
SEL_N = 16
WIN = 512

MLA_HEADS = 4
MLA_Q_LORA = 384
MLA_KV_LORA = 256
MLA_NOPE = 128
MLA_ROPE = 64
MLA_V = 128

DSA_HEADS = 4
DSA_DIM = 128
DSA_ROT = DSA_DIM // 4
IDX_HEADS = 8
IDX_DIM = 64
IDX_ROT = IDX_DIM // 4
IDX_TOPK = 256

BR_WIDTH = DA_HEADS * 2 * DA_DIM
D_FF = ((8 * D_MODEL + 3 * 256 - 1) // (3 * 256)) * 256

IN_LAYOUT = (
    ("a_q", DA_HEADS * 2 * DA_DIM), ("a_k", DA_HEADS * 2 * DA_DIM), ("a_v", DA_HEADS * 2 * DA_DIM),
    ("b_q", NSA_HEADS * NSA_DK),
    ("b_kc", NSA_DK), ("b_vc", NSA_DV), ("b_ks", NSA_DK), ("b_vs", NSA_DV),
    ("b_kw", NSA_DK), ("b_vw", NSA_DV), ("b_g", NSA_HEADS * 3),
    ("c_q", MLA_Q_LORA), ("c_kv", MLA_KV_LORA), ("c_kr", MLA_ROPE),
    ("d_q", DSA_HEADS * DSA_DIM), ("d_k", DSA_HEADS * DSA_DIM), ("d_v", DSA_HEADS * DSA_DIM),
    ("d_iq", IDX_HEADS * IDX_DIM), ("d_ik", IDX_DIM), ("d_iw", IDX_HEADS),
    ("gate", N_BRANCH * D_MODEL),
)
D_IN = sum(n for _, n in IN_LAYOUT)

kernel_name = "hybrid_gated_parallel_mixers"


def split_columns(z):
    names = [nm for nm, _ in IN_LAYOUT]
    offs = [int(o) for o in np.cumsum([n for _, n in IN_LAYOUT])[:-1]]
    return dict(zip(names, jnp.split(z, offs, axis=-1)))


def rmsnorm(x, g, eps=1e-6):
    xf = x.astype(jnp.float32)
    y = xf * lax.rsqrt(jnp.mean(xf * xf, axis=-1, keepdims=True) + eps)
    return (y * g.astype(jnp.float32)).astype(x.dtype)


def rope_tables(seq, rot_dim, dtype):
    inv = jnp.power(jnp.float32(ROPE_THETA), -jnp.arange(0, rot_dim, 2, dtype=jnp.float32) / rot_dim)
    ang = jnp.arange(seq, dtype=jnp.float32)[:, None] * inv[None, :]
    return jnp.cos(ang).astype(dtype), jnp.sin(ang).astype(dtype)


def apply_rope(x, cos, sin):
    half = cos.shape[-1]
    shape = (1, cos.shape[0]) + (1,) * (x.ndim - 3) + (half,)
    c = cos.reshape(shape)
    s = sin.reshape(shape)
    x1 = x[..., :half]
    x2 = x[..., half:2 * half]
    return jnp.concatenate([x1 * c - x2 * s, x2 * c + x1 * s, x[..., 2 * half:]], axis=-1)


def masked_softmax(s, mask):
    s = jnp.where(mask, s.astype(jnp.float32), NEG_INF)
    return jnp.where(mask, jax.nn.softmax(s, axis=-1), 0.0)


def sweep_query_blocks(fn, seq):
    out = lax.map(fn, jnp.arange(seq // Q_BLOCK) * Q_BLOCK)
    out = jnp.moveaxis(out, 0, 1)
    return out.reshape((out.shape[0], seq) + out.shape[3:])


def dense_causal_attention(q, k, v, scale):
    seq = q.shape[1]
    kpos = jnp.arange(seq)

    def block(qs):
        qb = lax.dynamic_slice_in_dim(q, qs, Q_BLOCK, axis=1)
        s = jnp.einsum("bqhd,bkhd->bhqk", qb, k).astype(jnp.float32) * scale
        mask = (qs + jnp.arange(Q_BLOCK))[:, None] >= kpos[None, :]
        p = masked_softmax(s, mask)
        return jnp.einsum("bhqk,bkhd->bqhd", p.astype(v.dtype), v)

    return sweep_query_blocks(block, seq)


def diff_attention(q, k, v, lam, lam_init, subln_g, cos, sin):
    B, S, H = q.shape[:3]
    q = apply_rope(q, cos, sin)
    k = apply_rope(k, cos, sin)
    scale = DA_DIM ** -0.5
    kpos = jnp.arange(S)

    def block(qs):
        qb = lax.dynamic_slice_in_dim(q, qs, Q_BLOCK, axis=1)
        s = jnp.einsum("bqhmd,bkhmd->bhmqk", qb, k).astype(jnp.float32) * scale
        mask = (qs + jnp.arange(Q_BLOCK))[:, None] >= kpos[None, :]
        p = masked_softmax(s, mask)
        a = p[:, :, 0] - lam * p[:, :, 1]
        return jnp.einsum("bhqk,bkhd->bqhd", a.astype(v.dtype), v)

    o = sweep_query_blocks(block, S)
    o = rmsnorm(o, subln_g) * (1.0 - lam_init)
    return o.reshape(B, S, H * 2 * DA_DIM)


def nsa_compress(tok, pe, w1, w2):
    S = tok.shape[1]
    nc = (S - CMP_LEN) // CMP_STRIDE + 1
    idx = jnp.arange(nc)[:, None] * CMP_STRIDE + jnp.arange(CMP_LEN)[None, :]
    blocks = tok[:, idx] + pe
    flat = blocks.reshape(blocks.shape[0], nc, CMP_LEN * tok.shape[-1])
    return jax.nn.gelu(flat @ w1) @ w2


def nsa_attention(q, kc_tok, vc_tok, ks, vs, kw, vw, gates,
                  pe_k, w1_k, w2_k, pe_v, w1_v, w2_v, cos, sin):
    B, S, H, _ = q.shape
    q = apply_rope(q, cos, sin)
    kc = nsa_compress(apply_rope(kc_tok, cos, sin), pe_k, w1_k, w2_k)
    vc = nsa_compress(vc_tok, pe_v, w1_v, w2_v)
    ks = apply_rope(ks, cos, sin)
    kw = apply_rope(kw, cos, sin)
    nc = kc.shape[1]
    ns = S // SEL_LEN
    n_sel = min(SEL_N, ns)
    c_start = jnp.arange(nc) * CMP_STRIDE
    c_end = c_start + CMP_LEN - 1
    s_start = jnp.arange(ns) * SEL_LEN
    overlap = ((c_start[:, None] < s_start[None, :] + SEL_LEN)
               & (c_end[:, None] >= s_start[None, :])).astype(jnp.float32)
    ks_blk = ks.reshape(B, ns, SEL_LEN, NSA_DK)
    vs_blk = vs.reshape(B, ns, SEL_LEN, NSA_DV)
    kw_pad = jnp.pad(kw, ((0, 0), (WIN, 0), (0, 0)))
    vw_pad = jnp.pad(vw, ((0, 0), (WIN, 0), (0, 0)))
    scale = NSA_DK ** -0.5
    blk_ids = jnp.arange(ns)

    def block(qs):
        qb = lax.dynamic_slice_in_dim(q, qs, Q_BLOCK, axis=1)
        gb = lax.dynamic_slice_in_dim(gates, qs, Q_BLOCK, axis=1)
        t = qs + jnp.arange(Q_BLOCK)
        sc = jnp.einsum("bqhd,bnd->bhqn", qb, kc) * scale
        pc = masked_softmax(sc, c_end[None, :] <= t[:, None])
        o_cmp = jnp.einsum("bhqn,bnd->bqhd", pc.astype(vc.dtype), vc)
        imp = jnp.einsum("bhqn,nj->bqj", pc, overlap)
        cur = t // SEL_LEN
        forced = ((blk_ids[None, :] == 0) | (blk_ids[None, :] == cur[:, None])
                  | (blk_ids[None, :] == cur[:, None] - 1))
        visible = s_start[None, :] <= t[:, None]
        score = jnp.where(visible[None], jnp.where(forced[None], FORCE_SCORE, imp), NEG_INF)
        _, sel = lax.top_k(score, n_sel)
        kg = jax.vmap(lambda kb, ib: kb[ib])(ks_blk, sel).reshape(B, Q_BLOCK, n_sel * SEL_LEN, NSA_DK)
        vg = jax.vmap(lambda vb, ib: vb[ib])(vs_blk, sel).reshape(B, Q_BLOCK, n_sel * SEL_LEN, NSA_DV)
        pos = (sel[..., None] * SEL_LEN + jnp.arange(SEL_LEN)).reshape(B, Q_BLOCK, n_sel * SEL_LEN)
        ss = jnp.einsum("bqhd,bqkd->bhqk", qb, kg) * scale
        ps = masked_softmax(ss, (pos <= t[None, :, None])[:, None])
        o_slc = jnp.einsum("bhqk,bqkd->bqhd", ps.astype(vg.dtype), vg)
        kwb = lax.dynamic_slice_in_dim(kw_pad, qs, Q_BLOCK + WIN, axis=1)
        vwb = lax.dynamic_slice_in_dim(vw_pad, qs, Q_BLOCK + WIN, axis=1)
        spos = qs - WIN + jnp.arange(Q_BLOCK + WIN)
        dist = t[:, None] - spos[None, :]
        wmask = (spos[None, :] >= 0) & (dist >= 0) & (dist < WIN)
        sw = jnp.einsum("bqhd,bkd->bhqk", qb, kwb) * scale
        pw = masked_softmax(sw, wmask)
        o_win = jnp.einsum("bhqk,bkd->bqhd", pw.astype(vwb.dtype), vwb)
        return gb[..., 0:1] * o_cmp + gb[..., 1:2] * o_slc + gb[..., 2:3] * o_win

    return sweep_query_blocks(block, S).reshape(B, S, H * NSA_DV)


def mla_attention(c_q, c_kv, k_rope, q_norm_g, w_uq, kv_norm_g, w_ukv, cos, sin):
    B, S, _ = c_q.shape
    q = (rmsnorm(c_q, q_norm_g) @ w_uq).reshape(B, S, MLA_HEADS, MLA_NOPE + MLA_ROPE)
    q = jnp.concatenate([q[..., :MLA_NOPE], apply_rope(q[..., MLA_NOPE:], cos, sin)], axis=-1)
    kv = (rmsnorm(c_kv, kv_norm_g) @ w_ukv).reshape(B, S, MLA_HEADS, MLA_NOPE + MLA_V)
    k_pe = apply_rope(k_rope, cos, sin)
    k = jnp.concatenate([kv[..., :MLA_NOPE],
                         jnp.broadcast_to(k_pe[:, :, None, :], (B, S, MLA_HEADS, MLA_ROPE))], axis=-1)
    v = kv[..., MLA_NOPE:]
    o = dense_causal_attention(q, k, v, (MLA_NOPE + MLA_ROPE) ** -0.5)
    return o.reshape(B, S, MLA_HEADS * MLA_V)


def dsa_attention(q, k, v, iq, ik, iw, ik_norm_g, cos, sin, icos, isin):
    B, S, H, _ = q.shape
    q = apply_rope(q, cos, sin)
    k = apply_rope(k, cos, sin)
    iq = apply_rope(iq.reshape(B, S, IDX_HEADS, IDX_DIM), icos, isin)
    ik = apply_rope(rmsnorm(ik, ik_norm_g), icos, isin)
    iw = iw * (IDX_HEADS ** -0.5 * IDX_DIM ** -0.5)
    top = min(IDX_TOPK, S // 4)
    kpos = jnp.arange(S)
    scale = DSA_DIM ** -0.5

    def block(qs):
        t = qs + jnp.arange(Q_BLOCK)
        qb = lax.dynamic_slice_in_dim(q, qs, Q_BLOCK, axis=1)
        iqb = lax.dynamic_slice_in_dim(iq, qs, Q_BLOCK, axis=1)
        iwb = lax.dynamic_slice_in_dim(iw, qs, Q_BLOCK, axis=1)
        rel = jax.nn.relu(jnp.einsum("bqhd,bkd->bqhk", iqb, ik).astype(jnp.float32))
        idx_score = jnp.einsum("bqh,bqhk->bqk", iwb.astype(jnp.float32), rel)
        idx_score = jnp.where((kpos[None, :] <= t[:, None])[None], idx_score, NEG_INF)
        _, sel = lax.top_k(idx_score, top)
        kg = jax.vmap(lambda kk, ii: kk[ii])(k, sel)
        vg = jax.vmap(lambda vv, ii: vv[ii])(v, sel)
        s = jnp.einsum("bqhd,bqkhd->bhqk", qb, kg) * scale
        p = masked_softmax(s, (sel <= t[None, :, None])[:, None])
        return jnp.einsum("bhqk,bqkhd->bqhd", p.astype(vg.dtype), vg)

    return sweep_query_blocks(block, S).reshape(B, S, H * DSA_DIM)


def setup_inputs(seed: int = 0) -> dict:
    key = jax.random.key(seed)
    ks = jax.random.split(key, 25)
    L = DEPTH

    def nrm(k, shape, scale):
        return jax.random.normal(k, shape, jnp.float32) * scale

    def gain(k, shape):
        return 1.0 + 0.02 * jax.random.normal(k, shape, jnp.float32)

    return {
        "x": nrm(ks[0], (BATCH, SEQ, D_MODEL), 1.0),
        "norm1_g": gain(ks[1], (L, D_MODEL)),
        "w_in": nrm(ks[2], (L, D_MODEL, D_IN), D_MODEL ** -0.5),
        "diff_lq1": nrm(ks[3], (L, DA_DIM), 0.1),
        "diff_lk1": nrm(ks[4], (L, DA_DIM), 0.1),
        "diff_lq2": nrm(ks[5], (L, DA_DIM), 0.1),
        "diff_lk2": nrm(ks[6], (L, DA_DIM), 0.1),
        "diff_subln_g": gain(ks[7], (L, 2 * DA_DIM)),
        "nsa_pe_k": nrm(ks[8], (L, CMP_LEN, NSA_DK), 0.1),
        "nsa_w1_k": nrm(ks[9], (L, CMP_LEN * NSA_DK, NSA_DK), (CMP_LEN * NSA_DK) ** -0.5),
        "nsa_w2_k": nrm(ks[10], (L, NSA_DK, NSA_DK), NSA_DK ** -0.5),
        "nsa_pe_v": nrm(ks[11], (L, CMP_LEN, NSA_DV), 0.1),
        "nsa_w1_v": nrm(ks[12], (L, CMP_LEN * NSA_DV, NSA_DV), (CMP_LEN * NSA_DV) ** -0.5),
        "nsa_w2_v": nrm(ks[13], (L, NSA_DV, NSA_DV), NSA_DV ** -0.5),
        "mla_q_norm_g": gain(ks[14], (L, MLA_Q_LORA)),
        "mla_w_uq": nrm(ks[15], (L, MLA_Q_LORA, MLA_HEADS * (MLA_NOPE + MLA_ROPE)), MLA_Q_LORA ** -0.5),
        "mla_kv_norm_g": gain(ks[16], (L, MLA_KV_LORA)),
        "mla_w_ukv": nrm(ks[17], (L, MLA_KV_LORA, MLA_HEADS * (MLA_NOPE + MLA_V)), MLA_KV_LORA ** -0.5),
        "idx_k_norm_g": gain(ks[18], (L, IDX_DIM)),
        "w_branch": nrm(ks[19], (L, N_BRANCH, BR_WIDTH, D_MODEL), BR_WIDTH ** -0.5),
        "w_out": nrm(ks[20], (L, D_MODEL, D_MODEL), D_MODEL ** -0.5),
        "norm2_g": gain(ks[21], (L, D_MODEL)),
        "w_gate_up": nrm(ks[22], (L, D_MODEL, 2 * D_FF), D_MODEL ** -0.5),
        "w_down": nrm(ks[23], (L, D_FF, D_MODEL), D_FF ** -0.5),
        "final_norm_g": gain(ks[24], (D_MODEL,)),
    }


def reference(x, norm1_g, w_in, diff_lq1, diff_lk1, diff_lq2, diff_lk2, diff_subln_g,
              nsa_pe_k, nsa_w1_k, nsa_w2_k, nsa_pe_v, nsa_w1_v, nsa_w2_v,
              mla_q_norm_g, mla_w_uq, mla_kv_norm_g, mla_w_ukv, idx_k_norm_g,
              w_branch, w_out, norm2_g, w_gate_up, w_down, final_norm_g):
    B, S, _ = x.shape
    dt = x.dtype
    rot_da = rope_tables(S, DA_ROT, dt)
    rot_nsa = rope_tables(S, NSA_ROT, dt)
    rot_mla = rope_tables(S, MLA_ROPE, dt)
    rot_dsa = rope_tables(S, DSA_ROT, dt)
    rot_idx = rope_tables(S, IDX_ROT, dt)

    for l in range(DEPTH):
        h = rmsnorm(x, norm1_g[l])
        p = split_columns(h @ w_in[l])

        lam_init = 0.8 - 0.6 * math.exp(-0.3 * l)
        lam = (jnp.exp(jnp.sum(diff_lq1[l] * diff_lk1[l]).astype(jnp.float32))
               - jnp.exp(jnp.sum(diff_lq2[l] * diff_lk2[l]).astype(jnp.float32)) + lam_init)
        o_a = diff_attention(p["a_q"].reshape(B, S, DA_HEADS, 2, DA_DIM),
                             p["a_k"].reshape(B, S, DA_HEADS, 2, DA_DIM),
                             p["a_v"].reshape(B, S, DA_HEADS, 2 * DA_DIM),
                             lam, lam_init, diff_subln_g[l], *rot_da)

        o_b = nsa_attention(p["b_q"].reshape(B, S, NSA_HEADS, NSA_DK),
                            p["b_kc"], p["b_vc"], p["b_ks"], p["b_vs"], p["b_kw"], p["b_vw"],
                            jax.nn.sigmoid(p["b_g"].reshape(B, S, NSA_HEADS, 3)),
                            nsa_pe_k[l], nsa_w1_k[l], nsa_w2_k[l],
                            nsa_pe_v[l], nsa_w1_v[l], nsa_w2_v[l], *rot_nsa)

        o_c = mla_attention(p["c_q"], p["c_kv"], p["c_kr"], mla_q_norm_g[l], mla_w_uq[l],
                            mla_kv_norm_g[l], mla_w_ukv[l], *rot_mla)

        o_d = dsa_attention(p["d_q"].reshape(B, S, DSA_HEADS, DSA_DIM),
                            p["d_k"].reshape(B, S, DSA_HEADS, DSA_DIM),
                            p["d_v"].reshape(B, S, DSA_HEADS, DSA_DIM),
                            p["d_iq"], p["d_ik"], p["d_iw"], idx_k_norm_g[l],
                            *rot_dsa, *rot_idx)

        br = jnp.stack([o_a, o_b, o_c, o_d], axis=2)
        br = jnp.einsum("bsnw,nwd->bsnd", br, w_branch[l])
        g = jax.nn.sigmoid(p["gate"].reshape(B, S, N_BRANCH, D_MODEL))
        x = x + jnp.sum(g * br, axis=2) @ w_out[l]

        h2 = rmsnorm(x, norm2_g[l])
        gate, up = jnp.split(h2 @ w_gate_up[l], 2, axis=-1)
        x = x + (jax.nn.silu(gate) * up) @ w_down[l]

    return rmsnorm(x, final_norm_g)
```

```python
import math
from contextlib import ExitStack
import numpy as np
import ml_dtypes
import concourse.bass as bass
import concourse.mybir as mybir
from concourse.bass_utils import run_bass_kernel_spmd

F32 = mybir.dt.float32
BF16 = mybir.dt.bfloat16
AF = mybir.ActivationFunctionType
ALU = mybir.AluOpType
AX = mybir.AxisListType

S = 4096
D = 1024
NT = S // 128
DEPTH = 4
D_IN = 9748
DFF = 2816
NEG = -30000.0
EPOCH = 16000
DEPOCH = 1000
TINY = 1e-30

OFF = {}
_o = 0
for _nm, _n in (("a_q", 512), ("a_k", 512), ("a_v", 512), ("b_q", 512), ("b_kc", 128), ("b_vc", 128),
                ("b_ks", 128), ("b_vs", 128), ("b_kw", 128), ("b_vw", 128), ("b_g", 12), ("c_q", 384),
                ("c_kv", 256), ("c_kr", 64), ("d_q", 512), ("d_k", 512), ("d_v", 512), ("d_iq", 512),
                ("d_ik", 64), ("d_iw", 8), ("gate", 4096)):
    OFF[_nm] = _o
    _o += _n
assert _o == D_IN


_BUID = [0]


class Buf:
    __slots__ = ("name", "w", "rs", "dsem", "dbase", "dcnt", "uid")

    def __init__(self, name):
        self.name = name
        self.w = None
        self.rs = {}
        self.dsem = None
        self.dbase = 0
        self.dcnt = 0
        _BUID[0] += 1
        self.uid = _BUID[0]


class Tn:
    def __init__(self, t, name):
        self.t = t
        self.b = Buf(name)

    def __getitem__(self, k):
        return self.t[k]


class FreePool:
    def __init__(self, items):
        self.items = items
        self.free_list = list(items)

    def alloc(self):
        assert self.free_list, "FreePool exhausted"
        return self.free_list.pop(0)

    def free(self, x):
        self.free_list.append(x)


class Pool:
    def __init__(self, items):
        self.items = items
        self.i = 0

    def next(self):
        x = self.items[self.i % len(self.items)]
        self.i += 1
        return x


ENGS = ("pe", "act", "dve", "pool", "sp")


class Prog:
    def __init__(self, nc, es):
        self.nc = nc
        self.es = es
        self.streams = {e: [] for e in ENGS}
        self.seq = {e: 0 for e in ENGS}
        self.esem = {e: [] for e in ENGS}
        self.wd = {e: {} for e in ENGS}
        self.dirty = {}
        self.nsem = 0
        self.ident = None
        self.free_d = []
        self.maxval = 0
        self.scopes = []

    def _newsem(self, name):
        self.nsem += 1
        return self.es.enter_context(self.nc.semaphore(name))

    def _ev_sem(self, ev):
        if ev[0] == "e":
            _, eng, seq = ev
            ep = (seq - 1) // EPOCH
            while len(self.esem[eng]) <= ep:
                self.esem[eng].append(self._newsem(f"e_{eng}_{len(self.esem[eng])}"))
            return self.esem[eng][ep], (seq - 1) % EPOCH + 1
        _, buf, cnt = ev
        if buf.dsem is None:
            if self.free_d:
                buf.dsem, buf.dbase = self.free_d.pop(0)
            else:
                buf.dsem, buf.dbase = self._newsem(f"d_{self.nsem}"), 0
        v = buf.dbase + 16 * cnt
        self.maxval = max(self.maxval, v)
        assert v < 60000
        return buf.dsem, v

    def release(self, buf):
        if buf.dsem is not None:
            self.free_d.append((buf.dsem, buf.dbase + 16 * buf.dcnt))
            buf.dsem = None
            buf.dcnt = 0
            buf.dbase = 0

    def _wait(self, eng, ev, raw):
        if ev[0] == "e":
            if ev[1] == eng and (eng == "pe" or eng == "sp" or not raw):
                return
            key = ("e", ev[1])
            val = ev[2]
        else:
            key = ("d", ev[1].uid)
            val = ev[2]
        if self.wd[eng].get(key, 0) >= val:
            return
        self.wd[eng][key] = val
        sem, v = self._ev_sem(ev)
        self.streams[eng].append(("w", sem, v))

    def _deps(self, eng, reads, writes):
        for b in reads:
            if b.w is not None:
                self._wait(eng, b.w, True)
        for b in writes:
            if b.w is not None:
                self._wait(eng, b.w, False)
            for ev in b.rs.values():
                self._wait(eng, ev, False)

    def _mark(self, me, reads, writes):
        for b in reads:
            k = ("e", me[1]) if me[0] == "e" else ("d", me[1].uid)
            b.rs[k] = me
        for b in writes:
            b.w = me
            b.rs = {}

    def op(self, eng, fn, reads=(), writes=()):
        self._deps(eng, reads, writes)
        self.seq[eng] += 1
        me = ("e", eng, self.seq[eng])
        sem, _ = self._ev_sem(me)
        self.streams[eng].append(("o", fn, sem, 1))
        self._mark(me, reads, writes)

    def dma(self, out, in_, R=(), W=(), sb=None, q="sp", slow=False):
        assert sb is not None
        self._deps(q, R, W)
        if sb.dcnt > 0:
            self._wait(q, ("d", sb, sb.dcnt), False)
        sb.dcnt += 1
        me = ("d", sb, sb.dcnt)
        sem, _ = self._ev_sem(me)
        if slow:
            fn = lambda e: e.dma_start(out=out, in_=in_, allow_slow_non_contiguous=True)
        else:
            fn = lambda e: e.dma_start(out=out, in_=in_)
        self.streams[q].append(("o", fn, sem, 16))
        self.dirty[sb.uid] = sb
        self._mark(me, R, W)

    def barrier(self):
        for eng in ENGS:
            for x in ENGS:
                if x != eng and self.seq[x] > 0:
                    ev = ("e", x, self.seq[x])
                    key = ("e", x)
                    if self.wd[eng].get(key, 0) < ev[2]:
                        self.wd[eng][key] = ev[2]
                        sem, v = self._ev_sem(ev)
                        self.streams[eng].append(("w", sem, v))
            for b in self.dirty.values():
                self._wait(eng, ("d", b, b.dcnt), False)
        self.dirty = {}

    def replay(self, e, eng):
        for it in self.streams[eng]:
            if it[0] == "w":
                e.wait_ge(it[1], it[2])
            else:
                it[1](e).then_inc(it[2], it[3])

    def mm(self, out, lhsT, rhs, start, stop, R, W):
        self.op("pe", lambda e: e.matmul(out, lhsT=lhsT, rhs=rhs, start=start, stop=stop), R, W)

    def tr(self, out, in_, R, W):
        k = in_.shape[0]
        idn = self.ident[:k, :k]
        self.op("pe", lambda e: e.transpose(out=out, in_=in_, identity=idn), R, W)

    def act(self, out, in_, func, R, W, scale=1.0, bias=None, accum=None):
        kw = {}
        if bias is not None:
            kw["bias"] = bias
        if accum is not None:
            kw["accum_out"] = accum
        self.op("act", lambda e: e.activation(out=out, in_=in_, func=func, scale=scale, **kw), R, W)

    def tt(self, eng, out, in0, in1, op, R, W):
        self.op(eng, lambda e: e.tensor_tensor(out=out, in0=in0, in1=in1, op=op), R, W)

    def ts(self, eng, out, in0, s1, s2, op0, op1, R, W, accum=None):
        if op1 is None:
            self.op(eng, lambda e: e.tensor_scalar(out=out, in0=in0, scalar1=s1, scalar2=None, op0=op0), R, W)
        elif accum is None:
            self.op(eng, lambda e: e.tensor_scalar(out=out, in0=in0, scalar1=s1, scalar2=s2, op0=op0, op1=op1), R, W)
        else:
            self.op(eng, lambda e: e.tensor_scalar(out=out, in0=in0, scalar1=s1, scalar2=s2, op0=op0, op1=op1,
                                                   accum_out=accum), R, W)

    def stt(self, out, in0, scalar, in1, op0, op1, R, W):
        self.op("dve", lambda e: e.scalar_tensor_tensor(out=out, in0=in0, scalar=scalar, in1=in1, op0=op0, op1=op1),
                R, W)

    def copy(self, eng, out, in_, R, W):
        if eng == "act":
            self.op("act", lambda e: e.activation(out=out, in_=in_, func=AF.Copy), R, W)
        else:
            self.op(eng, lambda e: e.tensor_copy(out=out, in_=in_), R, W)

    def memset(self, eng, ap, val, W):
        self.op(eng, lambda e: e.memset(ap, val), (), W)

    def red(self, out, in_, op, R, W):
        self.op("dve", lambda e: e.tensor_reduce(out=out, in_=in_, axis=AX.X, op=op), R, W)


class Ctx:
    pass


_UNIQ = [0]


_CURP = [None]


def sb(nc, es, name, shape, dt):
    _UNIQ[0] += 1
    nm = f"s{_UNIQ[0]}_{name}"
    t = Tn(es.enter_context(nc.sbuf_tensor(nm, list(shape), dt)), nm)
    P = _CURP[0]
    if P is not None and P.scopes:
        P.scopes[-1].append(t.b)
    return t


class scope:
    def __enter__(self):
        self.P = _CURP[0]
        self.P.scopes.append([])
        self.es = ExitStack()
        return self.es.__enter__()

    def __exit__(self, *a):
        r = self.es.__exit__(*a)
        for b in self.P.scopes.pop():
            self.P.release(b)
        return r


def sbpool(nc, es, name, n, shape, dt):
    return Pool([sb(nc, es, f"{name}{i}", shape, dt) for i in range(n)])


def bcast_rows(ap1d_row, n=128):
    return ap1d_row.partition_broadcast(n)


def build(n_layers, first_layer=0, final_norm=True, debug=False, stop_after=None, branches="ABCD"):
    nc = bass.Bass("TRN2", target_bir_lowering=False)
    es_top = ExitStack()
    C = Ctx()
    C.nc = nc
    L = DEPTH

    def din(name, shape, dt=F32):
        return nc.dram_tensor(name, list(shape), dt, kind="ExternalInput").ap()

    okind = "ExternalOutput" if debug else "Internal"

    def dscr(name, shape, dt=BF16):
        return nc.dram_tensor(name, list(shape), dt, kind=okind).ap()

    I = {}
    I["x"] = din("x", [S, D])
    for nm, shp in (("norm1_g", [L, D]), ("w_in", [L, D, D_IN]), ("diff_lq1", [L, 64]), ("diff_lk1", [L, 64]),
                    ("diff_lq2", [L, 64]), ("diff_lk2", [L, 64]), ("diff_subln_g", [L, 128]),
                    ("nsa_pe_k", [L, 32, 128]), ("nsa_w1_k", [L, 4096, 128]), ("nsa_w2_k", [L, 128, 128]),
                    ("nsa_pe_v", [L, 32, 128]), ("nsa_w1_v", [L, 4096, 128]), ("nsa_w2_v", [L, 128, 128]),
                    ("mla_q_norm_g", [L, 384]), ("mla_w_uq", [L, 384, 768]), ("mla_kv_norm_g", [L, 256]),
                    ("mla_w_ukv", [L, 256, 1024]), ("idx_k_norm_g", [L, 64]), ("w_branch", [L, 4, 512, D]),
                    ("w_out", [L, D, D]), ("norm2_g", [L, D]), ("w_gate_up", [L, D, 2 * DFF]),
                    ("w_down", [L, DFF, D]), ("final_norm_g", [1, D])):
        I[nm] = din(nm, shp)
    I["c_ident"] = din("c_ident", [128, 128], BF16)
    I["c_causal"] = din("c_causal", [128, 128], BF16)
    I["c_anti"] = din("c_anti", [128, 128], BF16)
    I["c_cmask"] = din("c_cmask", [S, 256], BF16)
    I["c_selb"] = din("c_selb", [S, 64], F32)
    I["c_negbig"] = din("c_negbig", [128, 128], F32)
    I["c_posbig"] = din("c_posbig", [128, 128], F32)
    I["c_overlap"] = din("c_overlap", [256, 64], BF16)
    for nm, half in (("da", 8), ("nsa", 16), ("mla", 32), ("dsa", 16), ("idx", 8)):
        I["cos_" + nm] = din("cos_" + nm, [S, half])
        I["sin_" + nm] = din("sin_" + nm, [S, half])
    out = nc.dram_tensor("out", [S, D], F32, kind="ExternalOutput").ap()

    Sx = {}
    Sx["xa"] = dscr("xa", [S, D], F32)
    Sx["xb"] = dscr("xb", [S, D], F32)
    for nm, rows in (("QT_A", 512), ("KT_A", 512), ("QT_B", 512), ("bkT", 384), ("vcT", 128), ("QT_Cn", 512),
                     ("QT_Cr", 256), ("KT_Cn", 512), ("kpeT", 64), ("QT_D", 512), ("KT_D", 512), ("iqT", 512),
                     ("ikT", 64)):
        Sx[nm] = dscr(nm, [rows, S])
    for nm, cols in (("V_A", 512), ("vs", 128), ("vw", 128), ("V_C", 512), ("V_D", 512), ("O_A", 512),
                     ("O_B", 512), ("O_C", 512), ("O_D", 512)):
        Sx[nm] = dscr(nm, [S, cols])
    Sx["gB"] = dscr("gB", [S, 12], F32)
    Sx["iw"] = dscr("iw", [S, 8], F32)
    Sx["kcmpT"] = dscr("kcmpT", [128, 256])
    Sx["vcmp"] = dscr("vcmp", [256, 128])

    es = es_top
    P = Prog(nc, es)
    C.P = P
    _CURP[0] = P
    ident = sb(nc, es, "ident", [128, 128], BF16)
    P.ident = ident
    causal = sb(nc, es, "causal", [128, 128], BF16)
    anti = sb(nc, es, "anti", [128, 128], BF16)
    P.dma(ident[:], I["c_ident"][:, :], W=[ident.b], sb=ident.b)
    P.dma(causal[:], I["c_causal"][:, :], W=[causal.b], sb=causal.b)
    P.dma(anti[:], I["c_anti"][:, :], W=[anti.b], sb=anti.b)
    psf = Pool([Tn(es.enter_context(nc.psum_tensor(f"psf{i}", [128, 512], F32)), f"psf{i}") for i in range(6)])
    psb = Pool([Tn(es.enter_context(nc.psum_tensor(f"psb{i}", [128, 1024], BF16)), f"psb{i}") for i in range(2)])
    C.psf, C.psb = psf, psb
    C.small = sbpool(nc, es, "small", 8, [128, 4], F32)
    C.junk = sbpool(nc, es, "junk", 2, [128, 1024], F32)

    def rms(src_ap, n, R, eps=1e-6):
        ss = C.small.next()
        jk = C.junk.next()
        P.memset("dve", ss[:], 0.0, [ss.b])
        P.act(jk[:, :n], src_ap, AF.Square, R + [ss.b], [jk.b, ss.b], accum=ss[:, 0:1])
        P.ts("dve", ss[:, 2:3], ss[:, 0:1], 1.0 / n, eps, ALU.mult, ALU.add, [ss.b], [ss.b])
        P.act(ss[:, 3:4], ss[:, 2:3], AF.Ln, [ss.b], [ss.b])
        P.act(ss[:, 1:2], ss[:, 3:4], AF.Exp, [ss.b], [ss.b], scale=-0.5)
        return ss

    def load_bcast(es_, name, row_ap, n, q="sp"):
        t = sb(nc, es_, name, [128, n], F32)
        P.dma(t[:], row_ap.partition_broadcast(128), W=[t.b], sb=t.b, q=q)
        return t

    def load_w_bf16(es_, name, dram_ap, kc, ncols, stage_pool, chunk_cols=512):
        wt = sb(nc, es_, name, [128, kc, ncols], BF16)
        src = dram_ap.rearrange("(k p) n -> p k n", p=128)
        for k in range(kc):
            sw = stage_pool.items[0].t.shape[-1]
            for c0 in range(0, ncols, sw):
                w = min(sw, ncols - c0)
                st = stage_pool.next()
                P.dma(st[:, :w], src[:, k, c0:c0 + w], W=[st.b], sb=st.b)
                P.copy("pool", wt[:, k, c0:c0 + w], st[:, :w], [st.b], [wt.b])
        return wt

    for li in range(n_layers):
        l = first_layer + li
        xsrc = I["x"] if li == 0 else Sx["xa"]
        lam_init = 0.8 - 0.6 * math.exp(-0.3 * l)

        with scope() as esA:
            hT = sb(nc, esA, "hT", [128, 8, S], BF16)
            with scope() as es0:
                g1 = load_bcast(es0, "g1", I["norm1_g"][l:l + 1, :], D)
                xs = sbpool(nc, es0, "xs", 2, [128, D], F32)
                hbp = sbpool(nc, es0, "hb", 2, [128, D], BF16)
                for tt in range(NT):
                    xt = xs.next()
                    P.dma(xt[:], xsrc[tt * 128:(tt + 1) * 128, :], W=[xt.b], sb=xt.b)
                    ss = rms(xt[:], D, [xt.b])
                    hb = hbp.next()
                    P.stt(hb[:], xt[:], ss[:, 1:2], g1[:], ALU.mult, ALU.mult, [xt.b, ss.b, g1.b], [hb.b])
                    pb = psb.next()
                    for k in range(8):
                        P.tr(pb[:, k * 128:(k + 1) * 128], hb[:, k * 128:(k + 1) * 128], [hb.b, ident.b], [pb.b])
                    P.copy("dve", hT[:, :, tt * 128:(tt + 1) * 128], pb[:].rearrange("p (k c) -> p k c", k=8),
                           [pb.b], [hT.b])
                P.barrier()
            if stop_after == "A0":
                break
            with scope() as es1:
                wst = sbpool(nc, es1, "wst", 1, [128, 8, 512], F32)
                wbf = sbpool(nc, es1, "wbf", 2, [128, 8, 512], BF16)
                zp = sbpool(nc, es1, "z", 4, [128, 1024], F32)
                zbp = FreePool(sbpool(nc, es1, "zb", 12, [128, 1024], BF16).items)
                ztp = sbpool(nc, es1, "zt", 4, [128, 4, 128], BF16)
                rtmp = sbpool(nc, es1, "rtmp", 3, [128, 4, 128], F32)
                tabs = {}
                for nm, half in (("da", 8), ("nsa", 16), ("mla", 32), ("dsa", 16), ("idx", 8)):
                    ct = sb(nc, es1, "cos_" + nm, [128, NT, half], F32)
                    st = sb(nc, es1, "sin_" + nm, [128, NT, half], F32)
                    P.dma(ct[:], I["cos_" + nm].rearrange("(t p) h -> p t h", p=128), W=[ct.b], sb=ct.b)
                    P.dma(st[:], I["sin_" + nm].rearrange("(t p) h -> p t h", p=128), W=[st.b], sb=st.b)
                    tabs[nm] = (ct, st, half)
                gq = load_bcast(es1, "gq", I["mla_q_norm_g"][l:l + 1, :], 384)
                gkv = load_bcast(es1, "gkv", I["mla_kv_norm_g"][l:l + 1, :], 256)
                gik = load_bcast(es1, "gik", I["idx_k_norm_g"][l:l + 1, :], 64)
                wstage = sbpool(nc, es1, "wstage", 2, [128, 1024], F32)
                wuq = load_w_bf16(es1, "wuq", I["mla_w_uq"][l], 3, 768, wstage)
                wukv = load_w_bf16(es1, "wukv", I["mla_w_ukv"][l], 2, 1024, wstage)
                smallio = sbpool(nc, es1, "smallio", 4, [128, 16], F32)

                def rope(z, col0, G, dh, roff, kind, tt):
                    ct, st, half = tabs[kind]
                    v = z[:, col0:col0 + G * dh].rearrange("p (g d) -> p g d", g=G)
                    x1 = v[:, :, roff:roff + half]
                    x2 = v[:, :, roff + half:roff + 2 * half]
                    cc = ct[:, tt:tt + 1, :].to_broadcast([128, G, half])
                    sn = st[:, tt:tt + 1, :].to_broadcast([128, G, half])
                    tm = rtmp.next()
                    t = [tm[:, i, :G * half].rearrange("p (g h) -> p g h", g=G) for i in range(4)]
                    Rr = [z.b, ct.b, st.b]
                    P.tt("dve", t[0], x1, cc, ALU.mult, Rr, [tm.b])
                    P.tt("dve", t[1], x2, sn, ALU.mult, Rr, [tm.b])
                    P.tt("pool", t[2], x2, cc, ALU.mult, Rr, [tm.b])
                    P.tt("pool", t[3], x1, sn, ALU.mult, Rr, [tm.b])
                    P.tt("dve", x1, t[0], t[1], ALU.subtract, [tm.b], [z.b])
                    P.tt("dve", x2, t[2], t[3], ALU.add, [tm.b], [z.b])

                def store_T(zb, col0, ncols, dst, row0, tt):
                    pb = psb.next()
                    nj = (ncols + 127) // 128
                    for j in range(nj):
                        w = min(128, ncols - j * 128)
                        P.tr(pb[:w, j * 128:(j + 1) * 128], zb[:, col0 + j * 128:col0 + j * 128 + w],
                             [zb.b, ident.b], [pb.b])
                    zt = ztp.next()
                    if ncols >= 128:
                        P.copy("dve", zt[:, :nj, :], pb[:, :nj * 128].rearrange("p (j c) -> p j c", j=nj),
                               [pb.b], [zt.b])
                        P.dma(dst[row0:row0 + ncols, tt * 128:(tt + 1) * 128].rearrange("(j p) c -> p j c", p=128),
                              zt[:, :nj, :], R=[zt.b], sb=zt.b)
                    else:
                        P.copy("dve", zt[:ncols, 0, :], pb[:ncols, 0:128], [pb.b], [zt.b])
                        P.dma(dst[row0:row0 + ncols, tt * 128:(tt + 1) * 128], zt[:ncols, 0, :], R=[zt.b], sb=zt.b)

                def store_tok(zb, col0, ncols, dst, tt):
                    P.dma(dst[tt * 128:(tt + 1) * 128, :], zb[:, col0:col0 + ncols], R=[zb.b], sb=zb.b)

                def tobf(z, zb, c0, n):
                    P.copy("dve", zb[:, c0:c0 + n], z[:, c0:c0 + n], [z.b], [zb.b])

                def h_rope_T(G, dh, kind, dst):
                    def h(tt, z, n):
                        rope(z, 0, G, dh, 0, kind, tt)
                        zb = zbp.alloc()
                        tobf(z, zb, 0, n)
                        yield
                        store_T(zb, 0, n, Sx[dst], 0, tt)
                        zbp.free(zb)
                    return h

                def h_tok(dst):
                    def h(tt, z, n):
                        zb = zbp.alloc()
                        tobf(z, zb, 0, n)
                        store_tok(zb, 0, n, Sx[dst], tt)
                        zbp.free(zb)
                        return
                        yield
                    return h

                def h_bk(tt, z, n):
                    rope(z, 0, 3, 128, 0, "nsa", tt)
                    zb = zbp.alloc()
                    tobf(z, zb, 0, 384)
                    yield
                    store_T(zb, 0, 384, Sx["bkT"], 0, tt)
                    zbp.free(zb)

                def h_bv(tt, z, n):
                    zb = zbp.alloc()
                    tobf(z, zb, 0, 384)
                    yield
                    store_T(zb, 0, 128, Sx["vcT"], 0, tt)
                    P.dma(Sx["vs"][tt * 128:(tt + 1) * 128, :], zb[:, 128:256], R=[zb.b], sb=zb.b)
                    P.dma(Sx["vw"][tt * 128:(tt + 1) * 128, :], zb[:, 256:384], R=[zb.b], sb=zb.b)
                    zbp.free(zb)

                def norm_proj(z, c0, n, gt, wt, ncout, tt, res):
                    ss = rms(z[:, c0:c0 + n], n, [z.b])
                    zb = zbp.alloc()
                    P.stt(zb[:, :n], z[:, c0:c0 + n], ss[:, 1:2], gt[:], ALU.mult, ALU.mult, [z.b, ss.b, gt.b], [zb.b])
                    kc = n // 128
                    yield
                    pb = psb.next()
                    for k in range(kc):
                        P.tr(pb[:, k * 128:(k + 1) * 128], zb[:, k * 128:(k + 1) * 128], [zb.b, ident.b], [pb.b])
                    zbp.free(zb)
                    zt = ztp.next()
                    P.copy("dve", zt[:, :kc, :], pb[:, :kc * 128].rearrange("p (j c) -> p j c", j=kc), [pb.b], [zt.b])
                    z2 = zp.next()
                    for c in range(0, ncout, 512):
                        w = min(512, ncout - c)
                        ps = psf.next()
                        for k in range(kc):
                            P.mm(ps[:, :w], zt[:, k, :], wt[:, k, c:c + w], k == 0, k == kc - 1, [zt.b, wt.b], [ps.b])
                        P.copy("act", z2[:, c:c + w], ps[:, :w], [ps.b], [z2.b])
                    res.append(z2)

                def h_cq(tt, z, n):
                    sg = smallio.next()
                    P.act(sg[:, :12], z[:, 384:396], AF.Sigmoid, [z.b], [sg.b])
                    P.dma(Sx["gB"][tt * 128:(tt + 1) * 128, :], sg[:, :12], R=[sg.b], sb=sg.b)
                    res = []
                    yield from norm_proj(z, 0, 384, gq, wuq, 768, tt, res)
                    q = res[0]
                    rope(q, 0, 4, 192, 128, "mla", tt)
                    qb = zbp.alloc()
                    q3 = q[:, :768].rearrange("p (g d) -> p g d", g=4)
                    P.copy("pool", qb[:, 0:512].rearrange("p (g d) -> p g d", g=4), q3[:, :, 0:128], [q.b], [qb.b])
                    P.copy("pool", qb[:, 512:768].rearrange("p (g d) -> p g d", g=4), q3[:, :, 128:192], [q.b], [qb.b])
                    yield
                    store_T(qb, 0, 512, Sx["QT_Cn"], 0, tt)
                    store_T(qb, 512, 256, Sx["QT_Cr"], 0, tt)
                    zbp.free(qb)

                def h_ckv(tt, z, n):
                    rope(z, 256, 1, 64, 0, "mla", tt)
                    zb = zbp.alloc()
                    tobf(z, zb, 256, 64)
                    res = []
                    g_ = norm_proj(z, 0, 256, gkv, wukv, 1024, tt, res)
                    next(g_)
                    yield
                    store_T(zb, 256, 64, Sx["kpeT"], 0, tt)
                    zbp.free(zb)
                    for _ in g_:
                        pass
                    kv = res[0]
                    kb = zbp.alloc()
                    kv3 = kv[:, :1024].rearrange("p (g d) -> p g d", g=4)
                    P.copy("pool", kb[:, 0:512].rearrange("p (g d) -> p g d", g=4), kv3[:, :, 0:128], [kv.b], [kb.b])
                    P.copy("pool", kb[:, 512:1024].rearrange("p (g d) -> p g d", g=4), kv3[:, :, 128:256], [kv.b],
                           [kb.b])
                    yield
                    store_T(kb, 0, 512, Sx["KT_Cn"], 0, tt)
                    store_tok(kb, 512, 512, Sx["V_C"], tt)
                    zbp.free(kb)

                def h_ik(tt, z, n):
                    sg = smallio.next()
                    P.ts("dve", sg[:, :8], z[:, 64:72], (8 ** -0.5) * (64 ** -0.5), None, ALU.mult, None, [z.b], [sg.b])
                    P.dma(Sx["iw"][tt * 128:(tt + 1) * 128, :], sg[:, :8], R=[sg.b], sb=sg.b)
                    ss = rms(z[:, 0:64], 64, [z.b])
                    P.stt(z[:, 0:64], z[:, 0:64], ss[:, 1:2], gik[:], ALU.mult, ALU.mult, [z.b, ss.b, gik.b], [z.b])
                    rope(z, 0, 1, 64, 0, "idx", tt)
                    zb = zbp.alloc()
                    tobf(z, zb, 0, 64)
                    yield
                    store_T(zb, 0, 64, Sx["ikT"], 0, tt)
                    zbp.free(zb)

                chunks = [
                    ([("a_q", 512)], h_rope_T(8, 64, "da", "QT_A")),
                    ([("a_k", 512)], h_rope_T(8, 64, "da", "KT_A")),
                    ([("a_v", 512)], h_tok("V_A")),
                    ([("b_q", 512)], h_rope_T(4, 128, "nsa", "QT_B")),
                    ([("b_kc", 128), ("b_ks", 128), ("b_kw", 128)], h_bk),
                    ([("b_vc", 128), ("b_vs", 128), ("b_vw", 128)], h_bv),
                    ([("c_q", 384), ("b_g", 12)], h_cq),
                    ([("c_kv", 256), ("c_kr", 64)], h_ckv),
                    ([("d_q", 512)], h_rope_T(4, 128, "dsa", "QT_D")),
                    ([("d_k", 512)], h_rope_T(4, 128, "dsa", "KT_D")),
                    ([("d_v", 512)], h_tok("V_D")),
                    ([("d_iq", 512)], h_rope_T(8, 64, "idx", "iqT")),
                    ([("d_ik", 64), ("d_iw", 8)], h_ik),
                ]
                if stop_after == "A1a":
                    chunks = chunks[:3]
                wsrc = I["w_in"][l].rearrange("(k p) n -> p k n", p=128)

                def load_chunk(ci):
                    segs, _ = chunks[ci]
                    st = wst.next()
                    wb = wbf.next()
                    c = 0
                    for nm, n in segs:
                        P.dma(st[:, :, c:c + n], wsrc[:, :, OFF[nm]:OFF[nm] + n], W=[st.b], sb=st.b)
                        c += n
                    P.copy("pool", wb[:, :, :c], st[:, :, :c], [st.b], [wb.b])
                    return wb, c

                nxt = load_chunk(0)
                active = []

                def advance(flush=False):
                    while True:
                        keep = []
                        for it in active:
                            if it[1] > 0 and not flush:
                                it[1] -= 1
                                keep.append(it)
                                continue
                            try:
                                next(it[0])
                                it[1] = 1
                                keep.append(it)
                            except StopIteration:
                                pass
                        active[:] = keep
                        if not flush or not active:
                            break

                for ci in range(len(chunks)):
                    wb, n = nxt
                    if ci + 1 < len(chunks):
                        nxt = load_chunk(ci + 1)
                    handler = chunks[ci][1]
                    for tt in range(NT):
                        ps = psf.next()
                        for k in range(8):
                            P.mm(ps[:, :n], hT[:, k, tt * 128:(tt + 1) * 128], wb[:, k, :n], k == 0, k == 7,
                                 [hT.b, wb.b], [ps.b])
                        z = zp.next()
                        P.copy("act", z[:, :n], ps[:, :n], [ps.b], [z.b])
                        g_ = handler(tt, z, n)
                        try:
                            next(g_)
                            active.append([g_, 1])
                        except StopIteration:
                            pass
                        advance()
                advance(flush=True)
                P.barrier()
        if stop_after in ("A0", "A1a", "A1"):
            break
        with scope() as es2:
            tokp = sbpool(nc, es2, "ctok", 2, [128, S], BF16)
            w1st = sb(nc, es2, "w1st", [128, 32, 128], F32)
            w1bp = sbpool(nc, es2, "w1b", 2, [128, 32, 128], BF16)
            peTp = sbpool(nc, es2, "peT", 2, [128, 32], F32)
            w2stp = sbpool(nc, es2, "w2st", 2, [128, 128], F32)
            w2bp = sbpool(nc, es2, "w2b", 2, [128, 128], BF16)
            ctmp = sbpool(nc, es2, "ctmp", 3, [128, 256], BF16)
            gxp = sbpool(nc, es2, "gx", 2, [128, 256], F32)
            gx2p = sbpool(nc, es2, "gx2", 2, [128, 256], F32)
            gTp = sbpool(nc, es2, "gT", 2, [128, 256], BF16)
            cout = sbpool(nc, es2, "cout", 2, [128, 256], BF16)
            for kind in ("k", "v"):
                src = Sx["bkT"][0:128, :] if kind == "k" else Sx["vcT"][:, :]
                tk = tokp.next()
                P.dma(tk[:], src, W=[tk.b], sb=tk.b)
                P.dma(w1st[:], I["nsa_w1_" + kind][l].rearrange("(l d) o -> d l o", d=128), W=[w1st.b], sb=w1st.b)
                wb = w1bp.next()
                P.copy("pool", wb[:], w1st[:], [w1st.b], [wb.b])
                pt = peTp.next()
                P.dma(pt[:], I["nsa_pe_" + kind][l].rearrange("l d -> d l"), W=[pt.b], sb=pt.b, slow=True)
                w2s = w2stp.next()
                P.dma(w2s[:], I["nsa_w2_" + kind][l], W=[w2s.b], sb=w2s.b)
                w2 = w2bp.next()
                P.copy("pool", w2[:], w2s[:], [w2s.b], [w2.b])
                ps = psf.next()
                tk3 = tk[:].rearrange("p (b s) -> p b s", s=16)
                for lp in range(32):
                    tm = ctmp.next()
                    P.ts("dve", tm[:, :255], tk3[:, lp // 16:lp // 16 + 255, lp % 16], pt[:, lp:lp + 1], None,
                         ALU.add, None, [tk.b, pt.b], [tm.b])
                    P.mm(ps[:, :255], wb[:, lp, :], tm[:, :255], lp == 0, lp == 31, [wb.b, tm.b], [ps.b])
                gx = gxp.next()
                gx2 = gx2p.next()
                P.copy("act", gx[:, :255], ps[:, :255], [ps.b], [gx.b])
                P.tt("dve", gx2[:, :255], gx[:, :255], gx[:, :255], ALU.mult, [gx.b], [gx2.b])
                P.ts("dve", gx2[:, :255], gx2[:, :255], 0.044715, 1.0, ALU.mult, ALU.add, [gx2.b], [gx2.b])
                P.tt("dve", gx2[:, :255], gx2[:, :255], gx[:, :255], ALU.mult, [gx2.b, gx.b], [gx2.b])
                P.act(gx2[:, :255], gx2[:, :255], AF.Sigmoid, [gx2.b], [gx2.b], scale=2.0 * math.sqrt(2.0 / math.pi))
                gT = gTp.next()
                P.memset("dve", gT[:], 0.0, [gT.b])
                P.tt("dve", gT[:, :255], gx[:, :255], gx2[:, :255], ALU.mult, [gx.b, gx2.b], [gT.b])
                co = cout.next()
                if kind == "k":
                    ps2 = psf.next()
                    P.mm(ps2[:, :256], w2[:], gT[:, :256], True, True, [w2.b, gT.b], [ps2.b])
                    P.copy("dve", co[:], ps2[:, :256], [ps2.b], [co.b])
                    P.dma(Sx["kcmpT"][:, :], co[:], R=[co.b], sb=co.b)
                else:
                    for g in range(2):
                        ps2 = psf.next()
                        P.mm(ps2[:, :128], gT[:, g * 128:(g + 1) * 128], w2[:], True, True, [w2.b, gT.b], [ps2.b])
                        P.copy("dve", co[:, g * 128:(g + 1) * 128], ps2[:, :128], [ps2.b], [co.b])
                    P.dma(Sx["vcmp"].rearrange("(g p) d -> p g d", p=128), co[:].rearrange("p (g d) -> p g d", g=2),
                          R=[co.b], sb=co.b)
            P.barrier()
        if stop_after == "A2":
            break

        psc = Pool(psf.items[0:2])
        accp = Pool(psf.items[2:6])

        class Job:
            def __init__(self, scores, exp, pv, post=None, pre=None):
                self.scores, self.exp, self.pv, self.post, self.pre = scores, exp, pv, post, pre

        def run_jobs(jobs):
            prev = None
            for j in jobs:
                if j.pre is not None:
                    if prev is not None:
                        prev.pv()
                        if prev.post is not None:
                            prev.post()
                        prev = None
                    j.pre()
                j.scores()
                j.exp()
                if prev is not None:
                    prev.pv()
                    if prev.post is not None:
                        prev.post()
                prev = j
            if prev is not None:
                prev.pv()
                if prev.post is not None:
                    prev.post()

        def head_jobs(ptp, score_fn, mask_fn, v_fn, acc, nv, kts, scale, post=None, pre=None):
            kts = list(kts)
            chs = [kts[i:i + 4] for i in range(0, len(kts), 4)]
            for ci, ch in enumerate(chs):
                st = {}

                def scores(ch=ch, st=st):
                    sbk = psc.next()
                    st["sb"] = sbk
                    for j, kt in enumerate(ch):
                        terms = list(score_fn(kt))
                        mks = mask_fn(kt)
                        terms.extend(mks)
                        for i, (lt, rh, bufs) in enumerate(terms):
                            P.mm(sbk[:, j * 128:(j + 1) * 128], lt, rh, i == 0, i == len(terms) - 1, bufs, [sbk.b])

                def exp(ch=ch, st=st):
                    pt = ptp.next()
                    st["pt"] = pt
                    n = len(ch) * 128
                    P.act(pt[:, :n], st["sb"][:, :n], AF.Exp, [st["sb"].b], [pt.b], scale=scale)

                def pv(ch=ch, st=st, ci=ci):
                    for j, kt in enumerate(ch):
                        va, vb = v_fn(kt)
                        P.mm(acc[:, :nv], st["pt"][:, j * 128:(j + 1) * 128], va, ci == 0 and j == 0,
                             ci == len(chs) - 1 and j == len(ch) - 1, [st["pt"].b] + vb, [acc.b])

                yield Job(scores, exp, pv, post if ci == len(chs) - 1 else None, pre if ci == 0 else None)

        def recip_sum(acc, col, dst_ap, dst_buf):
            P.ts("dve", dst_ap, acc[:, col:col + 1], TINY, None, ALU.add, None, [acc.b], [dst_buf])
            P.op("dve", lambda e: e.reciprocal(out=dst_ap, in_=dst_ap), [dst_buf], [dst_buf])

        def load_vaug(V, src, c0, nvv=129):
            P.dma(V[:, :, 0:128], src[:, c0:c0 + 128].rearrange("(t p) c -> p t c", p=128), W=[V.b], sb=V.b)
            P.memset("pool", V[:, :, 128:129], 1.0, [V.b])

        if "A" in branches:
          with scope() as esb:
            ptp = sbpool(nc, esb, "pt", 3, [128, 512], BF16)
            Kp = sbpool(nc, esb, "K", 2, [128, S], BF16)
            Qp = sbpool(nc, esb, "Q", 2, [128, S], BF16)
            Vp = sbpool(nc, esb, "V", 2, [128, NT, 129], BF16)
            of = sbpool(nc, esb, "of", 3, [128, 128], F32)
            obp = sbpool(nc, esb, "ob", 3, [128, 128], BF16)
            lqs = [load_bcast(esb, nm, I[nm][l:l + 1, :], 64) for nm in ("diff_lq1", "diff_lk1", "diff_lq2", "diff_lk2")]
            lam = sb(nc, esb, "lam", [128, 8], F32)
            ltmp = sb(nc, esb, "ltmp", [128, 64], F32)
            for i in range(2):
                P.tt("dve", ltmp[:], lqs[2 * i][:], lqs[2 * i + 1][:], ALU.mult, [lqs[2 * i].b, lqs[2 * i + 1].b], [ltmp.b])
                P.red(lam[:, i:i + 1], ltmp[:], ALU.add, [ltmp.b], [lam.b])
            P.act(lam[:, 2:4], lam[:, 0:2], AF.Exp, [lam.b], [lam.b])
            P.tt("dve", lam[:, 4:5], lam[:, 3:4], lam[:, 2:3], ALU.subtract, [lam.b], [lam.b])
            P.ts("dve", lam[:, 5:6], lam[:, 4:5], -lam_init, None, ALU.add, None, [lam.b], [lam.b])
            gsub = load_bcast(esb, "gsub", I["diff_subln_g"][l:l + 1, :], 128)
            P.ts("dve", gsub[:], gsub[:], 1.0 - lam_init, None, ALU.mult, None, [gsub.b], [gsub.b])

            def jobsA():
                for h in range(4):
                    K, Q, V = Kp.next(), Qp.next(), Vp.next()
                    P.dma(K[:], Sx["KT_A"][h * 128:(h + 1) * 128, :], W=[K.b], sb=K.b)
                    P.dma(Q[:], Sx["QT_A"][h * 128:(h + 1) * 128, :], W=[Q.b], sb=Q.b)
                    load_vaug(V, Sx["V_A"], h * 128)
                    for qt in range(NT):
                        accs = [accp.next(), accp.next()]

                        def post(h=h, qt=qt, accs=accs):
                            r = C.small.next()
                            recip_sum(accs[0], 128, r[:, 0:1], r.b)
                            recip_sum(accs[1], 128, r[:, 1:2], r.b)
                            P.tt("dve", r[:, 1:2], r[:, 1:2], lam[:, 5:6], ALU.mult, [r.b, lam.b], [r.b])
                            o = of.next()
                            P.ts("dve", o[:], accs[0][:, 0:128], r[:, 0:1], None, ALU.mult, None, [accs[0].b, r.b], [o.b])
                            P.stt(o[:], accs[1][:, 0:128], r[:, 1:2], o[:], ALU.mult, ALU.add, [accs[1].b, r.b, o.b], [o.b])
                            ss = rms(o[:], 128, [o.b])
                            ob = obp.next()
                            P.stt(ob[:], o[:], ss[:, 1:2], gsub[:], ALU.mult, ALU.mult, [o.b, ss.b, gsub.b], [ob.b])
                            P.dma(Sx["O_A"][qt * 128:(qt + 1) * 128, h * 128:(h + 1) * 128], ob[:], R=[ob.b], sb=ob.b)

                        for m in range(2):
                            def score_fn(kt, m=m, K=K, Q=Q, qt=qt):
                                return [(K[m * 64:(m + 1) * 64, kt * 128:(kt + 1) * 128],
                                         Q[m * 64:(m + 1) * 64, qt * 128:(qt + 1) * 128], [K.b, Q.b])]

                            def mask_fn(kt, qt=qt):
                                return [(causal[:], ident[:], [causal.b, ident.b])] if kt == qt else []

                            def v_fn(kt, V=V):
                                return V[:, kt, :], [V.b]

                            yield from head_jobs(ptp, score_fn, mask_fn, v_fn, accs[m], 129, range(qt + 1),
                                                 64 ** -0.5, post if m == 1 else None)
            run_jobs(jobsA())
            P.barrier()
        if stop_after == "BA":
            break

        if "C" in branches:
          with scope() as esb:
            ptp = sbpool(nc, esb, "pt", 3, [128, 512], BF16)
            Kp = sbpool(nc, esb, "K", 2, [128, S], BF16)
            Qp = sbpool(nc, esb, "Q", 2, [128, S], BF16)
            Qrp = sbpool(nc, esb, "Qr", 2, [64, S], BF16)
            Vp = sbpool(nc, esb, "V", 2, [128, NT, 129], BF16)
            kpe = sb(nc, esb, "kpe", [64, S], BF16)
            obp = sbpool(nc, esb, "ob", 3, [128, 128], BF16)
            P.dma(kpe[:], Sx["kpeT"][:, :], W=[kpe.b], sb=kpe.b)

            def jobsC():
                for h in range(4):
                    K, Q, Qr, V = Kp.next(), Qp.next(), Qrp.next(), Vp.next()
                    P.dma(K[:], Sx["KT_Cn"][h * 128:(h + 1) * 128, :], W=[K.b], sb=K.b)
                    P.dma(Q[:], Sx["QT_Cn"][h * 128:(h + 1) * 128, :], W=[Q.b], sb=Q.b)
                    P.dma(Qr[:], Sx["QT_Cr"][h * 64:(h + 1) * 64, :], W=[Qr.b], sb=Qr.b)
                    load_vaug(V, Sx["V_C"], h * 128)
                    for qt in range(NT):
                        acc = accp.next()

                        def post(h=h, qt=qt, acc=acc):
                            r = C.small.next()
                            recip_sum(acc, 128, r[:, 0:1], r.b)
                            ob = obp.next()
                            P.ts("dve", ob[:], acc[:, 0:128], r[:, 0:1], None, ALU.mult, None, [acc.b, r.b], [ob.b])
                            P.dma(Sx["O_C"][qt * 128:(qt + 1) * 128, h * 128:(h + 1) * 128], ob[:], R=[ob.b], sb=ob.b)

                        def score_fn(kt, K=K, Q=Q, Qr=Qr, qt=qt):
                            return [(K[:, kt * 128:(kt + 1) * 128], Q[:, qt * 128:(qt + 1) * 128], [K.b, Q.b]),
                                    (kpe[:, kt * 128:(kt + 1) * 128], Qr[:, qt * 128:(qt + 1) * 128], [kpe.b, Qr.b])]

                        def mask_fn(kt, qt=qt):
                            return [(causal[:], ident[:], [causal.b, ident.b])] if kt == qt else []

                        def v_fn(kt, V=V):
                            return V[:, kt, :], [V.b]

                        yield from head_jobs(ptp, score_fn, mask_fn, v_fn, acc, 129, range(qt + 1), 192 ** -0.5, post)
            run_jobs(jobsC())
            P.barrier()
        if stop_after == "BC":
            break

        if "D" in branches:
          with scope() as esb:
            ptp = sbpool(nc, esb, "pt", 3, [128, 512], BF16)
            iqp = sbpool(nc, esb, "iqt", 4, [128, 4, 128], BF16)
            rbAp = sbpool(nc, esb, "rbA", 2, [128, 4, 256], BF16)
            rbDp = sbpool(nc, esb, "rbD", 2, [128, 4, 256], BF16)
            wdp = sbpool(nc, esb, "wd", 2, [128, 8, 128], BF16)
            bjunk2 = sb(nc, esb, "bjunk2", [128, S], BF16)
            ik2 = sb(nc, esb, "ik2", [128, S], BF16)
            qdp = sbpool(nc, esb, "qd", 3, [128, 4, 128], BF16)
            Kd = [sb(nc, esb, f"Kd{i}", [128, S], BF16) for i in range(4)]
            Vd = [sb(nc, esb, f"Vd{i}", [128, NT, 129], BF16) for i in range(4)]
            iwt = sb(nc, esb, "iwt", [128, NT, 8], F32)
            idxp = sbpool(nc, esb, "idx", 2, [128, S], F32)
            Mkp = sbpool(nc, esb, "Mk", 4, [128, S], BF16)
            bjunk = sb(nc, esb, "bjunk", [128, S], BF16)
            negbig = sb(nc, esb, "negbig", [128, 128], F32)
            posbig = sb(nc, esb, "posbig", [128, 128], F32)
            dtmpp = sbpool(nc, esb, "dtmp", 2, [128, 128], F32)
            bs = sbpool(nc, esb, "bs", 4, [128, 32], F32)
            obp = sbpool(nc, esb, "ob", 3, [128, 128], BF16)
            P.dma(negbig[:], I["c_negbig"][:, :], W=[negbig.b], sb=negbig.b)
            P.dma(posbig[:], I["c_posbig"][:, :], W=[posbig.b], sb=posbig.b)
            P.dma(iwt[:], Sx["iw"].rearrange("(t p) h -> p t h", p=128), W=[iwt.b], sb=iwt.b)
            P.dma(ik2[0:64, :], Sx["ikT"][:, :], W=[ik2.b], sb=ik2.b)
            P.dma(ik2[64:128, :], Sx["ikT"][:, :], W=[ik2.b], sb=ik2.b)
            for i in range(4):
                P.dma(Kd[i][:], Sx["KT_D"][i * 128:(i + 1) * 128, :], W=[Kd[i].b], sb=Kd[i].b)
                load_vaug(Vd[i], Sx["V_D"], i * 128)
            NBIS = 16

            paccp = Pool([psf.items[5]])
            accp = Pool(psf.items[2:5])
            CW = 256

            def idx_accum(qt):
                Lq = (qt + 1) * 128
                idx = idxp.next()
                iq = iqp.next()
                P.dma(iq[:], Sx["iqT"][:, qt * 128:(qt + 1) * 128].rearrange("(g p) c -> p g c", p=128),
                      W=[iq.b], sb=iq.b)
                wd = wdp.next()
                for h in range(8):
                    P.ts("dve", wd[:, h, :], ident[:], iwt[:, qt, h:h + 1], None, ALU.mult, None,
                         [ident.b, iwt.b], [wd.b])
                for c0 in range(0, Lq, CW):
                    w = min(CW, Lq - c0)
                    rbs = (rbAp.next(), rbDp.next())
                    for h in range(8):
                        pbk = psb.next()
                        psv = pbk[:].bitcast(F32)
                        p0 = (h % 2) * 64
                        P.mm(psv[:, :w], iq[p0:p0 + 64, h // 2, :], ik2[p0:p0 + 64, c0:c0 + w],
                             True, True, [iq.b, ik2.b], [pbk.b])
                        rb = rbs[(h // 2) % 2]
                        sl = (h // 4) * 2 + h % 2
                        P.act(rb[:, sl, :w], psv[:, :w], AF.Relu, [pbk.b], [rb.b])
                    pacc = paccp.next()
                    for h in range(8):
                        rb = rbs[(h // 2) % 2]
                        sl = (h // 4) * 2 + h % 2
                        P.mm(pacc[:, :w], wd[:, h, :], rb[:, sl, :w], h == 0, h == 7, [wd.b, rb.b], [pacc.b])
                    P.copy("dve", idx[:, c0:c0 + w], pacc[:, :w], [pacc.b], [idx.b])
                b = bs.next()
                d0 = qt * 128
                if qt >= 2:
                    dtmp = dtmpp.next()
                    P.tt("dve", dtmp[:], idx[:, d0:Lq], posbig[:], ALU.add, [idx.b, posbig.b], [dtmp.b])
                    P.red(b[:, 0:1], dtmp[:], ALU.min, [dtmp.b], [b.b])
                    P.red(b[:, 1:2], idx[:, 0:d0], ALU.min, [idx.b], [b.b])
                    P.tt("dve", b[:, 0:1], b[:, 0:1], b[:, 1:2], ALU.min, [b.b], [b.b])
                P.tt("dve", idx[:, d0:Lq], idx[:, d0:Lq], negbig[:], ALU.add, [idx.b, negbig.b], [idx.b])
                if qt >= 2:
                    P.red(b[:, 1:2], idx[:, 0:Lq], ALU.max, [idx.b], [b.b])
                    P.tt("dve", b[:, 2:3], b[:, 1:2], b[:, 0:1], ALU.subtract, [b.b], [b.b])
                    P.memset("dve", b[:, 8:8 + NBIS], 0.0, [b.b])
                else:
                    P.memset("dve", b[:, 0:1], -1e29, [b.b])
                return dict(qt=qt, Lq=Lq, idx=idx, b=b)

            def bisect(states):
                sts = [x for x in states if x["qt"] >= 2]
                for it in range(NBIS):
                    f = 0.5 ** (it + 1)
                    for ci, x in enumerate(sts):
                        b = x["b"]
                        if ci == 0:
                            P.stt(b[:, 3:4], b[:, 2:3], -f, b[:, 0:1], ALU.mult, ALU.subtract, [b.b], [b.b])
                        else:
                            P.stt(b[:, 3:4], b[:, 2:3], f, b[:, 0:1], ALU.mult, ALU.add, [b.b], [b.b])
                    for ci, x in enumerate(sts):
                        b, idx, Lq = x["b"], x["idx"], x["Lq"]
                        if ci == 0:
                            P.act(bjunk[:, :Lq], idx[:, :Lq], AF.Sign, [idx.b, b.b], [bjunk.b, b.b], bias=b[:, 3:4],
                                  accum=b[:, 8 + it:9 + it])
                        else:
                            P.ts("dve", bjunk2[:, :Lq], idx[:, :Lq], b[:, 3:4], 0.0, ALU.is_ge, ALU.add, [idx.b, b.b],
                                 [bjunk2.b, b.b], accum=b[:, 8 + it:9 + it])
                    for ci, x in enumerate(sts):
                        b, Lq = x["b"], x["Lq"]
                        thr = (510.5 - Lq) if ci == 0 else 255.5
                        P.ts("dve", b[:, 5:6], b[:, 8 + it:9 + it], thr, f, ALU.is_ge, ALU.mult, [b.b], [b.b])
                        P.stt(b[:, 0:1], b[:, 5:6], b[:, 2:3], b[:, 0:1], ALU.mult, ALU.add, [b.b], [b.b])

            def indexer_pair(p):
                states = [idx_accum(qt) for qt in (2 * p, 2 * p + 1)]
                bisect(states)
                res = {}
                for x in states:
                    Mk = Mkp.next()
                    P.ts("pool", Mk[:, :x["Lq"]], x["idx"][:, :x["Lq"]], x["b"][:, 0:1], NEG, ALU.is_lt, ALU.mult,
                         [x["idx"].b, x["b"].b], [Mk.b])
                    res[x["qt"]] = Mk
                return res

            def jobsD():
                mks = dict(indexer_pair(0))
                for qt in range(NT):
                    st = {"Mk": mks[qt]}
                    qd = qdp.next()
                    P.dma(qd[:], Sx["QT_D"][:, qt * 128:(qt + 1) * 128].rearrange("(h p) c -> p h c", p=128),
                          W=[qd.b], sb=qd.b)

                    def pre(qt=qt, mks=mks):
                        if qt + 2 < NT:
                            mks.update(indexer_pair(qt // 2 + 1))

                    for h in range(4):
                        acc = accp.next()

                        def post(h=h, qt=qt, acc=acc):
                            r = C.small.next()
                            recip_sum(acc, 128, r[:, 0:1], r.b)
                            ob = obp.next()
                            P.ts("dve", ob[:], acc[:, 0:128], r[:, 0:1], None, ALU.mult, None, [acc.b, r.b], [ob.b])
                            P.dma(Sx["O_D"][qt * 128:(qt + 1) * 128, h * 128:(h + 1) * 128], ob[:], R=[ob.b], sb=ob.b)

                        def score_fn(kt, h=h, qd=qd):
                            return [(Kd[h][:, kt * 128:(kt + 1) * 128], qd[:, h, :], [Kd[h].b, qd.b])]

                        def mask_fn(kt, st=st):
                            Mk = st["Mk"]
                            return [(Mk[:, kt * 128:(kt + 1) * 128], ident[:], [Mk.b, ident.b])]

                        def v_fn(kt, h=h):
                            return Vd[h][:, kt, :], [Vd[h].b]

                        yield from head_jobs(ptp, score_fn, mask_fn, v_fn, acc, 129, range(qt + 1), 128 ** -0.5, post,
                                             pre if (h == 0 and qt % 2 == 0) else None)
            run_jobs(jobsD())
            P.barrier()
        if stop_after == "BD":
            break

        if "B" in branches:
          accp = Pool(psf.items[2:6])
          with scope() as esb:
            ptp = sbpool(nc, esb, "pt", 3, [128, 512], BF16)
            Qb = [sb(nc, esb, f"Qb{i}", [128, S], BF16) for i in range(4)]
            ks = sb(nc, esb, "ks", [128, S], BF16)
            kw = sb(nc, esb, "kw", [128, S], BF16)
            vsa = sb(nc, esb, "vsa", [128, NT, 129], BF16)
            vwa = sb(nc, esb, "vwa", [128, NT, 129], BF16)
            kcm = sb(nc, esb, "kcm", [128, 256], BF16)
            vca = sb(nc, esb, "vca", [128, 2, 193], BF16)
            gBt = sb(nc, esb, "gBt", [128, NT, 12], F32)
            selb = sb(nc, esb, "selb", [128, NT, 64], F32)
            cmp_ = sbpool(nc, esb, "cm", 2, [128, 256], BF16)
            Mkp = sbpool(nc, esb, "MkB", 2, [128, S], BF16)
            obf = sbpool(nc, esb, "obf", 2, [128, 4, 128], F32)
            impp = sbpool(nc, esb, "imp", 2, [128, 64], F32)
            scp = sbpool(nc, esb, "sc", 2, [128, 64], F32)
            cmpm = sb(nc, esb, "cmpm", [128, 64, 64], F32)
            rank = sbpool(nc, esb, "rank", 2, [128, 64], F32)
            obp = sbpool(nc, esb, "ob", 3, [128, 128], BF16)
            coefp = sbpool(nc, esb, "coef", 8, [128, 2], F32)
            for i in range(4):
                P.dma(Qb[i][:], Sx["QT_B"][i * 128:(i + 1) * 128, :], W=[Qb[i].b], sb=Qb[i].b)
            P.dma(ks[:], Sx["bkT"][128:256, :], W=[ks.b], sb=ks.b)
            P.dma(kw[:], Sx["bkT"][256:384, :], W=[kw.b], sb=kw.b)
            load_vaug(vsa, Sx["vs"], 0)
            load_vaug(vwa, Sx["vw"], 0)
            P.dma(kcm[:], Sx["kcmpT"][:, :], W=[kcm.b], sb=kcm.b)
            P.dma(vca[:, :, 0:128], Sx["vcmp"].rearrange("(g p) d -> p g d", p=128), W=[vca.b], sb=vca.b)
            P.memset("pool", vca[:, :, 128:129], 1.0, [vca.b])
            P.dma(vca[:, :, 129:193], I["c_overlap"].rearrange("(g p) j -> p g j", p=128), W=[vca.b], sb=vca.b)
            P.dma(gBt[:], Sx["gB"].rearrange("(t p) g -> p t g", p=128), W=[gBt.b], sb=gBt.b)
            P.dma(selb[:], I["c_selb"].rearrange("(t p) j -> p t j", p=128), W=[selb.b], sb=selb.b)

            def jobsB():
                QS = {}

                def setup(qt):
                    Lq = (qt + 1) * 128
                    cm = cmp_.next()
                    P.dma(cm[:], I["c_cmask"][qt * 128:(qt + 1) * 128, :], W=[cm.b], sb=cm.b)
                    of4 = obf.next()
                    imp = impp.next()
                    st = {}

                    def coef(acc, col, gcol, qt=qt):
                        cf = coefp.next()
                        recip_sum(acc, col, cf[:, 0:1], cf.b)
                        P.tt("dve", cf[:, 1:2], cf[:, 0:1], gBt[:, qt, gcol:gcol + 1], ALU.mult, [cf.b, gBt.b], [cf.b])
                        return cf
                    QS[qt] = (Lq, cm, of4, imp, st, coef)

                def cmp_jobs(qt):
                    Lq, cm, of4, imp, st, coef = QS[qt]
                    for h in range(4):
                        acc = accp.next()

                        def post(h=h, acc=acc, of4=of4, imp=imp, coef=coef):
                            cf = coef(acc, 128, 3 * h + 0)
                            P.ts("dve", of4[:, h, :], acc[:, 0:128], cf[:, 1:2], None, ALU.mult, None, [acc.b, cf.b], [of4.b])
                            if h == 0:
                                P.ts("dve", imp[:], acc[:, 129:193], cf[:, 0:1], None, ALU.mult, None, [acc.b, cf.b], [imp.b])
                            else:
                                P.stt(imp[:], acc[:, 129:193], cf[:, 0:1], imp[:], ALU.mult, ALU.add,
                                      [acc.b, cf.b, imp.b], [imp.b])

                        def score_fn(kt, h=h, qt=qt):
                            return [(kcm[:, kt * 128:(kt + 1) * 128], Qb[h][:, qt * 128:(qt + 1) * 128], [kcm.b, Qb[h].b])]

                        def mask_fn(kt, cm=cm):
                            return [(cm[:, kt * 128:(kt + 1) * 128], ident[:], [cm.b, ident.b])]

                        def v_fn(kt):
                            return vca[:, kt, :], [vca.b]

                        yield from head_jobs(ptp, score_fn, mask_fn, v_fn, acc, 193, range(2), 128 ** -0.5, post)


                def make_select(qt):
                    Lq, cm, of4, imp, st, coef = QS[qt]
                    def select(qt=qt, imp=imp, st=st, Lq=Lq):
                        sc = scp.next()
                        P.tt("dve", sc[:], imp[:], selb[:, qt, :], ALU.add, [imp.b, selb.b], [sc.b])
                        P.tt("dve", cmpm[:], sc[:].unsqueeze(1).to_broadcast([128, 64, 64]),
                             sc[:].unsqueeze(2).to_broadcast([128, 64, 64]), ALU.is_gt, [sc.b], [cmpm.b])
                        rk = rank.next()
                        P.red(rk[:], cmpm[:], ALU.add, [cmpm.b], [rk.b])
                        P.ts("dve", rk[:], rk[:], 15.5, NEG, ALU.is_gt, ALU.mult, [rk.b], [rk.b])
                        Mk = Mkp.next()
                        nb = Lq // 64
                        P.copy("pool", Mk[:, :Lq].rearrange("p (j c) -> p j c", c=64),
                               rk[:, :nb].unsqueeze(2).to_broadcast([128, nb, 64]), [rk.b], [Mk.b])
                        P.tt("pool", Mk[:, Lq - 128:Lq], Mk[:, Lq - 128:Lq], causal[:], ALU.add, [Mk.b, causal.b], [Mk.b])
                        st["Mk"] = Mk

                    return select

                def slc_jobs(qt):
                    Lq, cm, of4, imp, st, coef = QS[qt]
                    for h in range(4):
                        acc = accp.next()

                        def post(h=h, acc=acc, of4=of4, coef=coef):
                            cf = coef(acc, 128, 3 * h + 1)
                            P.stt(of4[:, h, :], acc[:, 0:128], cf[:, 1:2], of4[:, h, :], ALU.mult, ALU.add,
                                  [acc.b, cf.b, of4.b], [of4.b])

                        def score_fn(kt, h=h, qt=qt):
                            return [(ks[:, kt * 128:(kt + 1) * 128], Qb[h][:, qt * 128:(qt + 1) * 128], [ks.b, Qb[h].b])]

                        def mask_fn(kt, st=st):
                            Mk = st["Mk"]
                            return [(Mk[:, kt * 128:(kt + 1) * 128], ident[:], [Mk.b, ident.b])]

                        def v_fn(kt):
                            return vsa[:, kt, :], [vsa.b]

                        yield from head_jobs(ptp, score_fn, mask_fn, v_fn, acc, 129, range(qt + 1), 128 ** -0.5, post)


                def win_jobs(qt, pre):
                    Lq, cm, of4, imp, st, coef = QS[qt]
                    for h in range(4):
                        acc = accp.next()

                        def post(h=h, acc=acc, of4=of4, qt=qt, coef=coef):
                            cf = coef(acc, 128, 3 * h + 2)
                            P.stt(of4[:, h, :], acc[:, 0:128], cf[:, 1:2], of4[:, h, :], ALU.mult, ALU.add,
                                  [acc.b, cf.b, of4.b], [of4.b])
                            ob = obp.next()
                            P.copy("pool", ob[:], of4[:, h, :], [of4.b], [ob.b])
                            P.dma(Sx["O_B"][qt * 128:(qt + 1) * 128, h * 128:(h + 1) * 128], ob[:], R=[ob.b], sb=ob.b)

                        def score_fn(kt, h=h, qt=qt):
                            return [(kw[:, kt * 128:(kt + 1) * 128], Qb[h][:, qt * 128:(qt + 1) * 128], [kw.b, Qb[h].b])]

                        def mask_fn(kt, qt=qt):
                            if kt == qt:
                                return [(causal[:], ident[:], [causal.b, ident.b])]
                            if kt == qt - 4:
                                return [(anti[:], ident[:], [anti.b, ident.b])]
                            return []

                        def v_fn(kt):
                            return vwa[:, kt, :], [vwa.b]

                        yield from head_jobs(ptp, score_fn, mask_fn, v_fn, acc, 129, range(max(0, qt - 4), qt + 1),
                                             128 ** -0.5, post, pre if h == 0 else None)

                setup(0)
                yield from cmp_jobs(0)
                sel0 = make_select(0)
                first = True
                for qt in range(NT):
                    if qt + 1 < NT:
                        setup(qt + 1)
                        gen = cmp_jobs(qt + 1)
                        if first:
                            j0 = next(gen)
                            j0.pre = sel0
                            yield j0
                            first = False
                        yield from gen
                    yield from slc_jobs(qt)
                    yield from win_jobs(qt, make_select(qt + 1) if qt + 1 < NT else None)
            run_jobs(jobsB())
            P.barrier()
        if stop_after == "BB":
            break
        with scope() as esc:
            wstage = sbpool(nc, esc, "wstC", 2, [128, 1024], F32)
            Wg = load_w_bf16(esc, "Wg", I["w_in"][l][:, OFF["gate"]:OFF["gate"] + 4096], 8, 4096, wstage)
            Wb = load_w_bf16(esc, "Wb", I["w_branch"][l].rearrange("n w d -> (n w) d"), 16, D, wstage)
            Wo = load_w_bf16(esc, "Wo", I["w_out"][l], 8, D, wstage)
            g1 = load_bcast(esc, "g1c", I["norm1_g"][l:l + 1, :], D)
            xs = sbpool(nc, esc, "xsC", 2, [128, D], F32)
            xop = sbpool(nc, esc, "xoC", 2, [128, D], F32)
            hbp = sbpool(nc, esc, "hbC", 2, [128, D], BF16)
            hTp = sbpool(nc, esc, "hTC", 2, [128, 8, 128], BF16)
            ob4p = sbpool(nc, esc, "ob4", 1, [128, 4, 512], BF16)
            oTp = sbpool(nc, esc, "oT", 1, [128, 16, 128], BF16)
            mrg = sbpool(nc, esc, "mrg", 1, [128, D], F32)
            mbp = sbpool(nc, esc, "mb", 1, [128, D], BF16)
            mTp = sbpool(nc, esc, "mT", 2, [128, 8, 128], BF16)
            sgp = sbpool(nc, esc, "sg", 2, [128, 512], F32)
            tmpp = sbpool(nc, esc, "tmpC", 2, [128, 512], F32)
            for tt in range(NT):
                rows = slice(tt * 128, (tt + 1) * 128)
                xt = xs.next()
                P.dma(xt[:], xsrc[rows, :], W=[xt.b], sb=xt.b)
                ob4 = ob4p.next()
                for n_, nm in enumerate(("O_A", "O_B", "O_C", "O_D")):
                    P.dma(ob4[:, n_, :], Sx[nm][rows, :], W=[ob4.b], sb=ob4.b)
                ss = rms(xt[:], D, [xt.b])
                hb = hbp.next()
                P.stt(hb[:], xt[:], ss[:, 1:2], g1[:], ALU.mult, ALU.mult, [xt.b, ss.b, g1.b], [hb.b])
                pb = psb.next()
                for k in range(8):
                    P.tr(pb[:, k * 128:(k + 1) * 128], hb[:, k * 128:(k + 1) * 128], [hb.b, ident.b], [pb.b])
                hTt = hTp.next()
                P.copy("dve", hTt[:], pb[:].rearrange("p (k c) -> p k c", k=8), [pb.b], [hTt.b])
                oT = oTp.next()
                for g in range(2):
                    pb = psb.next()
                    for k in range(8):
                        kk = g * 8 + k
                        P.tr(pb[:, k * 128:(k + 1) * 128], ob4[:, kk // 4, (kk % 4) * 128:(kk % 4 + 1) * 128],
                             [ob4.b, ident.b], [pb.b])
                    P.copy("dve", oT[:, g * 8:(g + 1) * 8, :], pb[:].rearrange("p (k c) -> p k c", k=8), [pb.b], [oT.b])
                mg = mrg.next()
                for n_ in range(4):
                    for j in range(2):
                        cs = slice(j * 512, (j + 1) * 512)
                        pg = psf.next()
                        for k in range(8):
                            P.mm(pg[:, :], hTt[:, k, :], Wg[:, k, n_ * 1024 + j * 512:n_ * 1024 + (j + 1) * 512],
                                 k == 0, k == 7, [hTt.b, Wg.b], [pg.b])
                        sg = sgp.next()
                        P.act(sg[:], pg[:, :], AF.Sigmoid, [pg.b], [sg.b])
                        pbr = psf.next()
                        for k in range(4):
                            P.mm(pbr[:, :], oT[:, n_ * 4 + k, :], Wb[:, n_ * 4 + k, cs], k == 0, k == 3, [oT.b, Wb.b], [pbr.b])
                        if n_ == 0:
                            P.tt("dve", mg[:, cs], pbr[:, :], sg[:], ALU.mult, [pbr.b, sg.b], [mg.b])
                        else:
                            tm = tmpp.next()
                            P.tt("dve", tm[:], pbr[:, :], sg[:], ALU.mult, [pbr.b, sg.b], [tm.b])
                            P.tt("pool", mg[:, cs], mg[:, cs], tm[:], ALU.add, [mg.b, tm.b], [mg.b])
                mb = mbp.next()
                P.copy("pool", mb[:], mg[:], [mg.b], [mb.b])
                pb = psb.next()
                for k in range(8):
                    P.tr(pb[:, k * 128:(k + 1) * 128], mb[:, k * 128:(k + 1) * 128], [mb.b, ident.b], [pb.b])
                mT = mTp.next()
                P.copy("dve", mT[:], pb[:].rearrange("p (k c) -> p k c", k=8), [pb.b], [mT.b])
                xo = xop.next()
                for j in range(2):
                    cs = slice(j * 512, (j + 1) * 512)
                    po = psf.next()
                    for k in range(8):
                        P.mm(po[:, :], mT[:, k, :], Wo[:, k, cs], k == 0, k == 7, [mT.b, Wo.b], [po.b])
                    P.tt("dve", xo[:, cs], po[:, :], xt[:, cs], ALU.add, [po.b, xt.b], [xo.b])
                P.dma(Sx["xb"][rows, :], xo[:], R=[xo.b], sb=xo.b)
            P.barrier()
        if stop_after == "Ca":
            break

        with scope() as esd:
            wstage = sbpool(nc, esd, "wstD", 2, [128, 1024], F32)
            Wgu = load_w_bf16(esd, "Wgu", I["w_gate_up"][l], 8, 2 * DFF, wstage)
            Wd = load_w_bf16(esd, "Wd", I["w_down"][l], 22, D, wstage)
            g2 = load_bcast(esd, "g2", I["norm2_g"][l:l + 1, :], D)
            last = (li == n_layers - 1)
            if last and final_norm:
                gf = load_bcast(esd, "gf", I["final_norm_g"][0:1, :], D)
            xs = sbpool(nc, esd, "xsD", 2, [128, D], F32)
            xop = sbpool(nc, esd, "xoD", 2, [128, D], F32)
            hbp = sbpool(nc, esd, "hbD", 2, [128, D], BF16)
            hTp = sbpool(nc, esd, "hTD", 2, [128, 8, 128], BF16)
            actp = sbpool(nc, esd, "actb", 1, [128, DFF], BF16)
            aTp = sbpool(nc, esd, "aT", 1, [128, 22, 128], BF16)
            sgp = sbpool(nc, esd, "sgD", 3, [128, 256], F32)
            for tt in range(NT):
                rows = slice(tt * 128, (tt + 1) * 128)
                xt = xs.next()
                P.dma(xt[:], Sx["xb"][rows, :], W=[xt.b], sb=xt.b)
                ss = rms(xt[:], D, [xt.b])
                hb = hbp.next()
                P.stt(hb[:], xt[:], ss[:, 1:2], g2[:], ALU.mult, ALU.mult, [xt.b, ss.b, g2.b], [hb.b])
                pb = psb.next()
                for k in range(8):
                    P.tr(pb[:, k * 128:(k + 1) * 128], hb[:, k * 128:(k + 1) * 128], [hb.b, ident.b], [pb.b])
                hTt = hTp.next()
                P.copy("dve", hTt[:], pb[:].rearrange("p (k c) -> p k c", k=8), [pb.b], [hTt.b])
                ab = actp.next()
                for j in range(11):
                    pg = psf.next()
                    for part in range(2):
                        c0 = part * DFF + j * 256
                        for k in range(8):
                            P.mm(pg[:, part * 256:(part + 1) * 256], hTt[:, k, :], Wgu[:, k, c0:c0 + 256], k == 0, k == 7,
                                 [hTt.b, Wgu.b], [pg.b])
                    sg = sgp.next()
                    P.act(sg[:], pg[:, 0:256], AF.Silu, [pg.b], [sg.b])
                    P.tt("dve", ab[:, j * 256:(j + 1) * 256], pg[:, 256:512], sg[:], ALU.mult, [pg.b, sg.b], [ab.b])
                aT = aTp.next()
                for g0 in range(0, 22, 8):
                    ng = min(8, 22 - g0)
                    pb = psb.next()
                    for k in range(ng):
                        P.tr(pb[:, k * 128:(k + 1) * 128], ab[:, (g0 + k) * 128:(g0 + k + 1) * 128], [ab.b, ident.b], [pb.b])
                    P.copy("dve", aT[:, g0:g0 + ng, :], pb[:, :ng * 128].rearrange("p (k c) -> p k c", k=ng), [pb.b], [aT.b])
                xo = xop.next()
                for j in range(2):
                    cs = slice(j * 512, (j + 1) * 512)
                    pd = psf.next()
                    for k in range(22):
                        P.mm(pd[:, :], aT[:, k, :], Wd[:, k, cs], k == 0, k == 21, [aT.b, Wd.b], [pd.b])
                    P.tt("dve", xo[:, cs], pd[:, :], xt[:, cs], ALU.add, [pd.b, xt.b], [xo.b])
                if last:
                    if final_norm:
                        ss2 = rms(xo[:], D, [xo.b])
                        P.stt(xo[:], xo[:], ss2[:, 1:2], gf[:], ALU.mult, ALU.mult, [xo.b, ss2.b, gf.b], [xo.b])
                    P.dma(out[rows, :], xo[:], R=[xo.b], sb=xo.b)
                else:
                    P.dma(Sx["xa"][rows, :], xo[:], R=[xo.b], sb=xo.b)
            P.barrier()

    P.barrier()
    with nc.Block() as blk:
        @blk.tensor
        def _(e):
            P.replay(e, "pe")

        @blk.scalar
        def _(e):
            P.replay(e, "act")

        @blk.vector
        def _(e):
            P.replay(e, "dve")

        @blk.gpsimd
        def _(e):
            P.replay(e, "pool")

        @blk.sync
        def _(e):
            P.replay(e, "sp")
    es_top.close()
    C.nsem = P.nsem
    C.maxval = P.maxval
    C.counts = dict(P.seq)
    return nc, C


def host_consts():
    bf = ml_dtypes.bfloat16
    c = {}
    c["c_ident"] = np.eye(128, dtype=np.float32).astype(bf)
    t = np.arange(128)[:, None]
    s = np.arange(128)[None, :]
    c["c_causal"] = np.where(s > t, NEG, 0.0).astype(np.float32).astype(bf)
    c["c_anti"] = np.where(s <= t, NEG, 0.0).astype(np.float32).astype(bf)
    tt = np.arange(S)[:, None]
    n = np.arange(256)[None, :]
    c["c_cmask"] = np.where(16 * n + 31 > tt, NEG, 0.0).astype(np.float32).astype(bf)
    j = np.arange(64)[None, :]
    cur = tt // 64
    forced = (j == 0) | (j == cur) | (j == cur - 1)
    visible = (j * 64) <= tt
    c["c_selb"] = np.where(visible, np.where(forced, 1e9, 0.0), -1e30).astype(np.float32)
    c["c_negbig"] = np.where(s > t, -1e30, 0.0).astype(np.float32)
    c["c_posbig"] = np.where(s > t, 1e30, 0.0).astype(np.float32)
    nn = np.arange(256)[:, None]
    cs = nn * 16
    ce = cs + 31
    ss_ = np.arange(64)[None, :] * 64
    ov = ((cs < ss_ + 64) & (ce >= ss_)).astype(np.float32)
    ov[255, :] = 0.0
    c["c_overlap"] = ov.astype(bf)
    for nm, rot in (("da", 16), ("nsa", 32), ("mla", 64), ("dsa", 32), ("idx", 16)):
        inv = np.power(np.float32(500000.0), -np.arange(0, rot, 2, dtype=np.float32) / np.float32(rot)).astype(np.float32)
        ang = (np.arange(S, dtype=np.float32)[:, None] * inv[None, :]).astype(np.float32)
        c["cos_" + nm] = np.cos(ang).astype(np.float32)
        c["sin_" + nm] = np.sin(ang).astype(np.float32)
    return c


_CACHE = {}


def kernel(**inputs):
    x = np.ascontiguousarray(inputs["x"], dtype=np.float32)
    if "prog" not in _CACHE:
        _CACHE["prog"] = build(DEPTH)
    nc, C = _CACHE["prog"]
    consts = host_consts()
    base = {k: np.ascontiguousarray(v, dtype=np.float32) for k, v in inputs.items() if k != "x"}
    base["final_norm_g"] = base["final_norm_g"].reshape(1, D)
    base.update(consts)
    in_maps = []
    cmap = {0: 0, 1: 1, 4: 2, 5: 3}
    zx = np.zeros_like(x[0])
    for c in range(8):
        m = dict(base)
        m["x"] = x[cmap[c]] if c in cmap else zx
        in_maps.append(m)
    res = run_bass_kernel_spmd(nc, in_maps, core_ids=list(range(8)))
    inv = {b: c for c, b in cmap.items()}
    outs = [res.results[inv[b]]["out"] for b in range(4)]
    return np.stack(outs, axis=0).astype(np.float32)
```

```python
import math
from contextlib import ExitStack
import numpy as np
import ml_dtypes
import concourse.bass as bass
import concourse.mybir as mybir
from concourse.bass_utils import run_bass_kernel_spmd

F32 = mybir.dt.float32
BF16 = mybir.dt.bfloat16
AF = mybir.ActivationFunctionType
ALU = mybir.AluOpType
AX = mybir.AxisListType

S = 4096
D = 1024
NT = S // 128
DEPTH = 4
D_IN = 9748
DFF = 2816
NEG = -30000.0
EPOCH = 16000
DEPOCH = 1000
TINY = 1e-30

OFF = {}
_o = 0
for _nm, _n in (("a_q", 512), ("a_k", 512), ("a_v", 512), ("b_q", 512), ("b_kc", 128), ("b_vc", 128),
                ("b_ks", 128), ("b_vs", 128), ("b_kw", 128), ("b_vw", 128), ("b_g", 12), ("c_q", 384),
                ("c_kv", 256), ("c_kr", 64), ("d_q", 512), ("d_k", 512), ("d_v", 512), ("d_iq", 512),
                ("d_ik", 64), ("d_iw", 8), ("gate", 4096)):
    OFF[_nm] = _o
    _o += _n
assert _o == D_IN


_BUID = [0]


class Buf:
    __slots__ = ("name", "w", "rs", "dsem", "dbase", "dcnt", "uid")

    def __init__(self, name):
        self.name = name
        self.w = None
        self.rs = {}
        self.dsem = None
        self.dbase = 0
        self.dcnt = 0
        _BUID[0] += 1
        self.uid = _BUID[0]


class Tn:
    def __init__(self, t, name):
        self.t = t
        self.b = Buf(name)

    def __getitem__(self, k):
        return self.t[k]


class FreePool:
    def __init__(self, items):
        self.items = items
        self.free_list = list(items)

    def alloc(self):
        assert self.free_list, "FreePool exhausted"
        return self.free_list.pop(0)

    def free(self, x):
        self.free_list.append(x)


class Pool:
    def __init__(self, items):
        self.items = items
        self.i = 0

    def next(self):
        x = self.items[self.i % len(self.items)]
        self.i += 1
        return x


ENGS = ("pe", "act", "dve", "pool", "sp")


class Prog:
    def __init__(self, nc, es):
        self.nc = nc
        self.es = es
        self.streams = {e: [] for e in ENGS}
        self.seq = {e: 0 for e in ENGS}
        self.esem = {e: [] for e in ENGS}
        self.wd = {e: {} for e in ENGS}
        self.dirty = {}
        self.nsem = 0
        self.ident = None
        self.free_d = []
        self.maxval = 0
        self.scopes = []

    def _newsem(self, name):
        self.nsem += 1
        return self.es.enter_context(self.nc.semaphore(name))

    def _ev_sem(self, ev):
        if ev[0] == "e":
            _, eng, seq = ev
            ep = (seq - 1) // EPOCH
            while len(self.esem[eng]) <= ep:
                self.esem[eng].append(self._newsem(f"e_{eng}_{len(self.esem[eng])}"))
            return self.esem[eng][ep], (seq - 1) % EPOCH + 1
        _, buf, cnt = ev
        if buf.dsem is None:
            if self.free_d:
                buf.dsem, buf.dbase = self.free_d.pop(0)
            else:
                buf.dsem, buf.dbase = self._newsem(f"d_{self.nsem}"), 0
        v = buf.dbase + 16 * cnt
        self.maxval = max(self.maxval, v)
        assert v < 60000
        return buf.dsem, v

    def release(self, buf):
        if buf.dsem is not None:
            self.free_d.append((buf.dsem, buf.dbase + 16 * buf.dcnt))
            buf.dsem = None
            buf.dcnt = 0
            buf.dbase = 0

    def _wait(self, eng, ev, raw):
        if ev[0] == "e":
            if ev[1] == eng and (eng == "pe" or eng == "sp" or not raw):
                return
            key = ("e", ev[1])
            val = ev[2]
        else:
            key = ("d", ev[1].uid)
            val = ev[2]
        if self.wd[eng].get(key, 0) >= val:
            return
        self.wd[eng][key] = val
        sem, v = self._ev_sem(ev)
        self.streams[eng].append(("w", sem, v))

    def _deps(self, eng, reads, writes):
        for b in reads:
            if b.w is not None:
                self._wait(eng, b.w, True)
        for b in writes:
            if b.w is not None:
                self._wait(eng, b.w, False)
            for ev in b.rs.values():
                self._wait(eng, ev, False)

    def _mark(self, me, reads, writes):
        for b in reads:
            k = ("e", me[1]) if me[0] == "e" else ("d", me[1].uid)
            b.rs[k] = me
        for b in writes:
            b.w = me
            b.rs = {}

    def op(self, eng, fn, reads=(), writes=()):
        self._deps(eng, reads, writes)
        self.seq[eng] += 1
        me = ("e", eng, self.seq[eng])
        sem, _ = self._ev_sem(me)
        self.streams[eng].append(("o", fn, sem, 1))
        self._mark(me, reads, writes)

    def dma(self, out, in_, R=(), W=(), sb=None, q="sp", slow=False):
        assert sb is not None
        self._deps(q, R, W)
        if sb.dcnt > 0:
            self._wait(q, ("d", sb, sb.dcnt), False)
        sb.dcnt += 1
        me = ("d", sb, sb.dcnt)
        sem, _ = self._ev_sem(me)
        if slow:
            fn = lambda e: e.dma_start(out=out, in_=in_, allow_slow_non_contiguous=True)
        else:
            fn = lambda e: e.dma_start(out=out, in_=in_)
        self.streams[q].append(("o", fn, sem, 16))
        self.dirty[sb.uid] = sb
        self._mark(me, R, W)

    def barrier(self):
        for eng in ENGS:
            for x in ENGS:
                if x != eng and self.seq[x] > 0:
                    ev = ("e", x, self.seq[x])
                    key = ("e", x)
                    if self.wd[eng].get(key, 0) < ev[2]:
                        self.wd[eng][key] = ev[2]
                        sem, v = self._ev_sem(ev)
                        self.streams[eng].append(("w", sem, v))
            for b in self.dirty.values():
                self._wait(eng, ("d", b, b.dcnt), False)
        self.dirty = {}

    def replay(self, e, eng):
        for it in self.streams[eng]:
            if it[0] == "w":
                e.wait_ge(it[1], it[2])
            else:
                it[1](e).then_inc(it[2], it[3])

    def mm(self, out, lhsT, rhs, start, stop, R, W):
        self.op("pe", lambda e: e.matmul(out, lhsT=lhsT, rhs=rhs, start=start, stop=stop), R, W)

    def tr(self, out, in_, R, W):
        k = in_.shape[0]
        idn = self.ident[:k, :k]
        self.op("pe", lambda e: e.transpose(out=out, in_=in_, identity=idn), R, W)

    def act(self, out, in_, func, R, W, scale=1.0, bias=None, accum=None):
        kw = {}
        if bias is not None:
            kw["bias"] = bias
        if accum is not None:
            kw["accum_out"] = accum
        self.op("act", lambda e: e.activation(out=out, in_=in_, func=func, scale=scale, **kw), R, W)

    def tt(self, eng, out, in0, in1, op, R, W):
        self.op(eng, lambda e: e.tensor_tensor(out=out, in0=in0, in1=in1, op=op), R, W)

    def ts(self, eng, out, in0, s1, s2, op0, op1, R, W, accum=None):
        if op1 is None:
            self.op(eng, lambda e: e.tensor_scalar(out=out, in0=in0, scalar1=s1, scalar2=None, op0=op0), R, W)
        elif accum is None:
            self.op(eng, lambda e: e.tensor_scalar(out=out, in0=in0, scalar1=s1, scalar2=s2, op0=op0, op1=op1), R, W)
        else:
            self.op(eng, lambda e: e.tensor_scalar(out=out, in0=in0, scalar1=s1, scalar2=s2, op0=op0, op1=op1,
                                                   accum_out=accum), R, W)

    def stt(self, out, in0, scalar, in1, op0, op1, R, W):
        self.op("dve", lambda e: e.scalar_tensor_tensor(out=out, in0=in0, scalar=scalar, in1=in1, op0=op0, op1=op1),
                R, W)

    def copy(self, eng, out, in_, R, W):
        if eng == "act":
            self.op("act", lambda e: e.activation(out=out, in_=in_, func=AF.Copy), R, W)
        else:
            self.op(eng, lambda e: e.tensor_copy(out=out, in_=in_), R, W)

    def memset(self, eng, ap, val, W):
        self.op(eng, lambda e: e.memset(ap, val), (), W)

    def red(self, out, in_, op, R, W):
        self.op("dve", lambda e: e.tensor_reduce(out=out, in_=in_, axis=AX.X, op=op), R, W)


class Ctx:
    pass


_UNIQ = [0]


_CURP = [None]


def sb(nc, es, name, shape, dt):
    _UNIQ[0] += 1
    nm = f"s{_UNIQ[0]}_{name}"
    t = Tn(es.enter_context(nc.sbuf_tensor(nm, list(shape), dt)), nm)
    P = _CURP[0]
    if P is not None and P.scopes:
        P.scopes[-1].append(t.b)
    return t


class scope:
    def __enter__(self):
        self.P = _CURP[0]
        self.P.scopes.append([])
        self.es = ExitStack()
        return self.es.__enter__()

    def __exit__(self, *a):
        r = self.es.__exit__(*a)
        for b in self.P.scopes.pop():
            self.P.release(b)
        return r


def sbpool(nc, es, name, n, shape, dt):
    return Pool([sb(nc, es, f"{name}{i}", shape, dt) for i in range(n)])


def bcast_rows(ap1d_row, n=128):
    return ap1d_row.partition_broadcast(n)


def build(n_layers, first_layer=0, final_norm=True, debug=False, stop_after=None, branches="ABCD"):
    nc = bass.Bass("TRN2", target_bir_lowering=False)
    es_top = ExitStack()
    C = Ctx()
    C.nc = nc
    L = DEPTH

    def din(name, shape, dt=F32):
        return nc.dram_tensor(name, list(shape), dt, kind="ExternalInput").ap()

    okind = "ExternalOutput" if debug else "Internal"

    def dscr(name, shape, dt=BF16):
        return nc.dram_tensor(name, list(shape), dt, kind=okind).ap()

    I = {}
    I["x"] = din("x", [S, D])
    for nm, shp in (("norm1_g", [L, D]), ("w_in", [L, D, D_IN]), ("diff_lq1", [L, 64]), ("diff_lk1", [L, 64]),
                    ("diff_lq2", [L, 64]), ("diff_lk2", [L, 64]), ("diff_subln_g", [L, 128]),
                    ("nsa_pe_k", [L, 32, 128]), ("nsa_w1_k", [L, 4096, 128]), ("nsa_w2_k", [L, 128, 128]),
                    ("nsa_pe_v", [L, 32, 128]), ("nsa_w1_v", [L, 4096, 128]), ("nsa_w2_v", [L, 128, 128]),
                    ("mla_q_norm_g", [L, 384]), ("mla_w_uq", [L, 384, 768]), ("mla_kv_norm_g", [L, 256]),
                    ("mla_w_ukv", [L, 256, 1024]), ("idx_k_norm_g", [L, 64]), ("w_branch", [L, 4, 512, D]),
                    ("w_out", [L, D, D]), ("norm2_g", [L, D]), ("w_gate_up", [L, D, 2 * DFF]),
                    ("w_down", [L, DFF, D]), ("final_norm_g", [1, D])):
        I[nm] = din(nm, shp)
    I["c_ident"] = din("c_ident", [128, 128], BF16)
    I["c_causal"] = din("c_causal", [128, 128], BF16)
    I["c_anti"] = din("c_anti", [128, 128], BF16)
    I["c_cmask"] = din("c_cmask", [S, 256], BF16)
    I["c_selb"] = din("c_selb", [S, 64], F32)
    I["c_negbig"] = din("c_negbig", [128, 128], F32)
    I["c_posbig"] = din("c_posbig", [128, 128], F32)
    I["c_overlap"] = din("c_overlap", [256, 64], BF16)
    for nm, half in (("da", 8), ("nsa", 16), ("mla", 32), ("dsa", 16), ("idx", 8)):
        I["cos_" + nm] = din("cos_" + nm, [S, half])
        I["sin_" + nm] = din("sin_" + nm, [S, half])
    out = nc.dram_tensor("out", [S, D], F32, kind="ExternalOutput").ap()

    Sx = {}
    Sx["xa"] = dscr("xa", [S, D], F32)
    Sx["xb"] = dscr("xb", [S, D], F32)
    for nm, rows in (("QT_A", 512), ("KT_A", 512), ("QT_B", 512), ("bkT", 384), ("vcT", 128), ("QT_Cn", 512),
                     ("QT_Cr", 256), ("KT_Cn", 512), ("kpeT", 64), ("QT_D", 512), ("KT_D", 512), ("iqT", 512),
                     ("ikT", 64)):
        Sx[nm] = dscr(nm, [rows, S])
    for nm, cols in (("V_A", 512), ("vs", 128), ("vw", 128), ("V_C", 512), ("V_D", 512), ("O_A", 512),
                     ("O_B", 512), ("O_C", 512), ("O_D", 512)):
        Sx[nm] = dscr(nm, [S, cols])
    Sx["gB"] = dscr("gB", [S, 12], F32)
    Sx["iw"] = dscr("iw", [S, 8], F32)
    Sx["kcmpT"] = dscr("kcmpT", [128, 256])
    Sx["vcmp"] = dscr("vcmp", [256, 128])

    es = es_top
    P = Prog(nc, es)
    C.P = P
    _CURP[0] = P
    ident = sb(nc, es, "ident", [128, 128], BF16)
    P.ident = ident
    causal = sb(nc, es, "causal", [128, 128], BF16)
    anti = sb(nc, es, "anti", [128, 128], BF16)
    P.dma(ident[:], I["c_ident"][:, :], W=[ident.b], sb=ident.b)
    P.dma(causal[:], I["c_causal"][:, :], W=[causal.b], sb=causal.b)
    P.dma(anti[:], I["c_anti"][:, :], W=[anti.b], sb=anti.b)
    psf = Pool([Tn(es.enter_context(nc.psum_tensor(f"psf{i}", [128, 512], F32)), f"psf{i}") for i in range(6)])
    psb = Pool([Tn(es.enter_context(nc.psum_tensor(f"psb{i}", [128, 1024], BF16)), f"psb{i}") for i in range(2)])
    C.psf, C.psb = psf, psb
    C.small = sbpool(nc, es, "small", 8, [128, 4], F32)
    C.junk = sbpool(nc, es, "junk", 2, [128, 1024], F32)

    def rms(src_ap, n, R, eps=1e-6):
        ss = C.small.next()
        jk = C.junk.next()
        P.memset("dve", ss[:], 0.0, [ss.b])
        P.act(jk[:, :n], src_ap, AF.Square, R + [ss.b], [jk.b, ss.b], accum=ss[:, 0:1])
        P.ts("dve", ss[:, 2:3], ss[:, 0:1], 1.0 / n, eps, ALU.mult, ALU.add, [ss.b], [ss.b])
        P.act(ss[:, 3:4], ss[:, 2:3], AF.Ln, [ss.b], [ss.b])
        P.act(ss[:, 1:2], ss[:, 3:4], AF.Exp, [ss.b], [ss.b], scale=-0.5)
        return ss

    def load_bcast(es_, name, row_ap, n, q="sp"):
        t = sb(nc, es_, name, [128, n], F32)
        P.dma(t[:], row_ap.partition_broadcast(128), W=[t.b], sb=t.b, q=q)
        return t

    def load_w_bf16(es_, name, dram_ap, kc, ncols, stage_pool, chunk_cols=512):
        wt = sb(nc, es_, name, [128, kc, ncols], BF16)
        src = dram_ap.rearrange("(k p) n -> p k n", p=128)
        for k in range(kc):
            sw = stage_pool.items[0].t.shape[-1]
            for c0 in range(0, ncols, sw):
                w = min(sw, ncols - c0)
                st = stage_pool.next()
                P.dma(st[:, :w], src[:, k, c0:c0 + w], W=[st.b], sb=st.b)
                P.copy("pool", wt[:, k, c0:c0 + w], st[:, :w], [st.b], [wt.b])
        return wt

    for li in range(n_layers):
        l = first_layer + li
        xsrc = I["x"] if li == 0 else Sx["xa"]
        lam_init = 0.8 - 0.6 * math.exp(-0.3 * l)

        with scope() as esA:
            hT = sb(nc, esA, "hT", [128, 8, S], BF16)
            with scope() as es0:
                g1 = load_bcast(es0, "g1", I["norm1_g"][l:l + 1, :], D)
                xs = sbpool(nc, es0, "xs", 2, [128, D], F32)
                hbp = sbpool(nc, es0, "hb", 2, [128, D], BF16)
                for tt in range(NT):
                    xt = xs.next()
                    P.dma(xt[:], xsrc[tt * 128:(tt + 1) * 128, :], W=[xt.b], sb=xt.b)
                    ss = rms(xt[:], D, [xt.b])
                    hb = hbp.next()
                    P.stt(hb[:], xt[:], ss[:, 1:2], g1[:], ALU.mult, ALU.mult, [xt.b, ss.b, g1.b], [hb.b])
                    pb = psb.next()
                    for k in range(8):
                        P.tr(pb[:, k * 128:(k + 1) * 128], hb[:, k * 128:(k + 1) * 128], [hb.b, ident.b], [pb.b])
                    P.copy("dve", hT[:, :, tt * 128:(tt + 1) * 128], pb[:].rearrange("p (k c) -> p k c", k=8),
                           [pb.b], [hT.b])
                P.barrier()
            if stop_after == "A0":
                break
            with scope() as es1:
                wst = sbpool(nc, es1, "wst", 1, [128, 8, 512], F32)
                wbf = sbpool(nc, es1, "wbf", 2, [128, 8, 512], BF16)
                zp = sbpool(nc, es1, "z", 4, [128, 1024], F32)
                zbp = FreePool(sbpool(nc, es1, "zb", 12, [128, 1024], BF16).items)
                ztp = sbpool(nc, es1, "zt", 4, [128, 4, 128], BF16)
                rtmp = sbpool(nc, es1, "rtmp", 3, [128, 4, 128], F32)
                tabs = {}
                for nm, half in (("da", 8), ("nsa", 16), ("mla", 32), ("dsa", 16), ("idx", 8)):
                    ct = sb(nc, es1, "cos_" + nm, [128, NT, half], F32)
                    st = sb(nc, es1, "sin_" + nm, [128, NT, half], F32)
                    P.dma(ct[:], I["cos_" + nm].rearrange("(t p) h -> p t h", p=128), W=[ct.b], sb=ct.b)
                    P.dma(st[:], I["sin_" + nm].rearrange("(t p) h -> p t h", p=128), W=[st.b], sb=st.b)
                    tabs[nm] = (ct, st, half)
                gq = load_bcast(es1, "gq", I["mla_q_norm_g"][l:l + 1, :], 384)
                gkv = load_bcast(es1, "gkv", I["mla_kv_norm_g"][l:l + 1, :], 256)
                gik = load_bcast(es1, "gik", I["idx_k_norm_g"][l:l + 1, :], 64)
                wstage = sbpool(nc, es1, "wstage", 2, [128, 1024], F32)
                wuq = load_w_bf16(es1, "wuq", I["mla_w_uq"][l], 3, 768, wstage)
                wukv = load_w_bf16(es1, "wukv", I["mla_w_ukv"][l], 2, 1024, wstage)
                smallio = sbpool(nc, es1, "smallio", 4, [128, 16], F32)

                def rope(z, col0, G, dh, roff, kind, tt):
                    ct, st, half = tabs[kind]
                    v = z[:, col0:col0 + G * dh].rearrange("p (g d) -> p g d", g=G)
                    x1 = v[:, :, roff:roff + half]
                    x2 = v[:, :, roff + half:roff + 2 * half]
                    cc = ct[:, tt:tt + 1, :].to_broadcast([128, G, half])
                    sn = st[:, tt:tt + 1, :].to_broadcast([128, G, half])
                    tm = rtmp.next()
                    t = [tm[:, i, :G * half].rearrange("p (g h) -> p g h", g=G) for i in range(4)]
                    Rr = [z.b, ct.b, st.b]
                    P.tt("dve", t[0], x1, cc, ALU.mult, Rr, [tm.b])
                    P.tt("dve", t[1], x2, sn, ALU.mult, Rr, [tm.b])
                    P.tt("pool", t[2], x2, cc, ALU.mult, Rr, [tm.b])
                    P.tt("pool", t[3], x1, sn, ALU.mult, Rr, [tm.b])
                    P.tt("dve", x1, t[0], t[1], ALU.subtract, [tm.b], [z.b])
                    P.tt("dve", x2, t[2], t[3], ALU.add, [tm.b], [z.b])

                def store_T(zb, col0, ncols, dst, row0, tt):
                    pb = psb.next()
                    nj = (ncols + 127) // 128
                    for j in range(nj):
                        w = min(128, ncols - j * 128)
                        P.tr(pb[:w, j * 128:(j + 1) * 128], zb[:, col0 + j * 128:col0 + j * 128 + w],
                             [zb.b, ident.b], [pb.b])
                    zt = ztp.next()
                    if ncols >= 128:
                        P.copy("dve", zt[:, :nj, :], pb[:, :nj * 128].rearrange("p (j c) -> p j c", j=nj),
                               [pb.b], [zt.b])
                        P.dma(dst[row0:row0 + ncols, tt * 128:(tt + 1) * 128].rearrange("(j p) c -> p j c", p=128),
                              zt[:, :nj, :], R=[zt.b], sb=zt.b)
                    else:
                        P.copy("dve", zt[:ncols, 0, :], pb[:ncols, 0:128], [pb.b], [zt.b])
                        P.dma(dst[row0:row0 + ncols, tt * 128:(tt + 1) * 128], zt[:ncols, 0, :], R=[zt.b], sb=zt.b)

                def store_tok(zb, col0, ncols, dst, tt):
                    P.dma(dst[tt * 128:(tt + 1) * 128, :], zb[:, col0:col0 + ncols], R=[zb.b], sb=zb.b)

                def tobf(z, zb, c0, n):
                    P.copy("dve", zb[:, c0:c0 + n], z[:, c0:c0 + n], [z.b], [zb.b])

                def h_rope_T(G, dh, kind, dst):
                    def h(tt, z, n):
                        rope(z, 0, G, dh, 0, kind, tt)
                        zb = zbp.alloc()
                        tobf(z, zb, 0, n)
                        yield
                        store_T(zb, 0, n, Sx[dst], 0, tt)
                        zbp.free(zb)
                    return h

                def h_tok(dst):
                    def h(tt, z, n):
                        zb = zbp.alloc()
                        tobf(z, zb, 0, n)
                        store_tok(zb, 0, n, Sx[dst], tt)
                        zbp.free(zb)
                        return
                        yield
                    return h

                def h_bk(tt, z, n):
                    rope(z, 0, 3, 128, 0, "nsa", tt)
                    zb = zbp.alloc()
                    tobf(z, zb, 0, 384)
                    yield
                    store_T(zb, 0, 384, Sx["bkT"], 0, tt)
                    zbp.free(zb)

                def h_bv(tt, z, n):
                    zb = zbp.alloc()
                    tobf(z, zb, 0, 384)
                    yield
                    store_T(zb, 0, 128, Sx["vcT"], 0, tt)
                    P.dma(Sx["vs"][tt * 128:(tt + 1) * 128, :], zb[:, 128:256], R=[zb.b], sb=zb.b)
                    P.dma(Sx["vw"][tt * 128:(tt + 1) * 128, :], zb[:, 256:384], R=[zb.b], sb=zb.b)
                    zbp.free(zb)

                def norm_proj(z, c0, n, gt, wt, ncout, tt, res):
                    ss = rms(z[:, c0:c0 + n], n, [z.b])
                    zb = zbp.alloc()
                    P.stt(zb[:, :n], z[:, c0:c0 + n], ss[:, 1:2], gt[:], ALU.mult, ALU.mult, [z.b, ss.b, gt.b], [zb.b])
                    kc = n // 128
                    yield
                    pb = psb.next()
                    for k in range(kc):
                        P.tr(pb[:, k * 128:(k + 1) * 128], zb[:, k * 128:(k + 1) * 128], [zb.b, ident.b], [pb.b])
                    zbp.free(zb)
                    zt = ztp.next()
                    P.copy("dve", zt[:, :kc, :], pb[:, :kc * 128].rearrange("p (j c) -> p j c", j=kc), [pb.b], [zt.b])
                    z2 = zp.next()
                    for c in range(0, ncout, 512):
                        w = min(512, ncout - c)
                        ps = psf.next()
                        for k in range(kc):
                            P.mm(ps[:, :w], zt[:, k, :], wt[:, k, c:c + w], k == 0, k == kc - 1, [zt.b, wt.b], [ps.b])
                        P.copy("act", z2[:, c:c + w], ps[:, :w], [ps.b], [z2.b])
                    res.append(z2)

                def h_cq(tt, z, n):
                    sg = smallio.next()
                    P.act(sg[:, :12], z[:, 384:396], AF.Sigmoid, [z.b], [sg.b])
                    P.dma(Sx["gB"][tt * 128:(tt + 1) * 128, :], sg[:, :12], R=[sg.b], sb=sg.b)
                    res = []
                    yield from norm_proj(z, 0, 384, gq, wuq, 768, tt, res)
                    q = res[0]
                    rope(q, 0, 4, 192, 128, "mla", tt)
                    qb = zbp.alloc()
                    q3 = q[:, :768].rearrange("p (g d) -> p g d", g=4)
                    P.copy("pool", qb[:, 0:512].rearrange("p (g d) -> p g d", g=4), q3[:, :, 0:128], [q.b], [qb.b])
                    P.copy("pool", qb[:, 512:768].rearrange("p (g d) -> p g d", g=4), q3[:, :, 128:192], [q.b], [qb.b])
                    yield
                    store_T(qb, 0, 512, Sx["QT_Cn"], 0, tt)
                    store_T(qb, 512, 256, Sx["QT_Cr"], 0, tt)
                    zbp.free(qb)

                def h_ckv(tt, z, n):
                    rope(z, 256, 1, 64, 0, "mla", tt)
                    zb = zbp.alloc()
                    tobf(z, zb, 256, 64)
                    res = []
                    g_ = norm_proj(z, 0, 256, gkv, wukv, 1024, tt, res)
                    next(g_)
                    yield
                    store_T(zb, 256, 64, Sx["kpeT"], 0, tt)
                    zbp.free(zb)
                    for _ in g_:
                        pass
                    kv = res[0]
                    kb = zbp.alloc()
                    kv3 = kv[:, :1024].rearrange("p (g d) -> p g d", g=4)
                    P.copy("pool", kb[:, 0:512].rearrange("p (g d) -> p g d", g=4), kv3[:, :, 0:128], [kv.b], [kb.b])
                    P.copy("pool", kb[:, 512:1024].rearrange("p (g d) -> p g d", g=4), kv3[:, :, 128:256], [kv.b],
                           [kb.b])
                    yield
                    store_T(kb, 0, 512, Sx["KT_Cn"], 0, tt)
                    store_tok(kb, 512, 512, Sx["V_C"], tt)
                    zbp.free(kb)

                def h_ik(tt, z, n):
                    sg = smallio.next()
                    P.ts("dve", sg[:, :8], z[:, 64:72], (8 ** -0.5) * (64 ** -0.5), None, ALU.mult, None, [z.b], [sg.b])
                    P.dma(Sx["iw"][tt * 128:(tt + 1) * 128, :], sg[:, :8], R=[sg.b], sb=sg.b)
                    ss = rms(z[:, 0:64], 64, [z.b])
                    P.stt(z[:, 0:64], z[:, 0:64], ss[:, 1:2], gik[:], ALU.mult, ALU.mult, [z.b, ss.b, gik.b], [z.b])
                    rope(z, 0, 1, 64, 0, "idx", tt)
                    zb = zbp.alloc()
                    tobf(z, zb, 0, 64)
                    yield
                    store_T(zb, 0, 64, Sx["ikT"], 0, tt)
                    zbp.free(zb)

                chunks = [
                    ([("a_q", 512)], h_rope_T(8, 64, "da", "QT_A")),
                    ([("a_k", 512)], h_rope_T(8, 64, "da", "KT_A")),
                    ([("a_v", 512)], h_tok("V_A")),
                    ([("b_q", 512)], h_rope_T(4, 128, "nsa", "QT_B")),
                    ([("b_kc", 128), ("b_ks", 128), ("b_kw", 128)], h_bk),
                    ([("b_vc", 128), ("b_vs", 128), ("b_vw", 128)], h_bv),
                    ([("c_q", 384), ("b_g", 12)], h_cq),
                    ([("c_kv", 256), ("c_kr", 64)], h_ckv),
                    ([("d_q", 512)], h_rope_T(4, 128, "dsa", "QT_D")),
                    ([("d_k", 512)], h_rope_T(4, 128, "dsa", "KT_D")),
                    ([("d_v", 512)], h_tok("V_D")),
                    ([("d_iq", 512)], h_rope_T(8, 64, "idx", "iqT")),
                    ([("d_ik", 64), ("d_iw", 8)], h_ik),
                ]
                if stop_after == "A1a":
                    chunks = chunks[:3]
                wsrc = I["w_in"][l].rearrange("(k p) n -> p k n", p=128)

                def load_chunk(ci):
                    segs, _ = chunks[ci]
                    st = wst.next()
                    wb = wbf.next()
                    c = 0
                    for nm, n in segs:
                        P.dma(st[:, :, c:c + n], wsrc[:, :, OFF[nm]:OFF[nm] + n], W=[st.b], sb=st.b)
                        c += n
                    P.copy("pool", wb[:, :, :c], st[:, :, :c], [st.b], [wb.b])
                    return wb, c

                nxt = load_chunk(0)
                active = []

                def advance(flush=False):
                    while True:
                        keep = []
                        for it in active:
                            if it[1] > 0 and not flush:
                                it[1] -= 1
                                keep.append(it)
                                continue
                            try:
                                next(it[0])
                                it[1] = 1
                                keep.append(it)
                            except StopIteration:
                                pass
                        active[:] = keep
                        if not flush or not active:
                            break

                for ci in range(len(chunks)):
                    wb, n = nxt
                    if ci + 1 < len(chunks):
                        nxt = load_chunk(ci + 1)
                    handler = chunks[ci][1]
                    for tt in range(NT):
                        ps = psf.next()
                        for k in range(8):
                            P.mm(ps[:, :n], hT[:, k, tt * 128:(tt + 1) * 128], wb[:, k, :n], k == 0, k == 7,
                                 [hT.b, wb.b], [ps.b])
                        z = zp.next()
                        P.copy("act", z[:, :n], ps[:, :n], [ps.b], [z.b])
                        g_ = handler(tt, z, n)
                        try:
                            next(g_)
                            active.append([g_, 1])
                        except StopIteration:
                            pass
                        advance()
                advance(flush=True)
                P.barrier()
        if stop_after in ("A0", "A1a", "A1"):
            break
        with scope() as es2:
            tokp = sbpool(nc, es2, "ctok", 2, [128, S], BF16)
            w1st = sb(nc, es2, "w1st", [128, 32, 128], F32)
            w1bp = sbpool(nc, es2, "w1b", 2, [128, 32, 128], BF16)
            peTp = sbpool(nc, es2, "peT", 2, [128, 32], F32)
            w2stp = sbpool(nc, es2, "w2st", 2, [128, 128], F32)
            w2bp = sbpool(nc, es2, "w2b", 2, [128, 128], BF16)
            ctmp = sbpool(nc, es2, "ctmp", 3, [128, 256], BF16)
            gxp = sbpool(nc, es2, "gx", 2, [128, 256], F32)
            gx2p = sbpool(nc, es2, "gx2", 2, [128, 256], F32)
            gTp = sbpool(nc, es2, "gT", 2, [128, 256], BF16)
            cout = sbpool(nc, es2, "cout", 2, [128, 256], BF16)
            for kind in ("k", "v"):
                src = Sx["bkT"][0:128, :] if kind == "k" else Sx["vcT"][:, :]
                tk = tokp.next()
                P.dma(tk[:], src, W=[tk.b], sb=tk.b)
                P.dma(w1st[:], I["nsa_w1_" + kind][l].rearrange("(l d) o -> d l o", d=128), W=[w1st.b], sb=w1st.b)
                wb = w1bp.next()
                P.copy("pool", wb[:], w1st[:], [w1st.b], [wb.b])
                pt = peTp.next()
                P.dma(pt[:], I["nsa_pe_" + kind][l].rearrange("l d -> d l"), W=[pt.b], sb=pt.b, slow=True)
                w2s = w2stp.next()
                P.dma(w2s[:], I["nsa_w2_" + kind][l], W=[w2s.b], sb=w2s.b)
                w2 = w2bp.next()
                P.copy("pool", w2[:], w2s[:], [w2s.b], [w2.b])
                ps = psf.next()
                tk3 = tk[:].rearrange("p (b s) -> p b s", s=16)
                for lp in range(32):
                    tm = ctmp.next()
                    P.ts("dve", tm[:, :255], tk3[:, lp // 16:lp // 16 + 255, lp % 16], pt[:, lp:lp + 1], None,
                         ALU.add, None, [tk.b, pt.b], [tm.b])
                    P.mm(ps[:, :255], wb[:, lp, :], tm[:, :255], lp == 0, lp == 31, [wb.b, tm.b], [ps.b])
                gx = gxp.next()
                gx2 = gx2p.next()
                P.copy("act", gx[:, :255], ps[:, :255], [ps.b], [gx.b])
                P.tt("dve", gx2[:, :255], gx[:, :255], gx[:, :255], ALU.mult, [gx.b], [gx2.b])
                P.ts("dve", gx2[:, :255], gx2[:, :255], 0.044715, 1.0, ALU.mult, ALU.add, [gx2.b], [gx2.b])
                P.tt("dve", gx2[:, :255], gx2[:, :255], gx[:, :255], ALU.mult, [gx2.b, gx.b], [gx2.b])
                P.act(gx2[:, :255], gx2[:, :255], AF.Sigmoid, [gx2.b], [gx2.b], scale=2.0 * math.sqrt(2.0 / math.pi))
                gT = gTp.next()
                P.memset("dve", gT[:], 0.0, [gT.b])
                P.tt("dve", gT[:, :255], gx[:, :255], gx2[:, :255], ALU.mult, [gx.b, gx2.b], [gT.b])
                co = cout.next()
                if kind == "k":
                    ps2 = psf.next()
                    P.mm(ps2[:, :256], w2[:], gT[:, :256], True, True, [w2.b, gT.b], [ps2.b])
                    P.copy("dve", co[:], ps2[:, :256], [ps2.b], [co.b])
                    P.dma(Sx["kcmpT"][:, :], co[:], R=[co.b], sb=co.b)
                else:
                    for g in range(2):
                        ps2 = psf.next()
                        P.mm(ps2[:, :128], gT[:, g * 128:(g + 1) * 128], w2[:], True, True, [w2.b, gT.b], [ps2.b])
                        P.copy("dve", co[:, g * 128:(g + 1) * 128], ps2[:, :128], [ps2.b], [co.b])
                    P.dma(Sx["vcmp"].rearrange("(g p) d -> p g d", p=128), co[:].rearrange("p (g d) -> p g d", g=2),
                          R=[co.b], sb=co.b)
            P.barrier()
        if stop_after == "A2":
            break

        psc = Pool(psf.items[0:2])
        accp = Pool(psf.items[2:6])

        class Job:
            def __init__(self, scores, exp, pv, post=None, pre=None):
                self.scores, self.exp, self.pv, self.post, self.pre = scores, exp, pv, post, pre

        def run_jobs(jobs):
            prev = None
            for j in jobs:
                if j.pre is not None:
                    if prev is not None:
                        prev.pv()
                        if prev.post is not None:
                            prev.post()
                        prev = None
                    j.pre()
                j.scores()
                j.exp()
                if prev is not None:
                    prev.pv()
                    if prev.post is not None:
                        prev.post()
                prev = j
            if prev is not None:
                prev.pv()
                if prev.post is not None:
                    prev.post()

        def head_jobs(ptp, score_fn, mask_fn, v_fn, acc, nv, kts, scale, post=None, pre=None):
            kts = list(kts)
            chs = [kts[i:i + 4] for i in range(0, len(kts), 4)]
            for ci, ch in enumerate(chs):
                st = {}

                def scores(ch=ch, st=st):
                    sbk = psc.next()
                    st["sb"] = sbk
                    for j, kt in enumerate(ch):
                        terms = list(score_fn(kt))
                        mks = mask_fn(kt)
                        terms.extend(mks)
                        for i, (lt, rh, bufs) in enumerate(terms):
                            P.mm(sbk[:, j * 128:(j + 1) * 128], lt, rh, i == 0, i == len(terms) - 1, bufs, [sbk.b])

                def exp(ch=ch, st=st):
                    pt = ptp.next()
                    st["pt"] = pt
                    n = len(ch) * 128
                    P.act(pt[:, :n], st["sb"][:, :n], AF.Exp, [st["sb"].b], [pt.b], scale=scale)

                def pv(ch=ch, st=st, ci=ci):
                    for j, kt in enumerate(ch):
                        va, vb = v_fn(kt)
                        P.mm(acc[:, :nv], st["pt"][:, j * 128:(j + 1) * 128], va, ci == 0 and j == 0,
                             ci == len(chs) - 1 and j == len(ch) - 1, [st["pt"].b] + vb, [acc.b])

                yield Job(scores, exp, pv, post if ci == len(chs) - 1 else None, pre if ci == 0 else None)

        def recip_sum(acc, col, dst_ap, dst_buf):
            P.ts("dve", dst_ap, acc[:, col:col + 1], TINY, None, ALU.add, None, [acc.b], [dst_buf])
            P.op("dve", lambda e: e.reciprocal(out=dst_ap, in_=dst_ap), [dst_buf], [dst_buf])

        def load_vaug(V, src, c0, nvv=129):
            P.dma(V[:, :, 0:128], src[:, c0:c0 + 128].rearrange("(t p) c -> p t c", p=128), W=[V.b], sb=V.b)
            P.memset("pool", V[:, :, 128:129], 1.0, [V.b])

        if "A" in branches:
          with scope() as esb:
            ptp = sbpool(nc, esb, "pt", 3, [128, 512], BF16)
            Kp = sbpool(nc, esb, "K", 2, [128, S], BF16)
            Qp = sbpool(nc, esb, "Q", 2, [128, S], BF16)
            Vp = sbpool(nc, esb, "V", 2, [128, NT, 129], BF16)
            of = sbpool(nc, esb, "of", 3, [128, 128], F32)
            obp = sbpool(nc, esb, "ob", 3, [128, 128], BF16)
            lqs = [load_bcast(esb, nm, I[nm][l:l + 1, :], 64) for nm in ("diff_lq1", "diff_lk1", "diff_lq2", "diff_lk2")]
            lam = sb(nc, esb, "lam", [128, 8], F32)
            ltmp = sb(nc, esb, "ltmp", [128, 64], F32)
            for i in range(2):
                P.tt("dve", ltmp[:], lqs[2 * i][:], lqs[2 * i + 1][:], ALU.mult, [lqs[2 * i].b, lqs[2 * i + 1].b], [ltmp.b])
                P.red(lam[:, i:i + 1], ltmp[:], ALU.add, [ltmp.b], [lam.b])
            P.act(lam[:, 2:4], lam[:, 0:2], AF.Exp, [lam.b], [lam.b])
            P.tt("dve", lam[:, 4:5], lam[:, 3:4], lam[:, 2:3], ALU.subtract, [lam.b], [lam.b])
            P.ts("dve", lam[:, 5:6], lam[:, 4:5], -lam_init, None, ALU.add, None, [lam.b], [lam.b])
            gsub = load_bcast(esb, "gsub", I["diff_subln_g"][l:l + 1, :], 128)
            P.ts("dve", gsub[:], gsub[:], 1.0 - lam_init, None, ALU.mult, None, [gsub.b], [gsub.b])

            def jobsA():
                for h in range(4):
                    K, Q, V = Kp.next(), Qp.next(), Vp.next()
                    P.dma(K[:], Sx["KT_A"][h * 128:(h + 1) * 128, :], W=[K.b], sb=K.b)
                    P.dma(Q[:], Sx["QT_A"][h * 128:(h + 1) * 128, :], W=[Q.b], sb=Q.b)
                    load_vaug(V, Sx["V_A"], h * 128)
                    for qt in range(NT):
                        accs = [accp.next(), accp.next()]

                        def post(h=h, qt=qt, accs=accs):
                            r = C.small.next()
                            recip_sum(accs[0], 128, r[:, 0:1], r.b)
                            recip_sum(accs[1], 128, r[:, 1:2], r.b)
                            P.tt("dve", r[:, 1:2], r[:, 1:2], lam[:, 5:6], ALU.mult, [r.b, lam.b], [r.b])
                            o = of.next()
                            P.ts("dve", o[:], accs[0][:, 0:128], r[:, 0:1], None, ALU.mult, None, [accs[0].b, r.b], [o.b])
                            P.stt(o[:], accs[1][:, 0:128], r[:, 1:2], o[:], ALU.mult, ALU.add, [accs[1].b, r.b, o.b], [o.b])
                            ss = rms(o[:], 128, [o.b])
                            ob = obp.next()
                            P.stt(ob[:], o[:], ss[:, 1:2], gsub[:], ALU.mult, ALU.mult, [o.b, ss.b, gsub.b], [ob.b])
                            P.dma(Sx["O_A"][qt * 128:(qt + 1) * 128, h * 128:(h + 1) * 128], ob[:], R=[ob.b], sb=ob.b)

                        for m in range(2):
                            def score_fn(kt, m=m, K=K, Q=Q, qt=qt):
                                return [(K[m * 64:(m + 1) * 64, kt * 128:(kt + 1) * 128],
                                         Q[m * 64:(m + 1) * 64, qt * 128:(qt + 1) * 128], [K.b, Q.b])]

                            def mask_fn(kt, qt=qt):
                                return [(causal[:], ident[:], [causal.b, ident.b])] if kt == qt else []

                            def v_fn(kt, V=V):
                                return V[:, kt, :], [V.b]

                            yield from head_jobs(ptp, score_fn, mask_fn, v_fn, accs[m], 129, range(qt + 1),
                                                 64 ** -0.5, post if m == 1 else None)
            run_jobs(jobsA())
            P.barrier()
        if stop_after == "BA":
            break

        if "C" in branches:
          with scope() as esb:
            ptp = sbpool(nc, esb, "pt", 3, [128, 512], BF16)
            Kp = sbpool(nc, esb, "K", 2, [128, S], BF16)
            Qp = sbpool(nc, esb, "Q", 2, [128, S], BF16)
            Qrp = sbpool(nc, esb, "Qr", 2, [64, S], BF16)
            Vp = sbpool(nc, esb, "V", 2, [128, NT, 129], BF16)
            kpe = sb(nc, esb, "kpe", [64, S], BF16)
            obp = sbpool(nc, esb, "ob", 3, [128, 128], BF16)
            P.dma(kpe[:], Sx["kpeT"][:, :], W=[kpe.b], sb=kpe.b)

            def jobsC():
                for h in range(4):
                    K, Q, Qr, V = Kp.next(), Qp.next(), Qrp.next(), Vp.next()
                    P.dma(K[:], Sx["KT_Cn"][h * 128:(h + 1) * 128, :], W=[K.b], sb=K.b)
                    P.dma(Q[:], Sx["QT_Cn"][h * 128:(h + 1) * 128, :], W=[Q.b], sb=Q.b)
                    P.dma(Qr[:], Sx["QT_Cr"][h * 64:(h + 1) * 64, :], W=[Qr.b], sb=Qr.b)
                    load_vaug(V, Sx["V_C"], h * 128)
                    for qt in range(NT):
                        acc = accp.next()

                        def post(h=h, qt=qt, acc=acc):
                            r = C.small.next()
                            recip_sum(acc, 128, r[:, 0:1], r.b)
                            ob = obp.next()
                            P.ts("dve", ob[:], acc[:, 0:128], r[:, 0:1], None, ALU.mult, None, [acc.b, r.b], [ob.b])
                            P.dma(Sx["O_C"][qt * 128:(qt + 1) * 128, h * 128:(h + 1) * 128], ob[:], R=[ob.b], sb=ob.b)

                        def score_fn(kt, K=K, Q=Q, Qr=Qr, qt=qt):
                            return [(K[:, kt * 128:(kt + 1) * 128], Q[:, qt * 128:(qt + 1) * 128], [K.b, Q.b]),
                                    (kpe[:, kt * 128:(kt + 1) * 128], Qr[:, qt * 128:(qt + 1) * 128], [kpe.b, Qr.b])]

                        def mask_fn(kt, qt=qt):
                            return [(causal[:], ident[:], [causal.b, ident.b])] if kt == qt else []

                        def v_fn(kt, V=V):
                            return V[:, kt, :], [V.b]

                        yield from head_jobs(ptp, score_fn, mask_fn, v_fn, acc, 129, range(qt + 1), 192 ** -0.5, post)
            run_jobs(jobsC())
            P.barrier()
        if stop_after == "BC":
            break

        if "D" in branches:
          with scope() as esb:
            ptp = sbpool(nc, esb, "pt", 3, [128, 512], BF16)
            iqp = sbpool(nc, esb, "iqt", 4, [128, 4, 128], BF16)
            ik2 = sb(nc, esb, "ik2", [128, S], BF16)
            qdp = sbpool(nc, esb, "qd", 3, [128, 4, 128], BF16)
            Kd = [sb(nc, esb, f"Kd{i}", [128, S], BF16) for i in range(4)]
            Vd = [sb(nc, esb, f"Vd{i}", [128, NT, 129], BF16) for i in range(4)]
            iwt = sb(nc, esb, "iwt", [128, NT, 8], F32)
            idxp = sbpool(nc, esb, "idx", 4, [128, S], F32)
            Mkp = sbpool(nc, esb, "Mk", 4, [128, S], BF16)
            rp = sbpool(nc, esb, "relu", 2, [128, 512], F32)
            bjunk = sb(nc, esb, "bjunk", [128, S], BF16)
            negbig = sb(nc, esb, "negbig", [128, 128], F32)
            posbig = sb(nc, esb, "posbig", [128, 128], F32)
            dtmpp = sbpool(nc, esb, "dtmp", 2, [128, 128], F32)
            bs = sbpool(nc, esb, "bs", 6, [128, 32], F32)
            obp = sbpool(nc, esb, "ob", 3, [128, 128], BF16)
            P.dma(negbig[:], I["c_negbig"][:, :], W=[negbig.b], sb=negbig.b)
            P.dma(posbig[:], I["c_posbig"][:, :], W=[posbig.b], sb=posbig.b)
            P.dma(iwt[:], Sx["iw"].rearrange("(t p) h -> p t h", p=128), W=[iwt.b], sb=iwt.b)
            P.dma(ik2[0:64, :], Sx["ikT"][:, :], W=[ik2.b], sb=ik2.b)
            P.dma(ik2[64:128, :], Sx["ikT"][:, :], W=[ik2.b], sb=ik2.b)
            for i in range(4):
                P.dma(Kd[i][:], Sx["KT_D"][i * 128:(i + 1) * 128, :], W=[Kd[i].b], sb=Kd[i].b)
                load_vaug(Vd[i], Sx["V_D"], i * 128)
            NBIS = 16

            def idx_accum(qt, out):
                Lq = (qt + 1) * 128
                idx = idxp.next()
                iq = iqp.next()
                P.dma(iq[:], Sx["iqT"][:, qt * 128:(qt + 1) * 128].rearrange("(g p) c -> p g c", p=128),
                      W=[iq.b], sb=iq.b)
                for c0 in range(0, Lq, 512):
                    w = min(512, Lq - c0)
                    for h in range(8):
                        pbk = psb.next()
                        psv = pbk[:].bitcast(F32)
                        p0 = (h % 2) * 64
                        P.mm(psv[:, :w], iq[p0:p0 + 64, h // 2, :], ik2[p0:p0 + 64, c0:c0 + w],
                             True, True, [iq.b, ik2.b], [pbk.b])
                        r = rp.next()
                        P.act(r[:, :w], psv[:, :w], AF.Relu, [pbk.b], [r.b])
                        if h == 0:
                            P.ts("dve", idx[:, c0:c0 + w], r[:, :w], iwt[:, qt, 0:1], None, ALU.mult, None,
                                 [r.b, iwt.b], [idx.b])
                        else:
                            P.stt(idx[:, c0:c0 + w], r[:, :w], iwt[:, qt, h:h + 1], idx[:, c0:c0 + w], ALU.mult, ALU.add,
                                  [r.b, iwt.b, idx.b], [idx.b])
                    yield
                b = bs.next()
                d0 = qt * 128
                if qt >= 2:
                    dtmp = dtmpp.next()
                    P.tt("dve", dtmp[:], idx[:, d0:Lq], posbig[:], ALU.add, [idx.b, posbig.b], [dtmp.b])
                    P.red(b[:, 0:1], dtmp[:], ALU.min, [dtmp.b], [b.b])
                    P.red(b[:, 1:2], idx[:, 0:d0], ALU.min, [idx.b], [b.b])
                    P.tt("dve", b[:, 0:1], b[:, 0:1], b[:, 1:2], ALU.min, [b.b], [b.b])
                P.tt("dve", idx[:, d0:Lq], idx[:, d0:Lq], negbig[:], ALU.add, [idx.b, negbig.b], [idx.b])
                if qt >= 2:
                    P.red(b[:, 1:2], idx[:, 0:Lq], ALU.max, [idx.b], [b.b])
                    P.tt("dve", b[:, 2:3], b[:, 1:2], b[:, 0:1], ALU.subtract, [b.b], [b.b])
                    P.memset("dve", b[:, 8:8 + NBIS], 0.0, [b.b])
                else:
                    P.memset("dve", b[:, 0:1], -1e29, [b.b])
                out.append(dict(qt=qt, Lq=Lq, idx=idx, b=b))

            def bisect(states):
                sts = [x for x in states if x["qt"] >= 2]
                for it in range(NBIS):
                    f = 0.5 ** (it + 1)
                    for x in sts:
                        b = x["b"]
                        P.stt(b[:, 3:4], b[:, 2:3], -f, b[:, 0:1], ALU.mult, ALU.subtract, [b.b], [b.b])
                    for x in sts:
                        b, idx, Lq = x["b"], x["idx"], x["Lq"]
                        P.act(bjunk[:, :Lq], idx[:, :Lq], AF.Sign, [idx.b, b.b], [bjunk.b, b.b], bias=b[:, 3:4],
                              accum=b[:, 8 + it:9 + it])
                    for x in sts:
                        b, Lq = x["b"], x["Lq"]
                        P.ts("dve", b[:, 5:6], b[:, 8 + it:9 + it], 510.5 - Lq, f, ALU.is_ge, ALU.mult, [b.b], [b.b])
                        P.stt(b[:, 0:1], b[:, 5:6], b[:, 2:3], b[:, 0:1], ALU.mult, ALU.add, [b.b], [b.b])
                    yield

            def run_rr(gens):
                gens = list(gens)
                while gens:
                    nxt = []
                    for g_ in gens:
                        try:
                            next(g_)
                            nxt.append(g_)
                        except StopIteration:
                            pass
                    gens = nxt

            def accum_pair(p):
                out = []
                accd[p] = out
                for qt in (2 * p, 2 * p + 1):
                    yield from idx_accum(qt, out)

            def mk_pair(p):
                res = {}
                for x in accd.pop(p):
                    Mk = Mkp.next()
                    P.ts("pool", Mk[:, :x["Lq"]], x["idx"][:, :x["Lq"]], x["b"][:, 0:1], NEG, ALU.is_lt, ALU.mult,
                         [x["idx"].b, x["b"].b], [Mk.b])
                    res[x["qt"]] = Mk
                return res

            accd = {}
            NP = NT // 2

            def jobsD():
                run_rr([accum_pair(0)])
                run_rr([bisect(accd[0])])
                mks = dict(mk_pair(0))
                run_rr([accum_pair(1)])
                for qt in range(NT):
                    st = {"Mk": mks[qt]}
                    qd = qdp.next()
                    P.dma(qd[:], Sx["QT_D"][:, qt * 128:(qt + 1) * 128].rearrange("(h p) c -> p h c", p=128),
                          W=[qd.b], sb=qd.b)

                    def pre(qt=qt, mks=mks):
                        p1 = qt // 2 + 1
                        if p1 < NP:
                            gens = [bisect(accd[p1])]
                            if p1 + 1 < NP:
                                gens.append(accum_pair(p1 + 1))
                            run_rr(gens)
                            mks.update(mk_pair(p1))

                    for h in range(4):
                        acc = accp.next()

                        def post(h=h, qt=qt, acc=acc):
                            r = C.small.next()
                            recip_sum(acc, 128, r[:, 0:1], r.b)
                            ob = obp.next()
                            P.ts("dve", ob[:], acc[:, 0:128], r[:, 0:1], None, ALU.mult, None, [acc.b, r.b], [ob.b])
                            P.dma(Sx["O_D"][qt * 128:(qt + 1) * 128, h * 128:(h + 1) * 128], ob[:], R=[ob.b], sb=ob.b)

                        def score_fn(kt, h=h, qd=qd):
                            return [(Kd[h][:, kt * 128:(kt + 1) * 128], qd[:, h, :], [Kd[h].b, qd.b])]

                        def mask_fn(kt, st=st):
                            Mk = st["Mk"]
                            return [(Mk[:, kt * 128:(kt + 1) * 128], ident[:], [Mk.b, ident.b])]

                        def v_fn(kt, h=h):
                            return Vd[h][:, kt, :], [Vd[h].b]

                        yield from head_jobs(ptp, score_fn, mask_fn, v_fn, acc, 129, range(qt + 1), 128 ** -0.5, post,
                                             pre if (h == 0 and qt % 2 == 0) else None)
            run_jobs(jobsD())
            P.barrier()
        if stop_after == "BD":
            break

        if "B" in branches:
          with scope() as esb:
            ptp = sbpool(nc, esb, "pt", 3, [128, 512], BF16)
            Qb = [sb(nc, esb, f"Qb{i}", [128, S], BF16) for i in range(4)]
            ks = sb(nc, esb, "ks", [128, S], BF16)
            kw = sb(nc, esb, "kw", [128, S], BF16)
            vsa = sb(nc, esb, "vsa", [128, NT, 129], BF16)
            vwa = sb(nc, esb, "vwa", [128, NT, 129], BF16)
            kcm = sb(nc, esb, "kcm", [128, 256], BF16)
            vca = sb(nc, esb, "vca", [128, 2, 193], BF16)
            gBt = sb(nc, esb, "gBt", [128, NT, 12], F32)
            selb = sb(nc, esb, "selb", [128, NT, 64], F32)
            cmp_ = sbpool(nc, esb, "cm", 2, [128, 256], BF16)
            Mkp = sbpool(nc, esb, "MkB", 2, [128, S], BF16)
            obf = sbpool(nc, esb, "obf", 2, [128, 4, 128], F32)
            impp = sbpool(nc, esb, "imp", 2, [128, 64], F32)
            scp = sbpool(nc, esb, "sc", 2, [128, 64], F32)
            cmpm = sb(nc, esb, "cmpm", [128, 64, 64], F32)
            rank = sbpool(nc, esb, "rank", 2, [128, 64], F32)
            obp = sbpool(nc, esb, "ob", 3, [128, 128], BF16)
            coefp = sbpool(nc, esb, "coef", 8, [128, 2], F32)
            for i in range(4):
                P.dma(Qb[i][:], Sx["QT_B"][i * 128:(i + 1) * 128, :], W=[Qb[i].b], sb=Qb[i].b)
            P.dma(ks[:], Sx["bkT"][128:256, :], W=[ks.b], sb=ks.b)
            P.dma(kw[:], Sx["bkT"][256:384, :], W=[kw.b], sb=kw.b)
            load_vaug(vsa, Sx["vs"], 0)
            load_vaug(vwa, Sx["vw"], 0)
            P.dma(kcm[:], Sx["kcmpT"][:, :], W=[kcm.b], sb=kcm.b)
            P.dma(vca[:, :, 0:128], Sx["vcmp"].rearrange("(g p) d -> p g d", p=128), W=[vca.b], sb=vca.b)
            P.memset("pool", vca[:, :, 128:129], 1.0, [vca.b])
            P.dma(vca[:, :, 129:193], I["c_overlap"].rearrange("(g p) j -> p g j", p=128), W=[vca.b], sb=vca.b)
            P.dma(gBt[:], Sx["gB"].rearrange("(t p) g -> p t g", p=128), W=[gBt.b], sb=gBt.b)
            P.dma(selb[:], I["c_selb"].rearrange("(t p) j -> p t j", p=128), W=[selb.b], sb=selb.b)

            def jobsB():
                QS = {}

                def setup(qt):
                    Lq = (qt + 1) * 128
                    cm = cmp_.next()
                    P.dma(cm[:], I["c_cmask"][qt * 128:(qt + 1) * 128, :], W=[cm.b], sb=cm.b)
                    of4 = obf.next()
                    imp = impp.next()
                    st = {}

                    def coef(acc, col, gcol, qt=qt):
                        cf = coefp.next()
                        recip_sum(acc, col, cf[:, 0:1], cf.b)
                        P.tt("dve", cf[:, 1:2], cf[:, 0:1], gBt[:, qt, gcol:gcol + 1], ALU.mult, [cf.b, gBt.b], [cf.b])
                        return cf
                    QS[qt] = (Lq, cm, of4, imp, st, coef)

                def cmp_jobs(qt):
                    Lq, cm, of4, imp, st, coef = QS[qt]
                    for h in range(4):
                        acc = accp.next()

                        def post(h=h, acc=acc, of4=of4, imp=imp, coef=coef):
                            cf = coef(acc, 128, 3 * h + 0)
                            P.ts("dve", of4[:, h, :], acc[:, 0:128], cf[:, 1:2], None, ALU.mult, None, [acc.b, cf.b], [of4.b])
                            if h == 0:
                                P.ts("dve", imp[:], acc[:, 129:193], cf[:, 0:1], None, ALU.mult, None, [acc.b, cf.b], [imp.b])
                            else:
                                P.stt(imp[:], acc[:, 129:193], cf[:, 0:1], imp[:], ALU.mult, ALU.add,
                                      [acc.b, cf.b, imp.b], [imp.b])

                        def score_fn(kt, h=h, qt=qt):
                            return [(kcm[:, kt * 128:(kt + 1) * 128], Qb[h][:, qt * 128:(qt + 1) * 128], [kcm.b, Qb[h].b])]

                        def mask_fn(kt, cm=cm):
                            return [(cm[:, kt * 128:(kt + 1) * 128], ident[:], [cm.b, ident.b])]

                        def v_fn(kt):
                            return vca[:, kt, :], [vca.b]

                        yield from head_jobs(ptp, score_fn, mask_fn, v_fn, acc, 193, range(2), 128 ** -0.5, post)


                def make_select(qt):
                    Lq, cm, of4, imp, st, coef = QS[qt]
                    def select(qt=qt, imp=imp, st=st, Lq=Lq):
                        sc = scp.next()
                        P.tt("dve", sc[:], imp[:], selb[:, qt, :], ALU.add, [imp.b, selb.b], [sc.b])
                        P.tt("dve", cmpm[:], sc[:].unsqueeze(1).to_broadcast([128, 64, 64]),
                             sc[:].unsqueeze(2).to_broadcast([128, 64, 64]), ALU.is_gt, [sc.b], [cmpm.b])
                        rk = rank.next()
                        P.red(rk[:], cmpm[:], ALU.add, [cmpm.b], [rk.b])
                        P.ts("dve", rk[:], rk[:], 15.5, NEG, ALU.is_gt, ALU.mult, [rk.b], [rk.b])
                        Mk = Mkp.next()
                        nb = Lq // 64
                        P.copy("pool", Mk[:, :Lq].rearrange("p (j c) -> p j c", c=64),
                               rk[:, :nb].unsqueeze(2).to_broadcast([128, nb, 64]), [rk.b], [Mk.b])
                        P.tt("pool", Mk[:, Lq - 128:Lq], Mk[:, Lq - 128:Lq], causal[:], ALU.add, [Mk.b, causal.b], [Mk.b])
                        st["Mk"] = Mk

                    return select

                def slc_jobs(qt):
                    Lq, cm, of4, imp, st, coef = QS[qt]
                    for h in range(4):
                        acc = accp.next()

                        def post(h=h, acc=acc, of4=of4, coef=coef):
                            cf = coef(acc, 128, 3 * h + 1)
                            P.stt(of4[:, h, :], acc[:, 0:128], cf[:, 1:2], of4[:, h, :], ALU.mult, ALU.add,
                                  [acc.b, cf.b, of4.b], [of4.b])

                        def score_fn(kt, h=h, qt=qt):
                            return [(ks[:, kt * 128:(kt + 1) * 128], Qb[h][:, qt * 128:(qt + 1) * 128], [ks.b, Qb[h].b])]

                        def mask_fn(kt, st=st):
                            Mk = st["Mk"]
                            return [(Mk[:, kt * 128:(kt + 1) * 128], ident[:], [Mk.b, ident.b])]

                        def v_fn(kt):
                            return vsa[:, kt, :], [vsa.b]

                        yield from head_jobs(ptp, score_fn, mask_fn, v_fn, acc, 129, range(qt + 1), 128 ** -0.5, post)


                def win_jobs(qt, pre):
                    Lq, cm, of4, imp, st, coef = QS[qt]
                    for h in range(4):
                        acc = accp.next()

                        def post(h=h, acc=acc, of4=of4, qt=qt, coef=coef):
                            cf = coef(acc, 128, 3 * h + 2)
                            P.stt(of4[:, h, :], acc[:, 0:128], cf[:, 1:2], of4[:, h, :], ALU.mult, ALU.add,
                                  [acc.b, cf.b, of4.b], [of4.b])
                            ob = obp.next()
                            P.copy("pool", ob[:], of4[:, h, :], [of4.b], [ob.b])
                            P.dma(Sx["O_B"][qt * 128:(qt + 1) * 128, h * 128:(h + 1) * 128], ob[:], R=[ob.b], sb=ob.b)

                        def score_fn(kt, h=h, qt=qt):
                            return [(kw[:, kt * 128:(kt + 1) * 128], Qb[h][:, qt * 128:(qt + 1) * 128], [kw.b, Qb[h].b])]

                        def mask_fn(kt, qt=qt):
                            if kt == qt:
                                return [(causal[:], ident[:], [causal.b, ident.b])]
                            if kt == qt - 4:
                                return [(anti[:], ident[:], [anti.b, ident.b])]
                            return []

                        def v_fn(kt):
                            return vwa[:, kt, :], [vwa.b]

                        yield from head_jobs(ptp, score_fn, mask_fn, v_fn, acc, 129, range(max(0, qt - 4), qt + 1),
                                             128 ** -0.5, post, pre if h == 0 else None)

                setup(0)
                yield from cmp_jobs(0)
                sel0 = make_select(0)
                first = True
                for qt in range(NT):
                    if qt + 1 < NT:
                        setup(qt + 1)
                        gen = cmp_jobs(qt + 1)
                        if first:
                            j0 = next(gen)
                            j0.pre = sel0
                            yield j0
                            first = False
                        yield from gen
                    yield from slc_jobs(qt)
                    yield from win_jobs(qt, make_select(qt + 1) if qt + 1 < NT else None)
            run_jobs(jobsB())
            P.barrier()
        if stop_after == "BB":
            break
        with scope() as esc:
            wstage = sbpool(nc, esc, "wstC", 2, [128, 1024], F32)
            Wg = load_w_bf16(esc, "Wg", I["w_in"][l][:, OFF["gate"]:OFF["gate"] + 4096], 8, 4096, wstage)
            Wb = load_w_bf16(esc, "Wb", I["w_branch"][l].rearrange("n w d -> (n w) d"), 16, D, wstage)
            Wo = load_w_bf16(esc, "Wo", I["w_out"][l], 8, D, wstage)
            g1 = load_bcast(esc, "g1c", I["norm1_g"][l:l + 1, :], D)
            xs = sbpool(nc, esc, "xsC", 2, [128, D], F32)
            xop = sbpool(nc, esc, "xoC", 2, [128, D], F32)
            hbp = sbpool(nc, esc, "hbC", 2, [128, D], BF16)
            hTp = sbpool(nc, esc, "hTC", 2, [128, 8, 128], BF16)
            ob4p = sbpool(nc, esc, "ob4", 1, [128, 4, 512], BF16)
            oTp = sbpool(nc, esc, "oT", 1, [128, 16, 128], BF16)
            mrg = sbpool(nc, esc, "mrg", 1, [128, D], F32)
            mbp = sbpool(nc, esc, "mb", 1, [128, D], BF16)
            mTp = sbpool(nc, esc, "mT", 2, [128, 8, 128], BF16)
            sgp = sbpool(nc, esc, "sg", 2, [128, 512], F32)
            tmpp = sbpool(nc, esc, "tmpC", 2, [128, 512], F32)
            for tt in range(NT):
                rows = slice(tt * 128, (tt + 1) * 128)
                xt = xs.next()
                P.dma(xt[:], xsrc[rows, :], W=[xt.b], sb=xt.b)
                ob4 = ob4p.next()
                for n_, nm in enumerate(("O_A", "O_B", "O_C", "O_D")):
                    P.dma(ob4[:, n_, :], Sx[nm][rows, :], W=[ob4.b], sb=ob4.b)
                ss = rms(xt[:], D, [xt.b])
                hb = hbp.next()
                P.stt(hb[:], xt[:], ss[:, 1:2], g1[:], ALU.mult, ALU.mult, [xt.b, ss.b, g1.b], [hb.b])
                pb = psb.next()
                for k in range(8):
                    P.tr(pb[:, k * 128:(k + 1) * 128], hb[:, k * 128:(k + 1) * 128], [hb.b, ident.b], [pb.b])
                hTt = hTp.next()
                P.copy("dve", hTt[:], pb[:].rearrange("p (k c) -> p k c", k=8), [pb.b], [hTt.b])
                oT = oTp.next()
                for g in range(2):
                    pb = psb.next()
                    for k in range(8):
                        kk = g * 8 + k
                        P.tr(pb[:, k * 128:(k + 1) * 128], ob4[:, kk // 4, (kk % 4) * 128:(kk % 4 + 1) * 128],
                             [ob4.b, ident.b], [pb.b])
                    P.copy("dve", oT[:, g * 8:(g + 1) * 8, :], pb[:].rearrange("p (k c) -> p k c", k=8), [pb.b], [oT.b])
                mg = mrg.next()
                for n_ in range(4):
                    for j in range(2):
                        cs = slice(j * 512, (j + 1) * 512)
                        pg = psf.next()
                        for k in range(8):
                            P.mm(pg[:, :], hTt[:, k, :], Wg[:, k, n_ * 1024 + j * 512:n_ * 1024 + (j + 1) * 512],
                                 k == 0, k == 7, [hTt.b, Wg.b], [pg.b])
                        sg = sgp.next()
                        P.act(sg[:], pg[:, :], AF.Sigmoid, [pg.b], [sg.b])
                        pbr = psf.next()
                        for k in range(4):
                            P.mm(pbr[:, :], oT[:, n_ * 4 + k, :], Wb[:, n_ * 4 + k, cs], k == 0, k == 3, [oT.b, Wb.b], [pbr.b])
                        if n_ == 0:
                            P.tt("dve", mg[:, cs], pbr[:, :], sg[:], ALU.mult, [pbr.b, sg.b], [mg.b])
                        else:
                            tm = tmpp.next()
                            P.tt("dve", tm[:], pbr[:, :], sg[:], ALU.mult, [pbr.b, sg.b], [tm.b])
                            P.tt("pool", mg[:, cs], mg[:, cs], tm[:], ALU.add, [mg.b, tm.b], [mg.b])
                mb = mbp.next()
                P.copy("pool", mb[:], mg[:], [mg.b], [mb.b])
                pb = psb.next()
                for k in range(8):
                    P.tr(pb[:, k * 128:(k + 1) * 128], mb[:, k * 128:(k + 1) * 128], [mb.b, ident.b], [pb.b])
                mT = mTp.next()
                P.copy("dve", mT[:], pb[:].rearrange("p (k c) -> p k c", k=8), [pb.b], [mT.b])
                xo = xop.next()
                for j in range(2):
                    cs = slice(j * 512, (j + 1) * 512)
                    po = psf.next()
                    for k in range(8):
                        P.mm(po[:, :], mT[:, k, :], Wo[:, k, cs], k == 0, k == 7, [mT.b, Wo.b], [po.b])
                    P.tt("dve", xo[:, cs], po[:, :], xt[:, cs], ALU.add, [po.b, xt.b], [xo.b])
                P.dma(Sx["xb"][rows, :], xo[:], R=[xo.b], sb=xo.b)
            P.barrier()
        if stop_after == "Ca":
            break

        with scope() as esd:
            wstage = sbpool(nc, esd, "wstD", 2, [128, 1024], F32)
            Wgu = load_w_bf16(esd, "Wgu", I["w_gate_up"][l], 8, 2 * DFF, wstage)
            Wd = load_w_bf16(esd, "Wd", I["w_down"][l], 22, D, wstage)
            g2 = load_bcast(esd, "g2", I["norm2_g"][l:l + 1, :], D)
            last = (li == n_layers - 1)
            if last and final_norm:
                gf = load_bcast(esd, "gf", I["final_norm_g"][0:1, :], D)
            xs = sbpool(nc, esd, "xsD", 2, [128, D], F32)
            xop = sbpool(nc, esd, "xoD", 2, [128, D], F32)
            hbp = sbpool(nc, esd, "hbD", 2, [128, D], BF16)
            hTp = sbpool(nc, esd, "hTD", 2, [128, 8, 128], BF16)
            actp = sbpool(nc, esd, "actb", 1, [128, DFF], BF16)
            aTp = sbpool(nc, esd, "aT", 1, [128, 22, 128], BF16)
            sgp = sbpool(nc, esd, "sgD", 3, [128, 256], F32)
            for tt in range(NT):
                rows = slice(tt * 128, (tt + 1) * 128)
                xt = xs.next()
                P.dma(xt[:], Sx["xb"][rows, :], W=[xt.b], sb=xt.b)
                ss = rms(xt[:], D, [xt.b])
                hb = hbp.next()
                P.stt(hb[:], xt[:], ss[:, 1:2], g2[:], ALU.mult, ALU.mult, [xt.b, ss.b, g2.b], [hb.b])
                pb = psb.next()
                for k in range(8):
                    P.tr(pb[:, k * 128:(k + 1) * 128], hb[:, k * 128:(k + 1) * 128], [hb.b, ident.b], [pb.b])
                hTt = hTp.next()
                P.copy("dve", hTt[:], pb[:].rearrange("p (k c) -> p k c", k=8), [pb.b], [hTt.b])
                ab = actp.next()
                for j in range(11):
                    pg = psf.next()
                    for part in range(2):
                        c0 = part * DFF + j * 256
                        for k in range(8):
                            P.mm(pg[:, part * 256:(part + 1) * 256], hTt[:, k, :], Wgu[:, k, c0:c0 + 256], k == 0, k == 7,
                                 [hTt.b, Wgu.b], [pg.b])
                    sg = sgp.next()
                    P.act(sg[:], pg[:, 0:256], AF.Silu, [pg.b], [sg.b])
                    P.tt("dve", ab[:, j * 256:(j + 1) * 256], pg[:, 256:512], sg[:], ALU.mult, [pg.b, sg.b], [ab.b])
                aT = aTp.next()
                for g0 in range(0, 22, 8):
                    ng = min(8, 22 - g0)
                    pb = psb.next()
                    for k in range(ng):
                        P.tr(pb[:, k * 128:(k + 1) * 128], ab[:, (g0 + k) * 128:(g0 + k + 1) * 128], [ab.b, ident.b], [pb.b])
                    P.copy("dve", aT[:, g0:g0 + ng, :], pb[:, :ng * 128].rearrange("p (k c) -> p k c", k=ng), [pb.b], [aT.b])
                xo = xop.next()
                for j in range(2):
                    cs = slice(j * 512, (j + 1) * 512)
                    pd = psf.next()
                    for k in range(22):
                        P.mm(pd[:, :], aT[:, k, :], Wd[:, k, cs], k == 0, k == 21, [aT.b, Wd.b], [pd.b])
                    P.tt("dve", xo[:, cs], pd[:, :], xt[:, cs], ALU.add, [pd.b, xt.b], [xo.b])
                if last:
                    if final_norm:
                        ss2 = rms(xo[:], D, [xo.b])
                        P.stt(xo[:], xo[:], ss2[:, 1:2], gf[:], ALU.mult, ALU.mult, [xo.b, ss2.b, gf.b], [xo.b])
                    P.dma(out[rows, :], xo[:], R=[xo.b], sb=xo.b)
                else:
                    P.dma(Sx["xa"][rows, :], xo[:], R=[xo.b], sb=xo.b)
            P.barrier()

    P.barrier()
    with nc.Block() as blk:
        @blk.tensor
        def _(e):
            P.replay(e, "pe")

        @blk.scalar
        def _(e):
            P.replay(e, "act")

        @blk.vector
        def _(e):
            P.replay(e, "dve")

        @blk.gpsimd
        def _(e):
            P.replay(e, "pool")

        @blk.sync
        def _(e):
            P.replay(e, "sp")
    es_top.close()
    C.nsem = P.nsem
    C.maxval = P.maxval
    C.counts = dict(P.seq)
    return nc, C


def host_consts():
    bf = ml_dtypes.bfloat16
    c = {}
    c["c_ident"] = np.eye(128, dtype=np.float32).astype(bf)
    t = np.arange(128)[:, None]
    s = np.arange(128)[None, :]
    c["c_causal"] = np.where(s > t, NEG, 0.0).astype(np.float32).astype(bf)
    c["c_anti"] = np.where(s <= t, NEG, 0.0).astype(np.float32).astype(bf)
    tt = np.arange(S)[:, None]
    n = np.arange(256)[None, :]
    c["c_cmask"] = np.where(16 * n + 31 > tt, NEG, 0.0).astype(np.float32).astype(bf)
    j = np.arange(64)[None, :]
    cur = tt // 64
    forced = (j == 0) | (j == cur) | (j == cur - 1)
    visible = (j * 64) <= tt
    c["c_selb"] = np.where(visible, np.where(forced, 1e9, 0.0), -1e30).astype(np.float32)
    c["c_negbig"] = np.where(s > t, -1e30, 0.0).astype(np.float32)
    c["c_posbig"] = np.where(s > t, 1e30, 0.0).astype(np.float32)
    nn = np.arange(256)[:, None]
    cs = nn * 16
    ce = cs + 31
    ss_ = np.arange(64)[None, :] * 64
    ov = ((cs < ss_ + 64) & (ce >= ss_)).astype(np.float32)
    ov[255, :] = 0.0
    c["c_overlap"] = ov.astype(bf)
    for nm, rot in (("da", 16), ("nsa", 32), ("mla", 64), ("dsa", 32), ("idx", 16)):
        inv = np.power(np.float32(500000.0), -np.arange(0, rot, 2, dtype=np.float32) / np.float32(rot)).astype(np.float32)
        ang = (np.arange(S, dtype=np.float32)[:, None] * inv[None, :]).astype(np.float32)
        c["cos_" + nm] = np.cos(ang).astype(np.float32)
        c["sin_" + nm] = np.sin(ang).astype(np.float32)
    return c


_CACHE = {}


def kernel(**inputs):
    x = np.ascontiguousarray(inputs["x"], dtype=np.float32)
    if "prog" not in _CACHE:
        _CACHE["prog"] = build(DEPTH)
    nc, C = _CACHE["prog"]
    consts = host_consts()
    base = {k: np.ascontiguousarray(v, dtype=np.float32) for k, v in inputs.items() if k != "x"}
    base["final_norm_g"] = base["final_norm_g"].reshape(1, D)
    base.update(consts)
    in_maps = []
    cmap = {0: 0, 1: 1, 4: 2, 5: 3}
    zx = np.zeros_like(x[0])
    for c in range(8):
        m = dict(base)
        m["x"] = x[cmap[c]] if c in cmap else zx
        in_maps.append(m)
    res = run_bass_kernel_spmd(nc, in_maps, core_ids=list(range(8)))
    inv = {b: c for c, b in cmap.items()}
    outs = [res.results[inv[b]]["out"] for b in range(4)]
    return np.stack(outs, axis=0).astype(np.float32)
```

```python
import math
from contextlib import ExitStack
import numpy as np
import ml_dtypes
import concourse.bass as bass
import concourse.mybir as mybir
from concourse.bass_utils import run_bass_kernel_spmd

F32 = mybir.dt.float32
BF16 = mybir.dt.bfloat16
AF = mybir.ActivationFunctionType
ALU = mybir.AluOpType
AX = mybir.AxisListType

S = 4096
D = 1024
NT = S // 128
DEPTH = 4
D_IN = 9748
DFF = 2816
NEG = -30000.0
EPOCH = 16000
DEPOCH = 1000
TINY = 1e-30

OFF = {}
_o = 0
for _nm, _n in (("a_q", 512), ("a_k", 512), ("a_v", 512), ("b_q", 512), ("b_kc", 128), ("b_vc", 128),
                ("b_ks", 128), ("b_vs", 128), ("b_kw", 128), ("b_vw", 128), ("b_g", 12), ("c_q", 384),
                ("c_kv", 256), ("c_kr", 64), ("d_q", 512), ("d_k", 512), ("d_v", 512), ("d_iq", 512),
                ("d_ik", 64), ("d_iw", 8), ("gate", 4096)):
    OFF[_nm] = _o
    _o += _n
assert _o == D_IN


_BUID = [0]


class Buf:
    __slots__ = ("name", "w", "rs", "dsem", "dbase", "dcnt", "uid")

    def __init__(self, name):
        self.name = name
        self.w = None
        self.rs = {}
        self.dsem = None
        self.dbase = 0
        self.dcnt = 0
        _BUID[0] += 1
        self.uid = _BUID[0]


class Tn:
    def __init__(self, t, name):
        self.t = t
        self.b = Buf(name)

    def __getitem__(self, k):
        return self.t[k]


class FreePool:
    def __init__(self, items):
        self.items = items
        self.free_list = list(items)

    def alloc(self):
        assert self.free_list, "FreePool exhausted"
        return self.free_list.pop(0)

    def free(self, x):
        self.free_list.append(x)


class Pool:
    def __init__(self, items):
        self.items = items
        self.i = 0

    def next(self):
        x = self.items[self.i % len(self.items)]
        self.i += 1
        return x


ENGS = ("pe", "act", "dve", "pool", "sp")


class Prog:
    def __init__(self, nc, es):
        self.nc = nc
        self.es = es
        self.streams = {e: [] for e in ENGS}
        self.seq = {e: 0 for e in ENGS}
        self.esem = {e: [] for e in ENGS}
        self.wd = {e: {} for e in ENGS}
        self.dirty = {}
        self.nsem = 0
        self.ident = None
        self.free_d = []
        self.maxval = 0
        self.scopes = []

    def _newsem(self, name):
        self.nsem += 1
        return self.es.enter_context(self.nc.semaphore(name))

    def _ev_sem(self, ev):
        if ev[0] == "e":
            _, eng, seq = ev
            ep = (seq - 1) // EPOCH
            while len(self.esem[eng]) <= ep:
                self.esem[eng].append(self._newsem(f"e_{eng}_{len(self.esem[eng])}"))
            return self.esem[eng][ep], (seq - 1) % EPOCH + 1
        _, buf, cnt = ev
        if buf.dsem is None:
            if self.free_d:
                buf.dsem, buf.dbase = self.free_d.pop(0)
            else:
                buf.dsem, buf.dbase = self._newsem(f"d_{self.nsem}"), 0
        v = buf.dbase + 16 * cnt
        self.maxval = max(self.maxval, v)
        assert v < 60000
        return buf.dsem, v

    def release(self, buf):
        if buf.dsem is not None:
            self.free_d.append((buf.dsem, buf.dbase + 16 * buf.dcnt))
            buf.dsem = None
            buf.dcnt = 0
            buf.dbase = 0

    def _wait(self, eng, ev, raw):
        if ev[0] == "e":
            if ev[1] == eng and (eng == "pe" or eng == "sp" or not raw):
                return
            key = ("e", ev[1])
            val = ev[2]
        else:
            key = ("d", ev[1].uid)
            val = ev[2]
        if self.wd[eng].get(key, 0) >= val:
            return
        self.wd[eng][key] = val
        sem, v = self._ev_sem(ev)
        self.streams[eng].append(("w", sem, v))

    def _deps(self, eng, reads, writes):
        for b in reads:
            if b.w is not None:
                self._wait(eng, b.w, True)
        for b in writes:
            if b.w is not None:
                self._wait(eng, b.w, False)
            for ev in b.rs.values():
                self._wait(eng, ev, False)

    def _mark(self, me, reads, writes):
        for b in reads:
            k = ("e", me[1]) if me[0] == "e" else ("d", me[1].uid)
            b.rs[k] = me
        for b in writes:
            b.w = me
            b.rs = {}

    def op(self, eng, fn, reads=(), writes=()):
        self._deps(eng, reads, writes)
        self.seq[eng] += 1
        me = ("e", eng, self.seq[eng])
        sem, _ = self._ev_sem(me)
        self.streams[eng].append(("o", fn, sem, 1))
        self._mark(me, reads, writes)

    def dma(self, out, in_, R=(), W=(), sb=None, q="sp", slow=False):
        assert sb is not None
        self._deps(q, R, W)
        if sb.dcnt > 0:
            self._wait(q, ("d", sb, sb.dcnt), False)
        sb.dcnt += 1
        me = ("d", sb, sb.dcnt)
        sem, _ = self._ev_sem(me)
        if slow:
            fn = lambda e: e.dma_start(out=out, in_=in_, allow_slow_non_contiguous=True)
        else:
            fn = lambda e: e.dma_start(out=out, in_=in_)
        self.streams[q].append(("o", fn, sem, 16))
        self.dirty[sb.uid] = sb
        self._mark(me, R, W)

    def barrier(self):
        for eng in ENGS:
            for x in ENGS:
                if x != eng and self.seq[x] > 0:
                    ev = ("e", x, self.seq[x])
                    key = ("e", x)
                    if self.wd[eng].get(key, 0) < ev[2]:
                        self.wd[eng][key] = ev[2]
                        sem, v = self._ev_sem(ev)
                        self.streams[eng].append(("w", sem, v))
            for b in self.dirty.values():
                self._wait(eng, ("d", b, b.dcnt), False)
        self.dirty = {}

    def replay(self, e, eng):
        for it in self.streams[eng]:
            if it[0] == "w":
                e.wait_ge(it[1], it[2])
            else:
                it[1](e).then_inc(it[2], it[3])

    def mm(self, out, lhsT, rhs, start, stop, R, W):
        self.op("pe", lambda e: e.matmul(out, lhsT=lhsT, rhs=rhs, start=start, stop=stop), R, W)

    def tr(self, out, in_, R, W):
        k = in_.shape[0]
        idn = self.ident[:k, :k]
        self.op("pe", lambda e: e.transpose(out=out, in_=in_, identity=idn), R, W)

    def act(self, out, in_, func, R, W, scale=1.0, bias=None, accum=None):
        kw = {}
        if bias is not None:
            kw["bias"] = bias
        if accum is not None:
            kw["accum_out"] = accum
        self.op("act", lambda e: e.activation(out=out, in_=in_, func=func, scale=scale, **kw), R, W)

    def tt(self, eng, out, in0, in1, op, R, W):
        self.op(eng, lambda e: e.tensor_tensor(out=out, in0=in0, in1=in1, op=op), R, W)

    def ts(self, eng, out, in0, s1, s2, op0, op1, R, W, accum=None):
        if op1 is None:
            self.op(eng, lambda e: e.tensor_scalar(out=out, in0=in0, scalar1=s1, scalar2=None, op0=op0), R, W)
        elif accum is None:
            self.op(eng, lambda e: e.tensor_scalar(out=out, in0=in0, scalar1=s1, scalar2=s2, op0=op0, op1=op1), R, W)
        else:
            self.op(eng, lambda e: e.tensor_scalar(out=out, in0=in0, scalar1=s1, scalar2=s2, op0=op0, op1=op1,
                                                   accum_out=accum), R, W)

    def stt(self, out, in0, scalar, in1, op0, op1, R, W):
        self.op("dve", lambda e: e.scalar_tensor_tensor(out=out, in0=in0, scalar=scalar, in1=in1, op0=op0, op1=op1),
                R, W)

    def copy(self, eng, out, in_, R, W):
        if eng == "act":
            self.op("act", lambda e: e.activation(out=out, in_=in_, func=AF.Copy), R, W)
        else:
            self.op(eng, lambda e: e.tensor_copy(out=out, in_=in_), R, W)

    def memset(self, eng, ap, val, W):
        self.op(eng, lambda e: e.memset(ap, val), (), W)

    def red(self, out, in_, op, R, W):
        self.op("dve", lambda e: e.tensor_reduce(out=out, in_=in_, axis=AX.X, op=op), R, W)


class Ctx:
    pass


_UNIQ = [0]


_CURP = [None]


def sb(nc, es, name, shape, dt):
    _UNIQ[0] += 1
    nm = f"s{_UNIQ[0]}_{name}"
    t = Tn(es.enter_context(nc.sbuf_tensor(nm, list(shape), dt)), nm)
    P = _CURP[0]
    if P is not None and P.scopes:
        P.scopes[-1].append(t.b)
    return t


class scope:
    def __enter__(self):
        self.P = _CURP[0]
        self.P.scopes.append([])
        self.es = ExitStack()
        return self.es.__enter__()

    def __exit__(self, *a):
        r = self.es.__exit__(*a)
        for b in self.P.scopes.pop():
            self.P.release(b)
        return r


def sbpool(nc, es, name, n, shape, dt):
    return Pool([sb(nc, es, f"{name}{i}", shape, dt) for i in range(n)])


def bcast_rows(ap1d_row, n=128):
    return ap1d_row.partition_broadcast(n)


def build(n_layers, first_layer=0, final_norm=True, debug=False, stop_after=None, branches="ABCD"):
    nc = bass.Bass("TRN2", target_bir_lowering=False)
    es_top = ExitStack()
    C = Ctx()
    C.nc = nc
    L = DEPTH

    def din(name, shape, dt=F32):
        return nc.dram_tensor(name, list(shape), dt, kind="ExternalInput").ap()

    okind = "ExternalOutput" if debug else "Internal"

    def dscr(name, shape, dt=BF16):
        return nc.dram_tensor(name, list(shape), dt, kind=okind).ap()

    I = {}
    I["x"] = din("x", [S, D])
    for nm, shp in (("norm1_g", [L, D]), ("w_in", [L, D, D_IN]), ("diff_lq1", [L, 64]), ("diff_lk1", [L, 64]),
                    ("diff_lq2", [L, 64]), ("diff_lk2", [L, 64]), ("diff_subln_g", [L, 128]),
                    ("nsa_pe_k", [L, 32, 128]), ("nsa_w1_k", [L, 4096, 128]), ("nsa_w2_k", [L, 128, 128]),
                    ("nsa_pe_v", [L, 32, 128]), ("nsa_w1_v", [L, 4096, 128]), ("nsa_w2_v", [L, 128, 128]),
                    ("mla_q_norm_g", [L, 384]), ("mla_w_uq", [L, 384, 768]), ("mla_kv_norm_g", [L, 256]),
                    ("mla_w_ukv", [L, 256, 1024]), ("idx_k_norm_g", [L, 64]), ("w_branch", [L, 4, 512, D]),
                    ("w_out", [L, D, D]), ("norm2_g", [L, D]), ("w_gate_up", [L, D, 2 * DFF]),
                    ("w_down", [L, DFF, D]), ("final_norm_g", [1, D])):
        I[nm] = din(nm, shp)
    I["c_ident"] = din("c_ident", [128, 128], BF16)
    I["c_causal"] = din("c_causal", [128, 128], BF16)
    I["c_anti"] = din("c_anti", [128, 128], BF16)
    I["c_cmask"] = din("c_cmask", [S, 256], BF16)
    I["c_selb"] = din("c_selb", [S, 64], F32)
    I["c_negbig"] = din("c_negbig", [128, 128], F32)
    I["c_posbig"] = din("c_posbig", [128, 128], F32)
    I["c_overlap"] = din("c_overlap", [256, 64], BF16)
    for nm, half in (("da", 8), ("nsa", 16), ("mla", 32), ("dsa", 16), ("idx", 8)):
        I["cos_" + nm] = din("cos_" + nm, [S, half])
        I["sin_" + nm] = din("sin_" + nm, [S, half])
    out = nc.dram_tensor("out", [S, D], F32, kind="ExternalOutput").ap()

    Sx = {}
    Sx["xa"] = dscr("xa", [S, D], F32)
    Sx["xb"] = dscr("xb", [S, D], F32)
    for nm, rows in (("QT_A", 512), ("KT_A", 512), ("QT_B", 512), ("bkT", 384), ("vcT", 128), ("QT_Cn", 512),
                     ("QT_Cr", 256), ("KT_Cn", 512), ("kpeT", 64), ("QT_D", 512), ("KT_D", 512), ("iqT", 512),
                     ("ikT", 64)):
        Sx[nm] = dscr(nm, [rows, S])
    for nm, cols in (("V_A", 512), ("vs", 128), ("vw", 128), ("V_C", 512), ("V_D", 512), ("O_A", 512),
                     ("O_B", 512), ("O_C", 512), ("O_D", 512)):
        Sx[nm] = dscr(nm, [S, cols])
    Sx["gB"] = dscr("gB", [S, 12], F32)
    Sx["iw"] = dscr("iw", [S, 8], F32)
    Sx["kcmpT"] = dscr("kcmpT", [128, 256])
    Sx["vcmp"] = dscr("vcmp", [256, 128])

    es = es_top
    P = Prog(nc, es)
    C.P = P
    _CURP[0] = P
    ident = sb(nc, es, "ident", [128, 128], BF16)
    P.ident = ident
    causal = sb(nc, es, "causal", [128, 128], BF16)
    anti = sb(nc, es, "anti", [128, 128], BF16)
    P.dma(ident[:], I["c_ident"][:, :], W=[ident.b], sb=ident.b)
    P.dma(causal[:], I["c_causal"][:, :], W=[causal.b], sb=causal.b)
    P.dma(anti[:], I["c_anti"][:, :], W=[anti.b], sb=anti.b)
    psf = Pool([Tn(es.enter_context(nc.psum_tensor(f"psf{i}", [128, 512], F32)), f"psf{i}") for i in range(6)])
    psb = Pool([Tn(es.enter_context(nc.psum_tensor(f"psb{i}", [128, 1024], BF16)), f"psb{i}") for i in range(2)])
    C.psf, C.psb = psf, psb
    C.small = sbpool(nc, es, "small", 8, [128, 4], F32)
    C.junk = sbpool(nc, es, "junk", 2, [128, 1024], F32)

    def rms(src_ap, n, R, eps=1e-6):
        ss = C.small.next()
        jk = C.junk.next()
        P.memset("dve", ss[:], 0.0, [ss.b])
        P.act(jk[:, :n], src_ap, AF.Square, R + [ss.b], [jk.b, ss.b], accum=ss[:, 0:1])
        P.ts("dve", ss[:, 2:3], ss[:, 0:1], 1.0 / n, eps, ALU.mult, ALU.add, [ss.b], [ss.b])
        P.act(ss[:, 3:4], ss[:, 2:3], AF.Ln, [ss.b], [ss.b])
        P.act(ss[:, 1:2], ss[:, 3:4], AF.Exp, [ss.b], [ss.b], scale=-0.5)
        return ss

    def load_bcast(es_, name, row_ap, n, q="sp"):
        t = sb(nc, es_, name, [128, n], F32)
        P.dma(t[:], row_ap.partition_broadcast(128), W=[t.b], sb=t.b, q=q)
        return t

    def load_w_bf16(es_, name, dram_ap, kc, ncols, stage_pool, chunk_cols=512):
        wt = sb(nc, es_, name, [128, kc, ncols], BF16)
        src = dram_ap.rearrange("(k p) n -> p k n", p=128)
        for k in range(kc):
            sw = stage_pool.items[0].t.shape[-1]
            for c0 in range(0, ncols, sw):
                w = min(sw, ncols - c0)
                st = stage_pool.next()
                P.dma(st[:, :w], src[:, k, c0:c0 + w], W=[st.b], sb=st.b)
                P.copy("pool", wt[:, k, c0:c0 + w], st[:, :w], [st.b], [wt.b])
        return wt

    for li in range(n_layers):
        l = first_layer + li
        xsrc = I["x"] if li == 0 else Sx["xa"]
        lam_init = 0.8 - 0.6 * math.exp(-0.3 * l)

        with scope() as esA:
            hT = sb(nc, esA, "hT", [128, 8, S], BF16)
            with scope() as es0:
                g1 = load_bcast(es0, "g1", I["norm1_g"][l:l + 1, :], D)
                xs = sbpool(nc, es0, "xs", 2, [128, D], F32)
                hbp = sbpool(nc, es0, "hb", 2, [128, D], BF16)
                for tt in range(NT):
                    xt = xs.next()
                    P.dma(xt[:], xsrc[tt * 128:(tt + 1) * 128, :], W=[xt.b], sb=xt.b)
                    ss = rms(xt[:], D, [xt.b])
                    hb = hbp.next()
                    P.stt(hb[:], xt[:], ss[:, 1:2], g1[:], ALU.mult, ALU.mult, [xt.b, ss.b, g1.b], [hb.b])
                    pb = psb.next()
                    for k in range(8):
                        P.tr(pb[:, k * 128:(k + 1) * 128], hb[:, k * 128:(k + 1) * 128], [hb.b, ident.b], [pb.b])
                    P.copy("dve", hT[:, :, tt * 128:(tt + 1) * 128], pb[:].rearrange("p (k c) -> p k c", k=8),
                           [pb.b], [hT.b])
                P.barrier()
            if stop_after == "A0":
                break
            with scope() as es1:
                wst = sbpool(nc, es1, "wst", 1, [128, 8, 512], F32)
                wbf = sbpool(nc, es1, "wbf", 2, [128, 8, 512], BF16)
                zp = sbpool(nc, es1, "z", 4, [128, 1024], F32)
                zbp = FreePool(sbpool(nc, es1, "zb", 12, [128, 1024], BF16).items)
                ztp = sbpool(nc, es1, "zt", 4, [128, 4, 128], BF16)
                rtmp = sbpool(nc, es1, "rtmp", 3, [128, 4, 128], F32)
                tabs = {}
                for nm, half in (("da", 8), ("nsa", 16), ("mla", 32), ("dsa", 16), ("idx", 8)):
                    ct = sb(nc, es1, "cos_" + nm, [128, NT, half], F32)
                    st = sb(nc, es1, "sin_" + nm, [128, NT, half], F32)
                    P.dma(ct[:], I["cos_" + nm].rearrange("(t p) h -> p t h", p=128), W=[ct.b], sb=ct.b)
                    P.dma(st[:], I["sin_" + nm].rearrange("(t p) h -> p t h", p=128), W=[st.b], sb=st.b)
                    tabs[nm] = (ct, st, half)
                gq = load_bcast(es1, "gq", I["mla_q_norm_g"][l:l + 1, :], 384)
                gkv = load_bcast(es1, "gkv", I["mla_kv_norm_g"][l:l + 1, :], 256)
                gik = load_bcast(es1, "gik", I["idx_k_norm_g"][l:l + 1, :], 64)
                wstage = sbpool(nc, es1, "wstage", 2, [128, 1024], F32)
                wuq = load_w_bf16(es1, "wuq", I["mla_w_uq"][l], 3, 768, wstage)
                wukv = load_w_bf16(es1, "wukv", I["mla_w_ukv"][l], 2, 1024, wstage)
                smallio = sbpool(nc, es1, "smallio", 4, [128, 16], F32)

                def rope(z, col0, G, dh, roff, kind, tt):
                    ct, st, half = tabs[kind]
                    v = z[:, col0:col0 + G * dh].rearrange("p (g d) -> p g d", g=G)
                    x1 = v[:, :, roff:roff + half]
                    x2 = v[:, :, roff + half:roff + 2 * half]
                    cc = ct[:, tt:tt + 1, :].to_broadcast([128, G, half])
                    sn = st[:, tt:tt + 1, :].to_broadcast([128, G, half])
                    tm = rtmp.next()
                    t = [tm[:, i, :G * half].rearrange("p (g h) -> p g h", g=G) for i in range(4)]
                    Rr = [z.b, ct.b, st.b]
                    P.tt("dve", t[0], x1, cc, ALU.mult, Rr, [tm.b])
                    P.tt("dve", t[1], x2, sn, ALU.mult, Rr, [tm.b])
                    P.tt("pool", t[2], x2, cc, ALU.mult, Rr, [tm.b])
                    P.tt("pool", t[3], x1, sn, ALU.mult, Rr, [tm.b])
                    P.tt("dve", x1, t[0], t[1], ALU.subtract, [tm.b], [z.b])
                    P.tt("dve", x2, t[2], t[3], ALU.add, [tm.b], [z.b])

                def store_T(zb, col0, ncols, dst, row0, tt):
                    pb = psb.next()
                    nj = (ncols + 127) // 128
                    for j in range(nj):
                        w = min(128, ncols - j * 128)
                        P.tr(pb[:w, j * 128:(j + 1) * 128], zb[:, col0 + j * 128:col0 + j * 128 + w],
                             [zb.b, ident.b], [pb.b])
                    zt = ztp.next()
                    if ncols >= 128:
                        P.copy("dve", zt[:, :nj, :], pb[:, :nj * 128].rearrange("p (j c) -> p j c", j=nj),
                               [pb.b], [zt.b])
                        P.dma(dst[row0:row0 + ncols, tt * 128:(tt + 1) * 128].rearrange("(j p) c -> p j c", p=128),
                              zt[:, :nj, :], R=[zt.b], sb=zt.b)
                    else:
                        P.copy("dve", zt[:ncols, 0, :], pb[:ncols, 0:128], [pb.b], [zt.b])
                        P.dma(dst[row0:row0 + ncols, tt * 128:(tt + 1) * 128], zt[:ncols, 0, :], R=[zt.b], sb=zt.b)

                def store_tok(zb, col0, ncols, dst, tt):
                    P.dma(dst[tt * 128:(tt + 1) * 128, :], zb[:, col0:col0 + ncols], R=[zb.b], sb=zb.b)

                def tobf(z, zb, c0, n):
                    P.copy("dve", zb[:, c0:c0 + n], z[:, c0:c0 + n], [z.b], [zb.b])

                def h_rope_T(G, dh, kind, dst):
                    def h(tt, z, n):
                        rope(z, 0, G, dh, 0, kind, tt)
                        zb = zbp.alloc()
                        tobf(z, zb, 0, n)
                        yield
                        store_T(zb, 0, n, Sx[dst], 0, tt)
                        zbp.free(zb)
                    return h

                def h_tok(dst):
                    def h(tt, z, n):
                        zb = zbp.alloc()
                        tobf(z, zb, 0, n)
                        store_tok(zb, 0, n, Sx[dst], tt)
                        zbp.free(zb)
                        return
                        yield
                    return h

                def h_bk(tt, z, n):
                    rope(z, 0, 3, 128, 0, "nsa", tt)
                    zb = zbp.alloc()
                    tobf(z, zb, 0, 384)
                    yield
                    store_T(zb, 0, 384, Sx["bkT"], 0, tt)
                    zbp.free(zb)

                def h_bv(tt, z, n):
                    zb = zbp.alloc()
                    tobf(z, zb, 0, 384)
                    yield
                    store_T(zb, 0, 128, Sx["vcT"], 0, tt)
                    P.dma(Sx["vs"][tt * 128:(tt + 1) * 128, :], zb[:, 128:256], R=[zb.b], sb=zb.b)
                    P.dma(Sx["vw"][tt * 128:(tt + 1) * 128, :], zb[:, 256:384], R=[zb.b], sb=zb.b)
                    zbp.free(zb)

                def norm_proj(z, c0, n, gt, wt, ncout, tt, res):
                    ss = rms(z[:, c0:c0 + n], n, [z.b])
                    zb = zbp.alloc()
                    P.stt(zb[:, :n], z[:, c0:c0 + n], ss[:, 1:2], gt[:], ALU.mult, ALU.mult, [z.b, ss.b, gt.b], [zb.b])
                    kc = n // 128
                    yield
                    pb = psb.next()
                    for k in range(kc):
                        P.tr(pb[:, k * 128:(k + 1) * 128], zb[:, k * 128:(k + 1) * 128], [zb.b, ident.b], [pb.b])
                    zbp.free(zb)
                    zt = ztp.next()
                    P.copy("dve", zt[:, :kc, :], pb[:, :kc * 128].rearrange("p (j c) -> p j c", j=kc), [pb.b], [zt.b])
                    z2 = zp.next()
                    for c in range(0, ncout, 512):
                        w = min(512, ncout - c)
                        ps = psf.next()
                        for k in range(kc):
                            P.mm(ps[:, :w], zt[:, k, :], wt[:, k, c:c + w], k == 0, k == kc - 1, [zt.b, wt.b], [ps.b])
                        P.copy("act", z2[:, c:c + w], ps[:, :w], [ps.b], [z2.b])
                    res.append(z2)

                def h_cq(tt, z, n):
                    sg = smallio.next()
                    P.act(sg[:, :12], z[:, 384:396], AF.Sigmoid, [z.b], [sg.b])
                    P.dma(Sx["gB"][tt * 128:(tt + 1) * 128, :], sg[:, :12], R=[sg.b], sb=sg.b)
                    res = []
                    yield from norm_proj(z, 0, 384, gq, wuq, 768, tt, res)
                    q = res[0]
                    rope(q, 0, 4, 192, 128, "mla", tt)
                    qb = zbp.alloc()
                    q3 = q[:, :768].rearrange("p (g d) -> p g d", g=4)
                    P.copy("pool", qb[:, 0:512].rearrange("p (g d) -> p g d", g=4), q3[:, :, 0:128], [q.b], [qb.b])
                    P.copy("pool", qb[:, 512:768].rearrange("p (g d) -> p g d", g=4), q3[:, :, 128:192], [q.b], [qb.b])
                    yield
                    store_T(qb, 0, 512, Sx["QT_Cn"], 0, tt)
                    store_T(qb, 512, 256, Sx["QT_Cr"], 0, tt)
                    zbp.free(qb)

                def h_ckv(tt, z, n):
                    rope(z, 256, 1, 64, 0, "mla", tt)
                    zb = zbp.alloc()
                    tobf(z, zb, 256, 64)
                    res = []
                    g_ = norm_proj(z, 0, 256, gkv, wukv, 1024, tt, res)
                    next(g_)
                    yield
                    store_T(zb, 256, 64, Sx["kpeT"], 0, tt)
                    zbp.free(zb)
                    for _ in g_:
                        pass
                    kv = res[0]
                    kb = zbp.alloc()
                    kv3 = kv[:, :1024].rearrange("p (g d) -> p g d", g=4)
                    P.copy("pool", kb[:, 0:512].rearrange("p (g d) -> p g d", g=4), kv3[:, :, 0:128], [kv.b], [kb.b])
                    P.copy("pool", kb[:, 512:1024].rearrange("p (g d) -> p g d", g=4), kv3[:, :, 128:256], [kv.b],
                           [kb.b])
                    yield
                    store_T(kb, 0, 512, Sx["KT_Cn"], 0, tt)
                    store_tok(kb, 512, 512, Sx["V_C"], tt)
                    zbp.free(kb)

                def h_ik(tt, z, n):
                    sg = smallio.next()
                    P.ts("dve", sg[:, :8], z[:, 64:72], (8 ** -0.5) * (64 ** -0.5), None, ALU.mult, None, [z.b], [sg.b])
                    P.dma(Sx["iw"][tt * 128:(tt + 1) * 128, :], sg[:, :8], R=[sg.b], sb=sg.b)
                    ss = rms(z[:, 0:64], 64, [z.b])
                    P.stt(z[:, 0:64], z[:, 0:64], ss[:, 1:2], gik[:], ALU.mult, ALU.mult, [z.b, ss.b, gik.b], [z.b])
                    rope(z, 0, 1, 64, 0, "idx", tt)
                    zb = zbp.alloc()
                    tobf(z, zb, 0, 64)
                    yield
                    store_T(zb, 0, 64, Sx["ikT"], 0, tt)
                    zbp.free(zb)

                chunks = [
                    ([("a_q", 512)], h_rope_T(8, 64, "da", "QT_A")),
                    ([("a_k", 512)], h_rope_T(8, 64, "da", "KT_A")),
                    ([("a_v", 512)], h_tok("V_A")),
                    ([("b_q", 512)], h_rope_T(4, 128, "nsa", "QT_B")),
                    ([("b_kc", 128), ("b_ks", 128), ("b_kw", 128)], h_bk),
                    ([("b_vc", 128), ("b_vs", 128), ("b_vw", 128)], h_bv),
                    ([("c_q", 384), ("b_g", 12)], h_cq),
                    ([("c_kv", 256), ("c_kr", 64)], h_ckv),
                    ([("d_q", 512)], h_rope_T(4, 128, "dsa", "QT_D")),
                    ([("d_k", 512)], h_rope_T(4, 128, "dsa", "KT_D")),
                    ([("d_v", 512)], h_tok("V_D")),
                    ([("d_iq", 512)], h_rope_T(8, 64, "idx", "iqT")),
                    ([("d_ik", 64), ("d_iw", 8)], h_ik),
                ]
                if stop_after == "A1a":
                    chunks = chunks[:3]
                wsrc = I["w_in"][l].rearrange("(k p) n -> p k n", p=128)

                def load_chunk(ci):
                    segs, _ = chunks[ci]
                    st = wst.next()
                    wb = wbf.next()
                    c = 0
                    for nm, n in segs:
                        P.dma(st[:, :, c:c + n], wsrc[:, :, OFF[nm]:OFF[nm] + n], W=[st.b], sb=st.b)
                        c += n
                    P.copy("pool", wb[:, :, :c], st[:, :, :c], [st.b], [wb.b])
                    return wb, c

                nxt = load_chunk(0)
                active = []

                def advance(flush=False):
                    while True:
                        keep = []
                        for it in active:
                            if it[1] > 0 and not flush:
                                it[1] -= 1
                                keep.append(it)
                                continue
                            try:
                                next(it[0])
                                it[1] = 1
                                keep.append(it)
                            except StopIteration:
                                pass
                        active[:] = keep
                        if not flush or not active:
                            break

                for ci in range(len(chunks)):
                    wb, n = nxt
                    if ci + 1 < len(chunks):
                        nxt = load_chunk(ci + 1)
                    handler = chunks[ci][1]
                    for tt in range(NT):
                        ps = psf.next()
                        for k in range(8):
                            P.mm(ps[:, :n], hT[:, k, tt * 128:(tt + 1) * 128], wb[:, k, :n], k == 0, k == 7,
                                 [hT.b, wb.b], [ps.b])
                        z = zp.next()
                        P.copy("act", z[:, :n], ps[:, :n], [ps.b], [z.b])
                        g_ = handler(tt, z, n)
                        try:
                            next(g_)
                            active.append([g_, 1])
                        except StopIteration:
                            pass
                        advance()
                advance(flush=True)
                P.barrier()
        if stop_after in ("A0", "A1a", "A1"):
            break
        with scope() as es2:
            tokp = sbpool(nc, es2, "ctok", 2, [128, S], BF16)
            w1st = sb(nc, es2, "w1st", [128, 32, 128], F32)
            w1bp = sbpool(nc, es2, "w1b", 2, [128, 32, 128], BF16)
            peTp = sbpool(nc, es2, "peT", 2, [128, 32], F32)
            w2stp = sbpool(nc, es2, "w2st", 2, [128, 128], F32)
            w2bp = sbpool(nc, es2, "w2b", 2, [128, 128], BF16)
            ctmp = sbpool(nc, es2, "ctmp", 3, [128, 256], BF16)
            gxp = sbpool(nc, es2, "gx", 2, [128, 256], F32)
            gx2p = sbpool(nc, es2, "gx2", 2, [128, 256], F32)
            gTp = sbpool(nc, es2, "gT", 2, [128, 256], BF16)
            cout = sbpool(nc, es2, "cout", 2, [128, 256], BF16)
            for kind in ("k", "v"):
                src = Sx["bkT"][0:128, :] if kind == "k" else Sx["vcT"][:, :]
                tk = tokp.next()
                P.dma(tk[:], src, W=[tk.b], sb=tk.b)
                P.dma(w1st[:], I["nsa_w1_" + kind][l].rearrange("(l d) o -> d l o", d=128), W=[w1st.b], sb=w1st.b)
                wb = w1bp.next()
                P.copy("pool", wb[:], w1st[:], [w1st.b], [wb.b])
                pt = peTp.next()
                P.dma(pt[:], I["nsa_pe_" + kind][l].rearrange("l d -> d l"), W=[pt.b], sb=pt.b, slow=True)
                w2s = w2stp.next()
                P.dma(w2s[:], I["nsa_w2_" + kind][l], W=[w2s.b], sb=w2s.b)
                w2 = w2bp.next()
                P.copy("pool", w2[:], w2s[:], [w2s.b], [w2.b])
                ps = psf.next()
                tk3 = tk[:].rearrange("p (b s) -> p b s", s=16)
                for lp in range(32):
                    tm = ctmp.next()
                    P.ts("dve", tm[:, :255], tk3[:, lp // 16:lp // 16 + 255, lp % 16], pt[:, lp:lp + 1], None,
                         ALU.add, None, [tk.b, pt.b], [tm.b])
                    P.mm(ps[:, :255], wb[:, lp, :], tm[:, :255], lp == 0, lp == 31, [wb.b, tm.b], [ps.b])
                gx = gxp.next()
                gx2 = gx2p.next()
                P.copy("act", gx[:, :255], ps[:, :255], [ps.b], [gx.b])
                P.tt("dve", gx2[:, :255], gx[:, :255], gx[:, :255], ALU.mult, [gx.b], [gx2.b])
                P.ts("dve", gx2[:, :255], gx2[:, :255], 0.044715, 1.0, ALU.mult, ALU.add, [gx2.b], [gx2.b])
                P.tt("dve", gx2[:, :255], gx2[:, :255], gx[:, :255], ALU.mult, [gx2.b, gx.b], [gx2.b])
                P.act(gx2[:, :255], gx2[:, :255], AF.Sigmoid, [gx2.b], [gx2.b], scale=2.0 * math.sqrt(2.0 / math.pi))
                gT = gTp.next()
                P.memset("dve", gT[:], 0.0, [gT.b])
                P.tt("dve", gT[:, :255], gx[:, :255], gx2[:, :255], ALU.mult, [gx.b, gx2.b], [gT.b])
                co = cout.next()
                if kind == "k":
                    ps2 = psf.next()
                    P.mm(ps2[:, :256], w2[:], gT[:, :256], True, True, [w2.b, gT.b], [ps2.b])
                    P.copy("dve", co[:], ps2[:, :256], [ps2.b], [co.b])
                    P.dma(Sx["kcmpT"][:, :], co[:], R=[co.b], sb=co.b)
                else:
                    for g in range(2):
                        ps2 = psf.next()
                        P.mm(ps2[:, :128], gT[:, g * 128:(g + 1) * 128], w2[:], True, True, [w2.b, gT.b], [ps2.b])
                        P.copy("dve", co[:, g * 128:(g + 1) * 128], ps2[:, :128], [ps2.b], [co.b])
                    P.dma(Sx["vcmp"].rearrange("(g p) d -> p g d", p=128), co[:].rearrange("p (g d) -> p g d", g=2),
                          R=[co.b], sb=co.b)
            P.barrier()
        if stop_after == "A2":
            break

        psc = Pool(psf.items[0:2])
        accp = Pool(psf.items[2:6])

        class Job:
            def __init__(self, scores, exp, pv, post=None, pre=None):
                self.scores, self.exp, self.pv, self.post, self.pre = scores, exp, pv, post, pre

        def run_jobs(jobs):
            prev = None
            for j in jobs:
                if j.pre is not None:
                    if prev is not None:
                        prev.pv()
                        if prev.post is not None:
                            prev.post()
                        prev = None
                    j.pre()
                j.scores()
                j.exp()
                if prev is not None:
                    prev.pv()
                    if prev.post is not None:
                        prev.post()
                prev = j
            if prev is not None:
                prev.pv()
                if prev.post is not None:
                    prev.post()

        def head_jobs(ptp, score_fn, mask_fn, v_fn, acc, nv, kts, scale, post=None, pre=None):
            kts = list(kts)
            chs = [kts[i:i + 4] for i in range(0, len(kts), 4)]
            for ci, ch in enumerate(chs):
                st = {}

                def scores(ch=ch, st=st):
                    sbk = psc.next()
                    st["sb"] = sbk
                    for j, kt in enumerate(ch):
                        terms = list(score_fn(kt))
                        mks = mask_fn(kt)
                        terms.extend(mks)
                        for i, (lt, rh, bufs) in enumerate(terms):
                            P.mm(sbk[:, j * 128:(j + 1) * 128], lt, rh, i == 0, i == len(terms) - 1, bufs, [sbk.b])

                def exp(ch=ch, st=st):
                    pt = ptp.next()
                    st["pt"] = pt
                    n = len(ch) * 128
                    P.act(pt[:, :n], st["sb"][:, :n], AF.Exp, [st["sb"].b], [pt.b], scale=scale)

                def pv(ch=ch, st=st, ci=ci):
                    for j, kt in enumerate(ch):
                        va, vb = v_fn(kt)
                        P.mm(acc[:, :nv], st["pt"][:, j * 128:(j + 1) * 128], va, ci == 0 and j == 0,
                             ci == len(chs) - 1 and j == len(ch) - 1, [st["pt"].b] + vb, [acc.b])

                yield Job(scores, exp, pv, post if ci == len(chs) - 1 else None, pre if ci == 0 else None)

        def recip_sum(acc, col, dst_ap, dst_buf):
            P.ts("dve", dst_ap, acc[:, col:col + 1], TINY, None, ALU.add, None, [acc.b], [dst_buf])
            P.op("dve", lambda e: e.reciprocal(out=dst_ap, in_=dst_ap), [dst_buf], [dst_buf])

        def load_vaug(V, src, c0, nvv=129):
            P.dma(V[:, :, 0:128], src[:, c0:c0 + 128].rearrange("(t p) c -> p t c", p=128), W=[V.b], sb=V.b)
            P.memset("pool", V[:, :, 128:129], 1.0, [V.b])

        if "A" in branches:
          with scope() as esb:
            ptp = sbpool(nc, esb, "pt", 3, [128, 512], BF16)
            Kp = sbpool(nc, esb, "K", 2, [128, S], BF16)
            Qp = sbpool(nc, esb, "Qpad", 2, [128, 2, S], BF16)
            for q_ in Qp.items:
                P.memset("pool", q_[64:128, 0, :], 0.0, [q_.b])
                P.memset("pool", q_[0:64, 1, :], 0.0, [q_.b])
            Vp = sbpool(nc, esb, "V", 2, [128, NT, 129], BF16)
            of = sbpool(nc, esb, "of", 3, [128, 128], F32)
            obp = sbpool(nc, esb, "ob", 3, [128, 128], BF16)
            lqs = [load_bcast(esb, nm, I[nm][l:l + 1, :], 64) for nm in ("diff_lq1", "diff_lk1", "diff_lq2", "diff_lk2")]
            lam = sb(nc, esb, "lam", [128, 8], F32)
            ltmp = sb(nc, esb, "ltmp", [128, 64], F32)
            for i in range(2):
                P.tt("dve", ltmp[:], lqs[2 * i][:], lqs[2 * i + 1][:], ALU.mult, [lqs[2 * i].b, lqs[2 * i + 1].b], [ltmp.b])
                P.red(lam[:, i:i + 1], ltmp[:], ALU.add, [ltmp.b], [lam.b])
            P.act(lam[:, 2:4], lam[:, 0:2], AF.Exp, [lam.b], [lam.b])
            P.tt("dve", lam[:, 4:5], lam[:, 3:4], lam[:, 2:3], ALU.subtract, [lam.b], [lam.b])
            P.ts("dve", lam[:, 5:6], lam[:, 4:5], -lam_init, None, ALU.add, None, [lam.b], [lam.b])
            gsub = load_bcast(esb, "gsub", I["diff_subln_g"][l:l + 1, :], 128)
            P.ts("dve", gsub[:], gsub[:], 1.0 - lam_init, None, ALU.mult, None, [gsub.b], [gsub.b])

            def jobsA():
                for h in range(4):
                    K, Q, V = Kp.next(), Qp.next(), Vp.next()
                    P.dma(K[:], Sx["KT_A"][h * 128:(h + 1) * 128, :], W=[K.b], sb=K.b)
                    P.dma(Q[0:64, 0, :], Sx["QT_A"][h * 128:h * 128 + 64, :], W=[Q.b], sb=Q.b)
                    P.dma(Q[64:128, 1, :], Sx["QT_A"][h * 128 + 64:(h + 1) * 128, :], W=[Q.b], sb=Q.b)
                    load_vaug(V, Sx["V_A"], h * 128)
                    for qt in range(NT):
                        accs = [accp.next(), accp.next()]

                        def post(h=h, qt=qt, accs=accs):
                            r = C.small.next()
                            recip_sum(accs[0], 128, r[:, 0:1], r.b)
                            recip_sum(accs[1], 128, r[:, 1:2], r.b)
                            P.tt("dve", r[:, 1:2], r[:, 1:2], lam[:, 5:6], ALU.mult, [r.b, lam.b], [r.b])
                            o = of.next()
                            P.ts("dve", o[:], accs[0][:, 0:128], r[:, 0:1], None, ALU.mult, None, [accs[0].b, r.b], [o.b])
                            P.stt(o[:], accs[1][:, 0:128], r[:, 1:2], o[:], ALU.mult, ALU.add, [accs[1].b, r.b, o.b], [o.b])
                            ss = rms(o[:], 128, [o.b])
                            ob = obp.next()
                            P.stt(ob[:], o[:], ss[:, 1:2], gsub[:], ALU.mult, ALU.mult, [o.b, ss.b, gsub.b], [ob.b])
                            P.dma(Sx["O_A"][qt * 128:(qt + 1) * 128, h * 128:(h + 1) * 128], ob[:], R=[ob.b], sb=ob.b)

                        for m in range(2):
                            def score_fn(kt, m=m, K=K, Q=Q, qt=qt):
                                return [(K[:, kt * 128:(kt + 1) * 128],
                                         Q[:, m, qt * 128:(qt + 1) * 128], [K.b, Q.b])]

                            def mask_fn(kt, qt=qt):
                                return [(causal[:], ident[:], [causal.b, ident.b])] if kt == qt else []

                            def v_fn(kt, V=V):
                                return V[:, kt, :], [V.b]

                            yield from head_jobs(ptp, score_fn, mask_fn, v_fn, accs[m], 129, range(qt + 1),
                                                 64 ** -0.5, post if m == 1 else None)
            run_jobs(jobsA())
            P.barrier()
        if stop_after == "BA":
            break

        if "C" in branches:
          with scope() as esb:
            ptp = sbpool(nc, esb, "pt", 3, [128, 512], BF16)
            Kp = sbpool(nc, esb, "K", 2, [128, S], BF16)
            Qp = sbpool(nc, esb, "Q", 2, [128, S], BF16)
            Qrp = sbpool(nc, esb, "Qr", 2, [128, S], BF16)
            for q_ in Qrp.items:
                P.memset("pool", q_[64:128, :], 0.0, [q_.b])
            Vp = sbpool(nc, esb, "V", 2, [128, NT, 129], BF16)
            kpe = sb(nc, esb, "kpe", [128, S], BF16)
            P.memset("pool", kpe[64:128, :], 0.0, [kpe.b])
            obp = sbpool(nc, esb, "ob", 3, [128, 128], BF16)
            P.dma(kpe[0:64, :], Sx["kpeT"][:, :], W=[kpe.b], sb=kpe.b)

            def jobsC():
                for h in range(4):
                    K, Q, Qr, V = Kp.next(), Qp.next(), Qrp.next(), Vp.next()
                    P.dma(K[:], Sx["KT_Cn"][h * 128:(h + 1) * 128, :], W=[K.b], sb=K.b)
                    P.dma(Q[:], Sx["QT_Cn"][h * 128:(h + 1) * 128, :], W=[Q.b], sb=Q.b)
                    P.dma(Qr[0:64, :], Sx["QT_Cr"][h * 64:(h + 1) * 64, :], W=[Qr.b], sb=Qr.b)
                    load_vaug(V, Sx["V_C"], h * 128)
                    for qt in range(NT):
                        acc = accp.next()

                        def post(h=h, qt=qt, acc=acc):
                            r = C.small.next()
                            recip_sum(acc, 128, r[:, 0:1], r.b)
                            ob = obp.next()
                            P.ts("dve", ob[:], acc[:, 0:128], r[:, 0:1], None, ALU.mult, None, [acc.b, r.b], [ob.b])
                            P.dma(Sx["O_C"][qt * 128:(qt + 1) * 128, h * 128:(h + 1) * 128], ob[:], R=[ob.b], sb=ob.b)

                        def score_fn(kt, K=K, Q=Q, Qr=Qr, qt=qt):
                            return [(K[:, kt * 128:(kt + 1) * 128], Q[:, qt * 128:(qt + 1) * 128], [K.b, Q.b]),
                                    (kpe[:, kt * 128:(kt + 1) * 128], Qr[:, qt * 128:(qt + 1) * 128], [kpe.b, Qr.b])]

                        def mask_fn(kt, qt=qt):
                            return [(causal[:], ident[:], [causal.b, ident.b])] if kt == qt else []

                        def v_fn(kt, V=V):
                            return V[:, kt, :], [V.b]

                        yield from head_jobs(ptp, score_fn, mask_fn, v_fn, acc, 129, range(qt + 1), 192 ** -0.5, post)
            run_jobs(jobsC())
            P.barrier()
        if stop_after == "BC":
            break

        if "D" in branches:
          with scope() as esb:
            ptp = sbpool(nc, esb, "pt", 3, [128, 512], BF16)
            iqp = sbpool(nc, esb, "iqt", 4, [128, 8, 128], BF16)
            for q_ in iqp.items:
                v_ = q_[:].rearrange("p (g e) c -> p g e c", e=2)
                P.memset("pool", v_[64:128, :, 0, :], 0.0, [q_.b])
                P.memset("pool", v_[0:64, :, 1, :], 0.0, [q_.b])
            ik2 = sb(nc, esb, "ik2", [128, S], BF16)
            qdp = sbpool(nc, esb, "qd", 3, [128, 4, 128], BF16)
            Kd = [sb(nc, esb, f"Kd{i}", [128, S], BF16) for i in range(4)]
            Vd = [sb(nc, esb, f"Vd{i}", [128, NT, 129], BF16) for i in range(4)]
            iwt = sb(nc, esb, "iwt", [128, NT, 8], F32)
            idxp = sbpool(nc, esb, "idx", 4, [128, S], F32)
            Mkp = sbpool(nc, esb, "Mk", 4, [128, S], BF16)
            rp = sbpool(nc, esb, "relu", 2, [128, 512], F32)
            bjunk = sb(nc, esb, "bjunk", [128, S], BF16)
            negbig = sb(nc, esb, "negbig", [128, 128], F32)
            posbig = sb(nc, esb, "posbig", [128, 128], F32)
            dtmpp = sbpool(nc, esb, "dtmp", 2, [128, 128], F32)
            bs = sbpool(nc, esb, "bs", 6, [128, 32], F32)
            obp = sbpool(nc, esb, "ob", 3, [128, 128], BF16)
            P.dma(negbig[:], I["c_negbig"][:, :], W=[negbig.b], sb=negbig.b)
            P.dma(posbig[:], I["c_posbig"][:, :], W=[posbig.b], sb=posbig.b)
            P.dma(iwt[:], Sx["iw"].rearrange("(t p) h -> p t h", p=128), W=[iwt.b], sb=iwt.b)
            P.dma(ik2[0:64, :], Sx["ikT"][:, :], W=[ik2.b], sb=ik2.b)
            P.dma(ik2[64:128, :], Sx["ikT"][:, :], W=[ik2.b], sb=ik2.b)
            for i in range(4):
                P.dma(Kd[i][:], Sx["KT_D"][i * 128:(i + 1) * 128, :], W=[Kd[i].b], sb=Kd[i].b)
                load_vaug(Vd[i], Sx["V_D"], i * 128)
            NBIS = 16

            def idx_accum(qt, out):
                Lq = (qt + 1) * 128
                idx = idxp.next()
                iq = iqp.next()
                iqv = iq[:].rearrange("p (g e) c -> p g e c", e=2)
                srcv = Sx["iqT"][:, qt * 128:(qt + 1) * 128].rearrange("(g e p) c -> e p g c", e=2, p=64)
                P.dma(iqv[0:64, :, 0, :], srcv[0], W=[iq.b], sb=iq.b)
                P.dma(iqv[64:128, :, 1, :], srcv[1], W=[iq.b], sb=iq.b)
                for c0 in range(0, Lq, 512):
                    w = min(512, Lq - c0)
                    for h in range(8):
                        pbk = psb.next()
                        psv = pbk[:].bitcast(F32)
                        p0 = (h % 2) * 64
                        P.mm(psv[:, :w], iq[:, h, :], ik2[:, c0:c0 + w],
                             True, True, [iq.b, ik2.b], [pbk.b])
                        r = rp.next()
                        P.act(r[:, :w], psv[:, :w], AF.Relu, [pbk.b], [r.b])
                        if h == 0:
                            P.ts("dve", idx[:, c0:c0 + w], r[:, :w], iwt[:, qt, 0:1], None, ALU.mult, None,
                                 [r.b, iwt.b], [idx.b])
                        else:
                            P.stt(idx[:, c0:c0 + w], r[:, :w], iwt[:, qt, h:h + 1], idx[:, c0:c0 + w], ALU.mult, ALU.add,
                                  [r.b, iwt.b, idx.b], [idx.b])
                    yield
                b = bs.next()
                d0 = qt * 128
                if qt >= 2:
                    dtmp = dtmpp.next()
                    P.tt("dve", dtmp[:], idx[:, d0:Lq], posbig[:], ALU.add, [idx.b, posbig.b], [dtmp.b])
                    P.red(b[:, 0:1], dtmp[:], ALU.min, [dtmp.b], [b.b])
                    P.red(b[:, 1:2], idx[:, 0:d0], ALU.min, [idx.b], [b.b])
                    P.tt("dve", b[:, 0:1], b[:, 0:1], b[:, 1:2], ALU.min, [b.b], [b.b])
                P.tt("dve", idx[:, d0:Lq], idx[:, d0:Lq], negbig[:], ALU.add, [idx.b, negbig.b], [idx.b])
                if qt >= 2:
                    P.red(b[:, 1:2], idx[:, 0:Lq], ALU.max, [idx.b], [b.b])
                    P.tt("dve", b[:, 2:3], b[:, 1:2], b[:, 0:1], ALU.subtract, [b.b], [b.b])
                    P.memset("dve", b[:, 8:8 + NBIS], 0.0, [b.b])
                else:
                    P.memset("dve", b[:, 0:1], -1e29, [b.b])
                out.append(dict(qt=qt, Lq=Lq, idx=idx, b=b))

            def bisect(states):
                sts = [x for x in states if x["qt"] >= 2]
                for it in range(NBIS):
                    f = 0.5 ** (it + 1)
                    for x in sts:
                        b = x["b"]
                        P.stt(b[:, 3:4], b[:, 2:3], -f, b[:, 0:1], ALU.mult, ALU.subtract, [b.b], [b.b])
                    for x in sts:
                        b, idx, Lq = x["b"], x["idx"], x["Lq"]
                        P.act(bjunk[:, :Lq], idx[:, :Lq], AF.Sign, [idx.b, b.b], [bjunk.b, b.b], bias=b[:, 3:4],
                              accum=b[:, 8 + it:9 + it])
                    for x in sts:
                        b, Lq = x["b"], x["Lq"]
                        P.ts("dve", b[:, 5:6], b[:, 8 + it:9 + it], 510.5 - Lq, f, ALU.is_ge, ALU.mult, [b.b], [b.b])
                        P.stt(b[:, 0:1], b[:, 5:6], b[:, 2:3], b[:, 0:1], ALU.mult, ALU.add, [b.b], [b.b])
                    yield

            def run_rr(gens):
                gens = list(gens)
                while gens:
                    nxt = []
                    for g_ in gens:
                        try:
                            next(g_)
                            nxt.append(g_)
                        except StopIteration:
                            pass
                    gens = nxt

            def accum_pair(p):
                out = []
                accd[p] = out
                for qt in (2 * p, 2 * p + 1):
                    yield from idx_accum(qt, out)

            def mk_pair(p):
                res = {}
                for x in accd.pop(p):
                    Mk = Mkp.next()
                    P.ts("pool", Mk[:, :x["Lq"]], x["idx"][:, :x["Lq"]], x["b"][:, 0:1], NEG, ALU.is_lt, ALU.mult,
                         [x["idx"].b, x["b"].b], [Mk.b])
                    res[x["qt"]] = Mk
                return res

            accd = {}
            NP = NT // 2

            def jobsD():
                run_rr([accum_pair(0)])
                run_rr([bisect(accd[0])])
                mks = dict(mk_pair(0))
                run_rr([accum_pair(1)])
                for qt in range(NT):
                    st = {"Mk": mks[qt]}
                    qd = qdp.next()
                    P.dma(qd[:], Sx["QT_D"][:, qt * 128:(qt + 1) * 128].rearrange("(h p) c -> p h c", p=128),
                          W=[qd.b], sb=qd.b)

                    def pre(qt=qt, mks=mks):
                        p1 = qt // 2 + 1
                        if p1 < NP:
                            gens = [bisect(accd[p1])]
                            if p1 + 1 < NP:
                                gens.append(accum_pair(p1 + 1))
                            run_rr(gens)
                            mks.update(mk_pair(p1))

                    for h in range(4):
                        acc = accp.next()

                        def post(h=h, qt=qt, acc=acc):
                            r = C.small.next()
                            recip_sum(acc, 128, r[:, 0:1], r.b)
                            ob = obp.next()
                            P.ts("dve", ob[:], acc[:, 0:128], r[:, 0:1], None, ALU.mult, None, [acc.b, r.b], [ob.b])
                            P.dma(Sx["O_D"][qt * 128:(qt + 1) * 128, h * 128:(h + 1) * 128], ob[:], R=[ob.b], sb=ob.b)

                        def score_fn(kt, h=h, qd=qd):
                            return [(Kd[h][:, kt * 128:(kt + 1) * 128], qd[:, h, :], [Kd[h].b, qd.b])]

                        def mask_fn(kt, st=st):
                            Mk = st["Mk"]
                            return [(Mk[:, kt * 128:(kt + 1) * 128], ident[:], [Mk.b, ident.b])]

                        def v_fn(kt, h=h):
                            return Vd[h][:, kt, :], [Vd[h].b]

                        yield from head_jobs(ptp, score_fn, mask_fn, v_fn, acc, 129, range(qt + 1), 128 ** -0.5, post,
                                             pre if (h == 0 and qt % 2 == 0) else None)
            run_jobs(jobsD())
            P.barrier()
        if stop_after == "BD":
            break

        if "B" in branches:
          with scope() as esb:
            ptp = sbpool(nc, esb, "pt", 3, [128, 512], BF16)
            Qb = [sb(nc, esb, f"Qb{i}", [128, S], BF16) for i in range(4)]
            ks = sb(nc, esb, "ks", [128, S], BF16)
            kw = sb(nc, esb, "kw", [128, S], BF16)
            vsa = sb(nc, esb, "vsa", [128, NT, 129], BF16)
            vwa = sb(nc, esb, "vwa", [128, NT, 129], BF16)
            kcm = sb(nc, esb, "kcm", [128, 256], BF16)
            vca = sb(nc, esb, "vca", [128, 2, 193], BF16)
            gBt = sb(nc, esb, "gBt", [128, NT, 12], F32)
            selb = sb(nc, esb, "selb", [128, NT, 64], F32)
            cmp_ = sbpool(nc, esb, "cm", 2, [128, 256], BF16)
            Mkp = sbpool(nc, esb, "MkB", 2, [128, S], BF16)
            obf = sbpool(nc, esb, "obf", 2, [128, 4, 128], F32)
            impp = sbpool(nc, esb, "imp", 2, [128, 64], F32)
            scp = sbpool(nc, esb, "sc", 2, [128, 64], F32)
            cmpm = sb(nc, esb, "cmpm", [128, 64, 64], F32)
            rank = sbpool(nc, esb, "rank", 2, [128, 64], F32)
            obp = sbpool(nc, esb, "ob", 3, [128, 128], BF16)
            coefp = sbpool(nc, esb, "coef", 8, [128, 2], F32)
            for i in range(4):
                P.dma(Qb[i][:], Sx["QT_B"][i * 128:(i + 1) * 128, :], W=[Qb[i].b], sb=Qb[i].b)
            P.dma(ks[:], Sx["bkT"][128:256, :], W=[ks.b], sb=ks.b)
            P.dma(kw[:], Sx["bkT"][256:384, :], W=[kw.b], sb=kw.b)
            load_vaug(vsa, Sx["vs"], 0)
            load_vaug(vwa, Sx["vw"], 0)
            P.dma(kcm[:], Sx["kcmpT"][:, :], W=[kcm.b], sb=kcm.b)
            P.dma(vca[:, :, 0:128], Sx["vcmp"].rearrange("(g p) d -> p g d", p=128), W=[vca.b], sb=vca.b)
            P.memset("pool", vca[:, :, 128:129], 1.0, [vca.b])
            P.dma(vca[:, :, 129:193], I["c_overlap"].rearrange("(g p) j -> p g j", p=128), W=[vca.b], sb=vca.b)
            P.dma(gBt[:], Sx["gB"].rearrange("(t p) g -> p t g", p=128), W=[gBt.b], sb=gBt.b)
            P.dma(selb[:], I["c_selb"].rearrange("(t p) j -> p t j", p=128), W=[selb.b], sb=selb.b)

            def jobsB():
                QS = {}

                def setup(qt):
                    Lq = (qt + 1) * 128
                    cm = cmp_.next()
                    P.dma(cm[:], I["c_cmask"][qt * 128:(qt + 1) * 128, :], W=[cm.b], sb=cm.b)
                    of4 = obf.next()
                    imp = impp.next()
                    st = {}

                    def coef(acc, col, gcol, qt=qt):
                        cf = coefp.next()
                        recip_sum(acc, col, cf[:, 0:1], cf.b)
                        P.tt("dve", cf[:, 1:2], cf[:, 0:1], gBt[:, qt, gcol:gcol + 1], ALU.mult, [cf.b, gBt.b], [cf.b])
                        return cf
                    QS[qt] = (Lq, cm, of4, imp, st, coef)

                def cmp_jobs(qt):
                    Lq, cm, of4, imp, st, coef = QS[qt]
                    for h in range(4):
                        acc = accp.next()

                        def post(h=h, acc=acc, of4=of4, imp=imp, coef=coef):
                            cf = coef(acc, 128, 3 * h + 0)
                            P.ts("dve", of4[:, h, :], acc[:, 0:128], cf[:, 1:2], None, ALU.mult, None, [acc.b, cf.b], [of4.b])
                            if h == 0:
                                P.ts("dve", imp[:], acc[:, 129:193], cf[:, 0:1], None, ALU.mult, None, [acc.b, cf.b], [imp.b])
                            else:
                                P.stt(imp[:], acc[:, 129:193], cf[:, 0:1], imp[:], ALU.mult, ALU.add,
                                      [acc.b, cf.b, imp.b], [imp.b])

                        def score_fn(kt, h=h, qt=qt):
                            return [(kcm[:, kt * 128:(kt + 1) * 128], Qb[h][:, qt * 128:(qt + 1) * 128], [kcm.b, Qb[h].b])]

                        def mask_fn(kt, cm=cm):
                            return [(cm[:, kt * 128:(kt + 1) * 128], ident[:], [cm.b, ident.b])]

                        def v_fn(kt):
                            return vca[:, kt, :], [vca.b]

                        yield from head_jobs(ptp, score_fn, mask_fn, v_fn, acc, 193, range(2), 128 ** -0.5, post)


                def make_select(qt):
                    Lq, cm, of4, imp, st, coef = QS[qt]
                    def select(qt=qt, imp=imp, st=st, Lq=Lq):
                        sc = scp.next()
                        P.tt("dve", sc[:], imp[:], selb[:, qt, :], ALU.add, [imp.b, selb.b], [sc.b])
                        P.tt("dve", cmpm[:], sc[:].unsqueeze(1).to_broadcast([128, 64, 64]),
                             sc[:].unsqueeze(2).to_broadcast([128, 64, 64]), ALU.is_gt, [sc.b], [cmpm.b])
                        rk = rank.next()
                        P.red(rk[:], cmpm[:], ALU.add, [cmpm.b], [rk.b])
                        P.ts("dve", rk[:], rk[:], 15.5, NEG, ALU.is_gt, ALU.mult, [rk.b], [rk.b])
                        Mk = Mkp.next()
                        nb = Lq // 64
                        P.copy("pool", Mk[:, :Lq].rearrange("p (j c) -> p j c", c=64),
                               rk[:, :nb].unsqueeze(2).to_broadcast([128, nb, 64]), [rk.b], [Mk.b])
                        P.tt("pool", Mk[:, Lq - 128:Lq], Mk[:, Lq - 128:Lq], causal[:], ALU.add, [Mk.b, causal.b], [Mk.b])
                        st["Mk"] = Mk

                    return select

                def slc_jobs(qt):
                    Lq, cm, of4, imp, st, coef = QS[qt]
                    for h in range(4):
                        acc = accp.next()

                        def post(h=h, acc=acc, of4=of4, coef=coef):
                            cf = coef(acc, 128, 3 * h + 1)
                            P.stt(of4[:, h, :], acc[:, 0:128], cf[:, 1:2], of4[:, h, :], ALU.mult, ALU.add,
                                  [acc.b, cf.b, of4.b], [of4.b])

                        def score_fn(kt, h=h, qt=qt):
                            return [(ks[:, kt * 128:(kt + 1) * 128], Qb[h][:, qt * 128:(qt + 1) * 128], [ks.b, Qb[h].b])]

                        def mask_fn(kt, st=st):
                            Mk = st["Mk"]
                            return [(Mk[:, kt * 128:(kt + 1) * 128], ident[:], [Mk.b, ident.b])]

                        def v_fn(kt):
                            return vsa[:, kt, :], [vsa.b]

                        yield from head_jobs(ptp, score_fn, mask_fn, v_fn, acc, 129, range(qt + 1), 128 ** -0.5, post)


                def win_jobs(qt, pre):
                    Lq, cm, of4, imp, st, coef = QS[qt]
                    for h in range(4):
                        acc = accp.next()

                        def post(h=h, acc=acc, of4=of4, qt=qt, coef=coef):
                            cf = coef(acc, 128, 3 * h + 2)
                            P.stt(of4[:, h, :], acc[:, 0:128], cf[:, 1:2], of4[:, h, :], ALU.mult, ALU.add,
                                  [acc.b, cf.b, of4.b], [of4.b])
                            ob = obp.next()
                            P.copy("pool", ob[:], of4[:, h, :], [of4.b], [ob.b])
                            P.dma(Sx["O_B"][qt * 128:(qt + 1) * 128, h * 128:(h + 1) * 128], ob[:], R=[ob.b], sb=ob.b)

                        def score_fn(kt, h=h, qt=qt):
                            return [(kw[:, kt * 128:(kt + 1) * 128], Qb[h][:, qt * 128:(qt + 1) * 128], [kw.b, Qb[h].b])]

                        def mask_fn(kt, qt=qt):
                            if kt == qt:
                                return [(causal[:], ident[:], [causal.b, ident.b])]
                            if kt == qt - 4:
                                return [(anti[:], ident[:], [anti.b, ident.b])]
                            return []

                        def v_fn(kt):
                            return vwa[:, kt, :], [vwa.b]

                        yield from head_jobs(ptp, score_fn, mask_fn, v_fn, acc, 129, range(max(0, qt - 4), qt + 1),
                                             128 ** -0.5, post, pre if h == 0 else None)

                setup(0)
                yield from cmp_jobs(0)
                sel0 = make_select(0)
                first = True
                for qt in range(NT):
                    if qt + 1 < NT:
                        setup(qt + 1)
                        gen = cmp_jobs(qt + 1)
                        if first:
                            j0 = next(gen)
                            j0.pre = sel0
                            yield j0
                            first = False
                        yield from gen
                    yield from slc_jobs(qt)
                    yield from win_jobs(qt, make_select(qt + 1) if qt + 1 < NT else None)
            run_jobs(jobsB())
            P.barrier()
        if stop_after == "BB":
            break
        with scope() as esc:
            wstage = sbpool(nc, esc, "wstC", 2, [128, 1024], F32)
            Wg = load_w_bf16(esc, "Wg", I["w_in"][l][:, OFF["gate"]:OFF["gate"] + 4096], 8, 4096, wstage)
            Wb = load_w_bf16(esc, "Wb", I["w_branch"][l].rearrange("n w d -> (n w) d"), 16, D, wstage)
            Wo = load_w_bf16(esc, "Wo", I["w_out"][l], 8, D, wstage)
            g1 = load_bcast(esc, "g1c", I["norm1_g"][l:l + 1, :], D)
            xs = sbpool(nc, esc, "xsC", 2, [128, D], F32)
            xop = sbpool(nc, esc, "xoC", 2, [128, D], F32)
            hbp = sbpool(nc, esc, "hbC", 2, [128, D], BF16)
            hTp = sbpool(nc, esc, "hTC", 2, [128, 8, 128], BF16)
            ob4p = sbpool(nc, esc, "ob4", 1, [128, 4, 512], BF16)
            oTp = sbpool(nc, esc, "oT", 1, [128, 16, 128], BF16)
            mrg = sbpool(nc, esc, "mrg", 1, [128, D], F32)
            mbp = sbpool(nc, esc, "mb", 1, [128, D], BF16)
            mTp = sbpool(nc, esc, "mT", 2, [128, 8, 128], BF16)
            sgp = sbpool(nc, esc, "sg", 2, [128, 512], F32)
            tmpp = sbpool(nc, esc, "tmpC", 2, [128, 512], F32)
            for tt in range(NT):
                rows = slice(tt * 128, (tt + 1) * 128)
                xt = xs.next()
                P.dma(xt[:], xsrc[rows, :], W=[xt.b], sb=xt.b)
                ob4 = ob4p.next()
                for n_, nm in enumerate(("O_A", "O_B", "O_C", "O_D")):
                    P.dma(ob4[:, n_, :], Sx[nm][rows, :], W=[ob4.b], sb=ob4.b)
                ss = rms(xt[:], D, [xt.b])
                hb = hbp.next()
                P.stt(hb[:], xt[:], ss[:, 1:2], g1[:], ALU.mult, ALU.mult, [xt.b, ss.b, g1.b], [hb.b])
                pb = psb.next()
                for k in range(8):
                    P.tr(pb[:, k * 128:(k + 1) * 128], hb[:, k * 128:(k + 1) * 128], [hb.b, ident.b], [pb.b])
                hTt = hTp.next()
                P.copy("dve", hTt[:], pb[:].rearrange("p (k c) -> p k c", k=8), [pb.b], [hTt.b])
                oT = oTp.next()
                for g in range(2):
                    pb = psb.next()
                    for k in range(8):
                        kk = g * 8 + k
                        P.tr(pb[:, k * 128:(k + 1) * 128], ob4[:, kk // 4, (kk % 4) * 128:(kk % 4 + 1) * 128],
                             [ob4.b, ident.b], [pb.b])
                    P.copy("dve", oT[:, g * 8:(g + 1) * 8, :], pb[:].rearrange("p (k c) -> p k c", k=8), [pb.b], [oT.b])
                mg = mrg.next()
                for n_ in range(4):
                    for j in range(2):
                        cs = slice(j * 512, (j + 1) * 512)
                        pg = psf.next()
                        for k in range(8):
                            P.mm(pg[:, :], hTt[:, k, :], Wg[:, k, n_ * 1024 + j * 512:n_ * 1024 + (j + 1) * 512],
                                 k == 0, k == 7, [hTt.b, Wg.b], [pg.b])
                        sg = sgp.next()
                        P.act(sg[:], pg[:, :], AF.Sigmoid, [pg.b], [sg.b])
                        pbr = psf.next()
                        for k in range(4):
                            P.mm(pbr[:, :], oT[:, n_ * 4 + k, :], Wb[:, n_ * 4 + k, cs], k == 0, k == 3, [oT.b, Wb.b], [pbr.b])
                        if n_ == 0:
                            P.tt("dve", mg[:, cs], pbr[:, :], sg[:], ALU.mult, [pbr.b, sg.b], [mg.b])
                        else:
                            tm = tmpp.next()
                            P.tt("dve", tm[:], pbr[:, :], sg[:], ALU.mult, [pbr.b, sg.b], [tm.b])
                            P.tt("pool", mg[:, cs], mg[:, cs], tm[:], ALU.add, [mg.b, tm.b], [mg.b])
                mb = mbp.next()
                P.copy("pool", mb[:], mg[:], [mg.b], [mb.b])
                pb = psb.next()
                for k in range(8):
                    P.tr(pb[:, k * 128:(k + 1) * 128], mb[:, k * 128:(k + 1) * 128], [mb.b, ident.b], [pb.b])
                mT = mTp.next()
                P.copy("dve", mT[:], pb[:].rearrange("p (k c) -> p k c", k=8), [pb.b], [mT.b])
                xo = xop.next()
                for j in range(2):
                    cs = slice(j * 512, (j + 1) * 512)
                    po = psf.next()
                    for k in range(8):
                        P.mm(po[:, :], mT[:, k, :], Wo[:, k, cs], k == 0, k == 7, [mT.b, Wo.b], [po.b])
                    P.tt("dve", xo[:, cs], po[:, :], xt[:, cs], ALU.add, [po.b, xt.b], [xo.b])
                P.dma(Sx["xb"][rows, :], xo[:], R=[xo.b], sb=xo.b)
            P.barrier()
        if stop_after == "Ca":
            break

        with scope() as esd:
            wstage = sbpool(nc, esd, "wstD", 2, [128, 1024], F32)
            Wgu = load_w_bf16(esd, "Wgu", I["w_gate_up"][l], 8, 2 * DFF, wstage)
            Wd = load_w_bf16(esd, "Wd", I["w_down"][l], 22, D, wstage)
            g2 = load_bcast(esd, "g2", I["norm2_g"][l:l + 1, :], D)
            last = (li == n_layers - 1)
            if last and final_norm:
                gf = load_bcast(esd, "gf", I["final_norm_g"][0:1, :], D)
            xs = sbpool(nc, esd, "xsD", 2, [128, D], F32)
            xop = sbpool(nc, esd, "xoD", 2, [128, D], F32)
            hbp = sbpool(nc, esd, "hbD", 2, [128, D], BF16)
            hTp = sbpool(nc, esd, "hTD", 2, [128, 8, 128], BF16)
            actp = sbpool(nc, esd, "actb", 1, [128, DFF], BF16)
            aTp = sbpool(nc, esd, "aT", 1, [128, 22, 128], BF16)
            sgp = sbpool(nc, esd, "sgD", 3, [128, 256], F32)
            for tt in range(NT):
                rows = slice(tt * 128, (tt + 1) * 128)
                xt = xs.next()
                P.dma(xt[:], Sx["xb"][rows, :], W=[xt.b], sb=xt.b)
                ss = rms(xt[:], D, [xt.b])
                hb = hbp.next()
                P.stt(hb[:], xt[:], ss[:, 1:2], g2[:], ALU.mult, ALU.mult, [xt.b, ss.b, g2.b], [hb.b])
                pb = psb.next()
                for k in range(8):
                    P.tr(pb[:, k * 128:(k + 1) * 128], hb[:, k * 128:(k + 1) * 128], [hb.b, ident.b], [pb.b])
                hTt = hTp.next()
                P.copy("dve", hTt[:], pb[:].rearrange("p (k c) -> p k c", k=8), [pb.b], [hTt.b])
                ab = actp.next()
                for j in range(11):
                    pg = psf.next()
                    for part in range(2):
                        c0 = part * DFF + j * 256
                        for k in range(8):
                            P.mm(pg[:, part * 256:(part + 1) * 256], hTt[:, k, :], Wgu[:, k, c0:c0 + 256], k == 0, k == 7,
                                 [hTt.b, Wgu.b], [pg.b])
                    sg = sgp.next()
                    P.act(sg[:], pg[:, 0:256], AF.Silu, [pg.b], [sg.b])
                    P.tt("dve", ab[:, j * 256:(j + 1) * 256], pg[:, 256:512], sg[:], ALU.mult, [pg.b, sg.b], [ab.b])
                aT = aTp.next()
                for g0 in range(0, 22, 8):
                    ng = min(8, 22 - g0)
                    pb = psb.next()
                    for k in range(ng):
                        P.tr(pb[:, k * 128:(k + 1) * 128], ab[:, (g0 + k) * 128:(g0 + k + 1) * 128], [ab.b, ident.b], [pb.b])
                    P.copy("dve", aT[:, g0:g0 + ng, :], pb[:, :ng * 128].rearrange("p (k c) -> p k c", k=ng), [pb.b], [aT.b])
                xo = xop.next()
                for j in range(2):
                    cs = slice(j * 512, (j + 1) * 512)
                    pd = psf.next()
                    for k in range(22):
                        P.mm(pd[:, :], aT[:, k, :], Wd[:, k, cs], k == 0, k == 21, [aT.b, Wd.b], [pd.b])
                    P.tt("dve", xo[:, cs], pd[:, :], xt[:, cs], ALU.add, [pd.b, xt.b], [xo.b])
                if last:
                    if final_norm:
                        ss2 = rms(xo[:], D, [xo.b])
                        P.stt(xo[:], xo[:], ss2[:, 1:2], gf[:], ALU.mult, ALU.mult, [xo.b, ss2.b, gf.b], [xo.b])
                    P.dma(out[rows, :], xo[:], R=[xo.b], sb=xo.b)
                else:
                    P.dma(Sx["xa"][rows, :], xo[:], R=[xo.b], sb=xo.b)
            P.barrier()

    P.barrier()
    with nc.Block() as blk:
        @blk.tensor
        def _(e):
            P.replay(e, "pe")

        @blk.scalar
        def _(e):
            P.replay(e, "act")

        @blk.vector
        def _(e):
            P.replay(e, "dve")

        @blk.gpsimd
        def _(e):
            P.replay(e, "pool")

        @blk.sync
        def _(e):
            P.replay(e, "sp")
    es_top.close()
    C.nsem = P.nsem
    C.maxval = P.maxval
    C.counts = dict(P.seq)
    return nc, C


def host_consts():
    bf = ml_dtypes.bfloat16
    c = {}
    c["c_ident"] = np.eye(128, dtype=np.float32).astype(bf)
    t = np.arange(128)[:, None]
    s = np.arange(128)[None, :]
    c["c_causal"] = np.where(s > t, NEG, 0.0).astype(np.float32).astype(bf)
    c["c_anti"] = np.where(s <= t, NEG, 0.0).astype(np.float32).astype(bf)
    tt = np.arange(S)[:, None]
    n = np.arange(256)[None, :]
    c["c_cmask"] = np.where(16 * n + 31 > tt, NEG, 0.0).astype(np.float32).astype(bf)
    j = np.arange(64)[None, :]
    cur = tt // 64
    forced = (j == 0) | (j == cur) | (j == cur - 1)
    visible = (j * 64) <= tt
    c["c_selb"] = np.where(visible, np.where(forced, 1e9, 0.0), -1e30).astype(np.float32)
    c["c_negbig"] = np.where(s > t, -1e30, 0.0).astype(np.float32)
    c["c_posbig"] = np.where(s > t, 1e30, 0.0).astype(np.float32)
    nn = np.arange(256)[:, None]
    cs = nn * 16
    ce = cs + 31
    ss_ = np.arange(64)[None, :] * 64
    ov = ((cs < ss_ + 64) & (ce >= ss_)).astype(np.float32)
    ov[255, :] = 0.0
    c["c_overlap"] = ov.astype(bf)
    for nm, rot in (("da", 16), ("nsa", 32), ("mla", 64), ("dsa", 32), ("idx", 16)):
        inv = np.power(np.float32(500000.0), -np.arange(0, rot, 2, dtype=np.float32) / np.float32(rot)).astype(np.float32)
        ang = (np.arange(S, dtype=np.float32)[:, None] * inv[None, :]).astype(np.float32)
        c["cos_" + nm] = np.cos(ang).astype(np.float32)
        c["sin_" + nm] = np.sin(ang).astype(np.float32)
    return c


_CACHE = {}


def kernel(**inputs):
    x = np.ascontiguousarray(inputs["x"], dtype=np.float32)
    if "prog" not in _CACHE:
        _CACHE["prog"] = build(DEPTH)
    nc, C = _CACHE["prog"]
    consts = host_consts()
    base = {k: np.ascontiguousarray(v, dtype=np.float32) for k, v in inputs.items() if k != "x"}
    base["final_norm_g"] = base["final_norm_g"].reshape(1, D)
    base.update(consts)
    in_maps = []
    cmap = {0: 0, 1: 1, 4: 2, 5: 3}
    zx = np.zeros_like(x[0])
    for c in range(8):
        m = dict(base)
        m["x"] = x[cmap[c]] if c in cmap else zx
        in_maps.append(m)
    res = run_bass_kernel_spmd(nc, in_maps, core_ids=list(range(8)))
    inv = {b: c for c, b in cmap.items()}
    outs = [res.results[inv[b]]["out"] for b in range(4)]
    return np.stack(outs, axis=0).astype(np.float32)
```

```python
import math
from contextlib import ExitStack
import numpy as np
import ml_dtypes
import concourse.bass as bass
import concourse.mybir as mybir
from concourse.bass_utils import run_bass_kernel_spmd

F32 = mybir.dt.float32
BF16 = mybir.dt.bfloat16
AF = mybir.ActivationFunctionType
ALU = mybir.AluOpType
AX = mybir.AxisListType

S = 4096
D = 1024
NT = S // 128
DEPTH = 4
D_IN = 9748
DFF = 2816
NEG = -30000.0
EPOCH = 16000
DEPOCH = 1000
TINY = 1e-30

OFF = {}
_o = 0
for _nm, _n in (("a_q", 512), ("a_k", 512), ("a_v", 512), ("b_q", 512), ("b_kc", 128), ("b_vc", 128),
                ("b_ks", 128), ("b_vs", 128), ("b_kw", 128), ("b_vw", 128), ("b_g", 12), ("c_q", 384),
                ("c_kv", 256), ("c_kr", 64), ("d_q", 512), ("d_k", 512), ("d_v", 512), ("d_iq", 512),
                ("d_ik", 64), ("d_iw", 8), ("gate", 4096)):
    OFF[_nm] = _o
    _o += _n
assert _o == D_IN


_BUID = [0]


class Buf:
    __slots__ = ("name", "w", "rs", "dsem", "dbase", "dcnt", "uid")

    def __init__(self, name):
        self.name = name
        self.w = None
        self.rs = {}
        self.dsem = None
        self.dbase = 0
        self.dcnt = 0
        _BUID[0] += 1
        self.uid = _BUID[0]


class Tn:
    def __init__(self, t, name):
        self.t = t
        self.b = Buf(name)

    def __getitem__(self, k):
        return self.t[k]


class FreePool:
    def __init__(self, items):
        self.items = items
        self.free_list = list(items)

    def alloc(self):
        assert self.free_list, "FreePool exhausted"
        return self.free_list.pop(0)

    def free(self, x):
        self.free_list.append(x)


class Pool:
    def __init__(self, items):
        self.items = items
        self.i = 0

    def next(self):
        x = self.items[self.i % len(self.items)]
        self.i += 1
        return x


ENGS = ("pe", "act", "dve", "pool", "sp")


class Prog:
    def __init__(self, nc, es):
        self.nc = nc
        self.es = es
        self.streams = {e: [] for e in ENGS}
        self.seq = {e: 0 for e in ENGS}
        self.esem = {e: [] for e in ENGS}
        self.wd = {e: {} for e in ENGS}
        self.dirty = {}
        self.nsem = 0
        self.ident = None
        self.free_d = []
        self.maxval = 0
        self.scopes = []

    def _newsem(self, name):
        self.nsem += 1
        return self.es.enter_context(self.nc.semaphore(name))

    def _ev_sem(self, ev):
        if ev[0] == "e":
            _, eng, seq = ev
            ep = (seq - 1) // EPOCH
            while len(self.esem[eng]) <= ep:
                self.esem[eng].append(self._newsem(f"e_{eng}_{len(self.esem[eng])}"))
            return self.esem[eng][ep], (seq - 1) % EPOCH + 1
        _, buf, cnt = ev
        if buf.dsem is None:
            if self.free_d:
                buf.dsem, buf.dbase = self.free_d.pop(0)
            else:
                buf.dsem, buf.dbase = self._newsem(f"d_{self.nsem}"), 0
        v = buf.dbase + 16 * cnt
        self.maxval = max(self.maxval, v)
        assert v < 60000
        return buf.dsem, v

    def release(self, buf):
        if buf.dsem is not None:
            self.free_d.append((buf.dsem, buf.dbase + 16 * buf.dcnt))
            buf.dsem = None
            buf.dcnt = 0
            buf.dbase = 0

    def _wait(self, eng, ev, raw):
        if ev[0] == "e":
            if ev[1] == eng and (eng == "pe" or eng == "sp" or not raw):
                return
            key = ("e", ev[1])
            val = ev[2]
        else:
            key = ("d", ev[1].uid)
            val = ev[2]
        if self.wd[eng].get(key, 0) >= val:
            return
        self.wd[eng][key] = val
        sem, v = self._ev_sem(ev)
        self.streams[eng].append(("w", sem, v))

    def _deps(self, eng, reads, writes):
        for b in reads:
            if b.w is not None:
                self._wait(eng, b.w, True)
        for b in writes:
            if b.w is not None:
                self._wait(eng, b.w, False)
            for ev in b.rs.values():
                self._wait(eng, ev, False)

    def _mark(self, me, reads, writes):
        for b in reads:
            k = ("e", me[1]) if me[0] == "e" else ("d", me[1].uid)
            b.rs[k] = me
        for b in writes:
            b.w = me
            b.rs = {}

    def op(self, eng, fn, reads=(), writes=()):
        self._deps(eng, reads, writes)
        self.seq[eng] += 1
        me = ("e", eng, self.seq[eng])
        sem, _ = self._ev_sem(me)
        self.streams[eng].append(("o", fn, sem, 1))
        self._mark(me, reads, writes)

    def dma(self, out, in_, R=(), W=(), sb=None, q="sp", slow=False):
        assert sb is not None
        self._deps(q, R, W)
        if sb.dcnt > 0:
            self._wait(q, ("d", sb, sb.dcnt), False)
        sb.dcnt += 1
        me = ("d", sb, sb.dcnt)
        sem, _ = self._ev_sem(me)
        if slow:
            fn = lambda e: e.dma_start(out=out, in_=in_, allow_slow_non_contiguous=True)
        else:
            fn = lambda e: e.dma_start(out=out, in_=in_)
        self.streams[q].append(("o", fn, sem, 16))
        self.dirty[sb.uid] = sb
        self._mark(me, R, W)

    def barrier(self):
        for eng in ENGS:
            for x in ENGS:
                if x != eng and self.seq[x] > 0:
                    ev = ("e", x, self.seq[x])
                    key = ("e", x)
                    if self.wd[eng].get(key, 0) < ev[2]:
                        self.wd[eng][key] = ev[2]
                        sem, v = self._ev_sem(ev)
                        self.streams[eng].append(("w", sem, v))
            for b in self.dirty.values():
                self._wait(eng, ("d", b, b.dcnt), False)
        self.dirty = {}

    def replay(self, e, eng):
        for it in self.streams[eng]:
            if it[0] == "w":
                e.wait_ge(it[1], it[2])
            else:
                it[1](e).then_inc(it[2], it[3])

    def mm(self, out, lhsT, rhs, start, stop, R, W):
        self.op("pe", lambda e: e.matmul(out, lhsT=lhsT, rhs=rhs, start=start, stop=stop), R, W)

    def tr(self, out, in_, R, W):
        k = in_.shape[0]
        idn = self.ident[:k, :k]
        self.op("pe", lambda e: e.transpose(out=out, in_=in_, identity=idn), R, W)

    def act(self, out, in_, func, R, W, scale=1.0, bias=None, accum=None):
        kw = {}
        if bias is not None:
            kw["bias"] = bias
        if accum is not None:
            kw["accum_out"] = accum
        self.op("act", lambda e: e.activation(out=out, in_=in_, func=func, scale=scale, **kw), R, W)

    def tt(self, eng, out, in0, in1, op, R, W):
        self.op(eng, lambda e: e.tensor_tensor(out=out, in0=in0, in1=in1, op=op), R, W)

    def ts(self, eng, out, in0, s1, s2, op0, op1, R, W, accum=None):
        if op1 is None:
            self.op(eng, lambda e: e.tensor_scalar(out=out, in0=in0, scalar1=s1, scalar2=None, op0=op0), R, W)
        elif accum is None:
            self.op(eng, lambda e: e.tensor_scalar(out=out, in0=in0, scalar1=s1, scalar2=s2, op0=op0, op1=op1), R, W)
        else:
            self.op(eng, lambda e: e.tensor_scalar(out=out, in0=in0, scalar1=s1, scalar2=s2, op0=op0, op1=op1,
                                                   accum_out=accum), R, W)

    def stt(self, out, in0, scalar, in1, op0, op1, R, W):
        self.op("dve", lambda e: e.scalar_tensor_tensor(out=out, in0=in0, scalar=scalar, in1=in1, op0=op0, op1=op1),
                R, W)

    def copy(self, eng, out, in_, R, W):
        if eng == "act":
            self.op("act", lambda e: e.activation(out=out, in_=in_, func=AF.Copy), R, W)
        else:
            self.op(eng, lambda e: e.tensor_copy(out=out, in_=in_), R, W)

    def memset(self, eng, ap, val, W):
        self.op(eng, lambda e: e.memset(ap, val), (), W)

    def red(self, out, in_, op, R, W):
        self.op("dve", lambda e: e.tensor_reduce(out=out, in_=in_, axis=AX.X, op=op), R, W)


class Ctx:
    pass


_UNIQ = [0]


_CURP = [None]


def sb(nc, es, name, shape, dt):
    _UNIQ[0] += 1
    nm = f"s{_UNIQ[0]}_{name}"
    t = Tn(es.enter_context(nc.sbuf_tensor(nm, list(shape), dt)), nm)
    P = _CURP[0]
    if P is not None and P.scopes:
        P.scopes[-1].append(t.b)
    return t


class scope:
    def __enter__(self):
        self.P = _CURP[0]
        self.P.scopes.append([])
        self.es = ExitStack()
        return self.es.__enter__()

    def __exit__(self, *a):
        r = self.es.__exit__(*a)
        for b in self.P.scopes.pop():
            self.P.release(b)
        return r


def sbpool(nc, es, name, n, shape, dt):
    return Pool([sb(nc, es, f"{name}{i}", shape, dt) for i in range(n)])


def bcast_rows(ap1d_row, n=128):
    return ap1d_row.partition_broadcast(n)


def build(n_layers, first_layer=0, final_norm=True, debug=False, stop_after=None, branches="ABCD"):
    nc = bass.Bass("TRN2", target_bir_lowering=False)
    es_top = ExitStack()
    C = Ctx()
    C.nc = nc
    L = DEPTH

    def din(name, shape, dt=F32):
        return nc.dram_tensor(name, list(shape), dt, kind="ExternalInput").ap()

    okind = "ExternalOutput" if debug else "Internal"

    def dscr(name, shape, dt=BF16):
        return nc.dram_tensor(name, list(shape), dt, kind=okind).ap()

    I = {}
    I["x"] = din("x", [S, D])
    for nm, shp in (("norm1_g", [L, D]), ("w_in", [L, D, D_IN]), ("diff_lq1", [L, 64]), ("diff_lk1", [L, 64]),
                    ("diff_lq2", [L, 64]), ("diff_lk2", [L, 64]), ("diff_subln_g", [L, 128]),
                    ("nsa_pe_k", [L, 32, 128]), ("nsa_w1_k", [L, 4096, 128]), ("nsa_w2_k", [L, 128, 128]),
                    ("nsa_pe_v", [L, 32, 128]), ("nsa_w1_v", [L, 4096, 128]), ("nsa_w2_v", [L, 128, 128]),
                    ("mla_q_norm_g", [L, 384]), ("mla_w_uq", [L, 384, 768]), ("mla_kv_norm_g", [L, 256]),
                    ("mla_w_ukv", [L, 256, 1024]), ("idx_k_norm_g", [L, 64]), ("w_branch", [L, 4, 512, D]),
                    ("w_out", [L, D, D]), ("norm2_g", [L, D]), ("w_gate_up", [L, D, 2 * DFF]),
                    ("w_down", [L, DFF, D]), ("final_norm_g", [1, D])):
        I[nm] = din(nm, shp)
    I["c_ident"] = din("c_ident", [128, 128], BF16)
    I["c_causal"] = din("c_causal", [128, 128], BF16)
    I["c_anti"] = din("c_anti", [128, 128], BF16)
    I["c_cmask"] = din("c_cmask", [S, 256], BF16)
    I["c_selb"] = din("c_selb", [S, 64], F32)
    I["c_negbig"] = din("c_negbig", [128, 128], F32)
    I["c_posbig"] = din("c_posbig", [128, 128], F32)
    I["c_overlap"] = din("c_overlap", [256, 64], BF16)
    for nm, half in (("da", 8), ("nsa", 16), ("mla", 32), ("dsa", 16), ("idx", 8)):
        I["cos_" + nm] = din("cos_" + nm, [S, half])
        I["sin_" + nm] = din("sin_" + nm, [S, half])
    out = nc.dram_tensor("out", [S, D], F32, kind="ExternalOutput").ap()

    Sx = {}
    Sx["xa"] = dscr("xa", [S, D], F32)
    Sx["xb"] = dscr("xb", [S, D], F32)
    for nm, rows in (("QT_A", 512), ("KT_A", 512), ("QT_B", 512), ("bkT", 384), ("vcT", 128), ("QT_Cn", 512),
                     ("QT_Cr", 256), ("KT_Cn", 512), ("kpeT", 64), ("QT_D", 512), ("KT_D", 512), ("iqT", 512),
                     ("ikT", 64)):
        Sx[nm] = dscr(nm, [rows, S])
    for nm, cols in (("V_A", 512), ("vs", 128), ("vw", 128), ("V_C", 512), ("V_D", 512), ("O_A", 512),
                     ("O_B", 512), ("O_C", 512), ("O_D", 512)):
        Sx[nm] = dscr(nm, [S, cols])
    Sx["gB"] = dscr("gB", [S, 12], F32)
    Sx["iw"] = dscr("iw", [S, 8], F32)
    Sx["kcmpT"] = dscr("kcmpT", [128, 256])
    Sx["vcmp"] = dscr("vcmp", [256, 128])

    es = es_top
    P = Prog(nc, es)
    C.P = P
    _CURP[0] = P
    ident = sb(nc, es, "ident", [128, 128], BF16)
    P.ident = ident
    causal = sb(nc, es, "causal", [128, 128], BF16)
    anti = sb(nc, es, "anti", [128, 128], BF16)
    P.dma(ident[:], I["c_ident"][:, :], W=[ident.b], sb=ident.b)
    P.dma(causal[:], I["c_causal"][:, :], W=[causal.b], sb=causal.b)
    P.dma(anti[:], I["c_anti"][:, :], W=[anti.b], sb=anti.b)
    psf = Pool([Tn(es.enter_context(nc.psum_tensor(f"psf{i}", [128, 512], F32)), f"psf{i}") for i in range(6)])
    psb = Pool([Tn(es.enter_context(nc.psum_tensor(f"psb{i}", [128, 1024], BF16)), f"psb{i}") for i in range(2)])
    C.psf, C.psb = psf, psb
    C.small = sbpool(nc, es, "small", 8, [128, 4], F32)
    C.junk = sbpool(nc, es, "junk", 2, [128, 1024], F32)

    def rms(src_ap, n, R, eps=1e-6):
        ss = C.small.next()
        jk = C.junk.next()
        P.memset("dve", ss[:], 0.0, [ss.b])
        P.act(jk[:, :n], src_ap, AF.Square, R + [ss.b], [jk.b, ss.b], accum=ss[:, 0:1])
        P.ts("dve", ss[:, 2:3], ss[:, 0:1], 1.0 / n, eps, ALU.mult, ALU.add, [ss.b], [ss.b])
        P.act(ss[:, 3:4], ss[:, 2:3], AF.Ln, [ss.b], [ss.b])
        P.act(ss[:, 1:2], ss[:, 3:4], AF.Exp, [ss.b], [ss.b], scale=-0.5)
        return ss

    def load_bcast(es_, name, row_ap, n, q="sp"):
        t = sb(nc, es_, name, [128, n], F32)
        P.dma(t[:], row_ap.partition_broadcast(128), W=[t.b], sb=t.b, q=q)
        return t

    def load_w_bf16(es_, name, dram_ap, kc, ncols, stage_pool, chunk_cols=512):
        wt = sb(nc, es_, name, [128, kc, ncols], BF16)
        src = dram_ap.rearrange("(k p) n -> p k n", p=128)
        for k in range(kc):
            sw = stage_pool.items[0].t.shape[-1]
            for c0 in range(0, ncols, sw):
                w = min(sw, ncols - c0)
                st = stage_pool.next()
                P.dma(st[:, :w], src[:, k, c0:c0 + w], W=[st.b], sb=st.b)
                P.copy("pool", wt[:, k, c0:c0 + w], st[:, :w], [st.b], [wt.b])
        return wt

    for li in range(n_layers):
        l = first_layer + li
        xsrc = I["x"] if li == 0 else Sx["xa"]
        lam_init = 0.8 - 0.6 * math.exp(-0.3 * l)

        with scope() as esA:
            hT = sb(nc, esA, "hT", [128, 8, S], BF16)
            with scope() as es0:
                g1 = load_bcast(es0, "g1", I["norm1_g"][l:l + 1, :], D)
                xs = sbpool(nc, es0, "xs", 2, [128, D], F32)
                hbp = sbpool(nc, es0, "hb", 2, [128, D], BF16)
                for tt in range(NT):
                    xt = xs.next()
                    P.dma(xt[:], xsrc[tt * 128:(tt + 1) * 128, :], W=[xt.b], sb=xt.b)
                    ss = rms(xt[:], D, [xt.b])
                    hb = hbp.next()
                    P.stt(hb[:], xt[:], ss[:, 1:2], g1[:], ALU.mult, ALU.mult, [xt.b, ss.b, g1.b], [hb.b])
                    pb = psb.next()
                    for k in range(8):
                        P.tr(pb[:, k * 128:(k + 1) * 128], hb[:, k * 128:(k + 1) * 128], [hb.b, ident.b], [pb.b])
                    P.copy("dve", hT[:, :, tt * 128:(tt + 1) * 128], pb[:].rearrange("p (k c) -> p k c", k=8),
                           [pb.b], [hT.b])
                P.barrier()
            if stop_after == "A0":
                break
            with scope() as es1:
                wst = sbpool(nc, es1, "wst", 1, [128, 8, 512], F32)
                wbf = sbpool(nc, es1, "wbf", 2, [128, 8, 512], BF16)
                zp = sbpool(nc, es1, "z", 4, [128, 1024], F32)
                zbp = FreePool(sbpool(nc, es1, "zb", 12, [128, 1024], BF16).items)
                ztp = sbpool(nc, es1, "zt", 4, [128, 4, 128], BF16)
                rtmp = sbpool(nc, es1, "rtmp", 3, [128, 4, 128], F32)
                tabs = {}
                for nm, half in (("da", 8), ("nsa", 16), ("mla", 32), ("dsa", 16), ("idx", 8)):
                    ct = sb(nc, es1, "cos_" + nm, [128, NT, half], F32)
                    st = sb(nc, es1, "sin_" + nm, [128, NT, half], F32)
                    P.dma(ct[:], I["cos_" + nm].rearrange("(t p) h -> p t h", p=128), W=[ct.b], sb=ct.b)
                    P.dma(st[:], I["sin_" + nm].rearrange("(t p) h -> p t h", p=128), W=[st.b], sb=st.b)
                    tabs[nm] = (ct, st, half)
                gq = load_bcast(es1, "gq", I["mla_q_norm_g"][l:l + 1, :], 384)
                gkv = load_bcast(es1, "gkv", I["mla_kv_norm_g"][l:l + 1, :], 256)
                gik = load_bcast(es1, "gik", I["idx_k_norm_g"][l:l + 1, :], 64)
                wstage = sbpool(nc, es1, "wstage", 2, [128, 1024], F32)
                wuq = load_w_bf16(es1, "wuq", I["mla_w_uq"][l], 3, 768, wstage)
                wukv = load_w_bf16(es1, "wukv", I["mla_w_ukv"][l], 2, 1024, wstage)
                smallio = sbpool(nc, es1, "smallio", 4, [128, 16], F32)

                def rope(z, col0, G, dh, roff, kind, tt):
                    ct, st, half = tabs[kind]
                    v = z[:, col0:col0 + G * dh].rearrange("p (g d) -> p g d", g=G)
                    x1 = v[:, :, roff:roff + half]
                    x2 = v[:, :, roff + half:roff + 2 * half]
                    cc = ct[:, tt:tt + 1, :].to_broadcast([128, G, half])
                    sn = st[:, tt:tt + 1, :].to_broadcast([128, G, half])
                    tm = rtmp.next()
                    t = [tm[:, i, :G * half].rearrange("p (g h) -> p g h", g=G) for i in range(4)]
                    Rr = [z.b, ct.b, st.b]
                    P.tt("dve", t[0], x1, cc, ALU.mult, Rr, [tm.b])
                    P.tt("dve", t[1], x2, sn, ALU.mult, Rr, [tm.b])
                    P.tt("pool", t[2], x2, cc, ALU.mult, Rr, [tm.b])
                    P.tt("pool", t[3], x1, sn, ALU.mult, Rr, [tm.b])
                    P.tt("dve", x1, t[0], t[1], ALU.subtract, [tm.b], [z.b])
                    P.tt("dve", x2, t[2], t[3], ALU.add, [tm.b], [z.b])

                def store_T(zb, col0, ncols, dst, row0, tt):
                    pb = psb.next()
                    nj = (ncols + 127) // 128
                    for j in range(nj):
                        w = min(128, ncols - j * 128)
                        P.tr(pb[:w, j * 128:(j + 1) * 128], zb[:, col0 + j * 128:col0 + j * 128 + w],
                             [zb.b, ident.b], [pb.b])
                    zt = ztp.next()
                    if ncols >= 128:
                        P.copy("dve", zt[:, :nj, :], pb[:, :nj * 128].rearrange("p (j c) -> p j c", j=nj),
                               [pb.b], [zt.b])
                        P.dma(dst[row0:row0 + ncols, tt * 128:(tt + 1) * 128].rearrange("(j p) c -> p j c", p=128),
                              zt[:, :nj, :], R=[zt.b], sb=zt.b)
                    else:
                        P.copy("dve", zt[:ncols, 0, :], pb[:ncols, 0:128], [pb.b], [zt.b])
                        P.dma(dst[row0:row0 + ncols, tt * 128:(tt + 1) * 128], zt[:ncols, 0, :], R=[zt.b], sb=zt.b)

                def store_tok(zb, col0, ncols, dst, tt):
                    P.dma(dst[tt * 128:(tt + 1) * 128, :], zb[:, col0:col0 + ncols], R=[zb.b], sb=zb.b)

                def tobf(z, zb, c0, n):
                    P.copy("dve", zb[:, c0:c0 + n], z[:, c0:c0 + n], [z.b], [zb.b])

                def h_rope_T(G, dh, kind, dst):
                    def h(tt, z, n):
                        rope(z, 0, G, dh, 0, kind, tt)
                        zb = zbp.alloc()
                        tobf(z, zb, 0, n)
                        yield
                        store_T(zb, 0, n, Sx[dst], 0, tt)
                        zbp.free(zb)
                    return h

                def h_tok(dst):
                    def h(tt, z, n):
                        zb = zbp.alloc()
                        tobf(z, zb, 0, n)
                        store_tok(zb, 0, n, Sx[dst], tt)
                        zbp.free(zb)
                        return
                        yield
                    return h

                def h_bk(tt, z, n):
                    rope(z, 0, 3, 128, 0, "nsa", tt)
                    zb = zbp.alloc()
                    tobf(z, zb, 0, 384)
                    yield
                    store_T(zb, 0, 384, Sx["bkT"], 0, tt)
                    zbp.free(zb)

                def h_bv(tt, z, n):
                    zb = zbp.alloc()
                    tobf(z, zb, 0, 384)
                    yield
                    store_T(zb, 0, 128, Sx["vcT"], 0, tt)
                    P.dma(Sx["vs"][tt * 128:(tt + 1) * 128, :], zb[:, 128:256], R=[zb.b], sb=zb.b)
                    P.dma(Sx["vw"][tt * 128:(tt + 1) * 128, :], zb[:, 256:384], R=[zb.b], sb=zb.b)
                    zbp.free(zb)

                def norm_proj(z, c0, n, gt, wt, ncout, tt, res):
                    ss = rms(z[:, c0:c0 + n], n, [z.b])
                    zb = zbp.alloc()
                    P.stt(zb[:, :n], z[:, c0:c0 + n], ss[:, 1:2], gt[:], ALU.mult, ALU.mult, [z.b, ss.b, gt.b], [zb.b])
                    kc = n // 128
                    yield
                    pb = psb.next()
                    for k in range(kc):
                        P.tr(pb[:, k * 128:(k + 1) * 128], zb[:, k * 128:(k + 1) * 128], [zb.b, ident.b], [pb.b])
                    zbp.free(zb)
                    zt = ztp.next()
                    P.copy("dve", zt[:, :kc, :], pb[:, :kc * 128].rearrange("p (j c) -> p j c", j=kc), [pb.b], [zt.b])
                    z2 = zp.next()
                    for c in range(0, ncout, 512):
                        w = min(512, ncout - c)
                        ps = psf.next()
                        for k in range(kc):
                            P.mm(ps[:, :w], zt[:, k, :], wt[:, k, c:c + w], k == 0, k == kc - 1, [zt.b, wt.b], [ps.b])
                        P.copy("act", z2[:, c:c + w], ps[:, :w], [ps.b], [z2.b])
                    res.append(z2)

                def h_cq(tt, z, n):
                    sg = smallio.next()
                    P.act(sg[:, :12], z[:, 384:396], AF.Sigmoid, [z.b], [sg.b])
                    P.dma(Sx["gB"][tt * 128:(tt + 1) * 128, :], sg[:, :12], R=[sg.b], sb=sg.b)
                    res = []
                    yield from norm_proj(z, 0, 384, gq, wuq, 768, tt, res)
                    q = res[0]
                    rope(q, 0, 4, 192, 128, "mla", tt)
                    qb = zbp.alloc()
                    q3 = q[:, :768].rearrange("p (g d) -> p g d", g=4)
                    P.copy("pool", qb[:, 0:512].rearrange("p (g d) -> p g d", g=4), q3[:, :, 0:128], [q.b], [qb.b])
                    P.copy("pool", qb[:, 512:768].rearrange("p (g d) -> p g d", g=4), q3[:, :, 128:192], [q.b], [qb.b])
                    yield
                    store_T(qb, 0, 512, Sx["QT_Cn"], 0, tt)
                    store_T(qb, 512, 256, Sx["QT_Cr"], 0, tt)
                    zbp.free(qb)

                def h_ckv(tt, z, n):
                    rope(z, 256, 1, 64, 0, "mla", tt)
                    zb = zbp.alloc()
                    tobf(z, zb, 256, 64)
                    res = []
                    g_ = norm_proj(z, 0, 256, gkv, wukv, 1024, tt, res)
                    next(g_)
                    yield
                    store_T(zb, 256, 64, Sx["kpeT"], 0, tt)
                    zbp.free(zb)
                    for _ in g_:
                        pass
                    kv = res[0]
                    kb = zbp.alloc()
                    kv3 = kv[:, :1024].rearrange("p (g d) -> p g d", g=4)
                    P.copy("pool", kb[:, 0:512].rearrange("p (g d) -> p g d", g=4), kv3[:, :, 0:128], [kv.b], [kb.b])
                    P.copy("pool", kb[:, 512:1024].rearrange("p (g d) -> p g d", g=4), kv3[:, :, 128:256], [kv.b],
                           [kb.b])
                    yield
                    store_T(kb, 0, 512, Sx["KT_Cn"], 0, tt)
                    store_tok(kb, 512, 512, Sx["V_C"], tt)
                    zbp.free(kb)

                def h_ik(tt, z, n):
                    sg = smallio.next()
                    P.ts("dve", sg[:, :8], z[:, 64:72], (8 ** -0.5) * (64 ** -0.5), None, ALU.mult, None, [z.b], [sg.b])
                    P.dma(Sx["iw"][tt * 128:(tt + 1) * 128, :], sg[:, :8], R=[sg.b], sb=sg.b)
                    ss = rms(z[:, 0:64], 64, [z.b])
                    P.stt(z[:, 0:64], z[:, 0:64], ss[:, 1:2], gik[:], ALU.mult, ALU.mult, [z.b, ss.b, gik.b], [z.b])
                    rope(z, 0, 1, 64, 0, "idx", tt)
                    zb = zbp.alloc()
                    tobf(z, zb, 0, 64)
                    yield
                    store_T(zb, 0, 64, Sx["ikT"], 0, tt)
                    zbp.free(zb)

                chunks = [
                    ([("a_q", 512)], h_rope_T(8, 64, "da", "QT_A")),
                    ([("a_k", 512)], h_rope_T(8, 64, "da", "KT_A")),
                    ([("a_v", 512)], h_tok("V_A")),
                    ([("b_q", 512)], h_rope_T(4, 128, "nsa", "QT_B")),
                    ([("b_kc", 128), ("b_ks", 128), ("b_kw", 128)], h_bk),
                    ([("b_vc", 128), ("b_vs", 128), ("b_vw", 128)], h_bv),
                    ([("c_q", 384), ("b_g", 12)], h_cq),
                    ([("c_kv", 256), ("c_kr", 64)], h_ckv),
                    ([("d_q", 512)], h_rope_T(4, 128, "dsa", "QT_D")),
                    ([("d_k", 512)], h_rope_T(4, 128, "dsa", "KT_D")),
                    ([("d_v", 512)], h_tok("V_D")),
                    ([("d_iq", 512)], h_rope_T(8, 64, "idx", "iqT")),
                    ([("d_ik", 64), ("d_iw", 8)], h_ik),
                ]
                if stop_after == "A1a":
                    chunks = chunks[:3]
                wsrc = I["w_in"][l].rearrange("(k p) n -> p k n", p=128)

                def load_chunk(ci):
                    segs, _ = chunks[ci]
                    st = wst.next()
                    wb = wbf.next()
                    c = 0
                    for nm, n in segs:
                        P.dma(st[:, :, c:c + n], wsrc[:, :, OFF[nm]:OFF[nm] + n], W=[st.b], sb=st.b)
                        c += n
                    P.copy("pool", wb[:, :, :c], st[:, :, :c], [st.b], [wb.b])
                    return wb, c

                nxt = load_chunk(0)
                active = []

                def advance(flush=False):
                    while True:
                        keep = []
                        for it in active:
                            if it[1] > 0 and not flush:
                                it[1] -= 1
                                keep.append(it)
                                continue
                            try:
                                next(it[0])
                                it[1] = 1
                                keep.append(it)
                            except StopIteration:
                                pass
                        active[:] = keep
                        if not flush or not active:
                            break

                for ci in range(len(chunks)):
                    wb, n = nxt
                    if ci + 1 < len(chunks):
                        nxt = load_chunk(ci + 1)
                    handler = chunks[ci][1]
                    for tt in range(NT):
                        ps = psf.next()
                        for k in range(8):
                            P.mm(ps[:, :n], hT[:, k, tt * 128:(tt + 1) * 128], wb[:, k, :n], k == 0, k == 7,
                                 [hT.b, wb.b], [ps.b])
                        z = zp.next()
                        P.copy("act", z[:, :n], ps[:, :n], [ps.b], [z.b])
                        g_ = handler(tt, z, n)
                        try:
                            next(g_)
                            active.append([g_, 1])
                        except StopIteration:
                            pass
                        advance()
                advance(flush=True)
                P.barrier()
        if stop_after in ("A0", "A1a", "A1"):
            break
        with scope() as es2:
            tokp = sbpool(nc, es2, "ctok", 2, [128, S], BF16)
            w1st = sb(nc, es2, "w1st", [128, 32, 128], F32)
            w1bp = sbpool(nc, es2, "w1b", 2, [128, 32, 128], BF16)
            peTp = sbpool(nc, es2, "peT", 2, [128, 32], F32)
            w2stp = sbpool(nc, es2, "w2st", 2, [128, 128], F32)
            w2bp = sbpool(nc, es2, "w2b", 2, [128, 128], BF16)
            ctmp = sbpool(nc, es2, "ctmp", 3, [128, 256], BF16)
            gxp = sbpool(nc, es2, "gx", 2, [128, 256], F32)
            gx2p = sbpool(nc, es2, "gx2", 2, [128, 256], F32)
            gTp = sbpool(nc, es2, "gT", 2, [128, 256], BF16)
            cout = sbpool(nc, es2, "cout", 2, [128, 256], BF16)
            for kind in ("k", "v"):
                src = Sx["bkT"][0:128, :] if kind == "k" else Sx["vcT"][:, :]
                tk = tokp.next()
                P.dma(tk[:], src, W=[tk.b], sb=tk.b)
                P.dma(w1st[:], I["nsa_w1_" + kind][l].rearrange("(l d) o -> d l o", d=128), W=[w1st.b], sb=w1st.b)
                wb = w1bp.next()
                P.copy("pool", wb[:], w1st[:], [w1st.b], [wb.b])
                pt = peTp.next()
                P.dma(pt[:], I["nsa_pe_" + kind][l].rearrange("l d -> d l"), W=[pt.b], sb=pt.b, slow=True)
                w2s = w2stp.next()
                P.dma(w2s[:], I["nsa_w2_" + kind][l], W=[w2s.b], sb=w2s.b)
                w2 = w2bp.next()
                P.copy("pool", w2[:], w2s[:], [w2s.b], [w2.b])
                ps = psf.next()
                tk3 = tk[:].rearrange("p (b s) -> p b s", s=16)
                for lp in range(32):
                    tm = ctmp.next()
                    P.ts("dve", tm[:, :255], tk3[:, lp // 16:lp // 16 + 255, lp % 16], pt[:, lp:lp + 1], None,
                         ALU.add, None, [tk.b, pt.b], [tm.b])
                    P.mm(ps[:, :255], wb[:, lp, :], tm[:, :255], lp == 0, lp == 31, [wb.b, tm.b], [ps.b])
                gx = gxp.next()
                gx2 = gx2p.next()
                P.copy("act", gx[:, :255], ps[:, :255], [ps.b], [gx.b])
                P.tt("dve", gx2[:, :255], gx[:, :255], gx[:, :255], ALU.mult, [gx.b], [gx2.b])
                P.ts("dve", gx2[:, :255], gx2[:, :255], 0.044715, 1.0, ALU.mult, ALU.add, [gx2.b], [gx2.b])
                P.tt("dve", gx2[:, :255], gx2[:, :255], gx[:, :255], ALU.mult, [gx2.b, gx.b], [gx2.b])
                P.act(gx2[:, :255], gx2[:, :255], AF.Sigmoid, [gx2.b], [gx2.b], scale=2.0 * math.sqrt(2.0 / math.pi))
                gT = gTp.next()
                P.memset("dve", gT[:], 0.0, [gT.b])
                P.tt("dve", gT[:, :255], gx[:, :255], gx2[:, :255], ALU.mult, [gx.b, gx2.b], [gT.b])
                co = cout.next()
                if kind == "k":
                    ps2 = psf.next()
                    P.mm(ps2[:, :256], w2[:], gT[:, :256], True, True, [w2.b, gT.b], [ps2.b])
                    P.copy("dve", co[:], ps2[:, :256], [ps2.b], [co.b])
                    P.dma(Sx["kcmpT"][:, :], co[:], R=[co.b], sb=co.b)
                else:
                    for g in range(2):
                        ps2 = psf.next()
                        P.mm(ps2[:, :128], gT[:, g * 128:(g + 1) * 128], w2[:], True, True, [w2.b, gT.b], [ps2.b])
                        P.copy("dve", co[:, g * 128:(g + 1) * 128], ps2[:, :128], [ps2.b], [co.b])
                    P.dma(Sx["vcmp"].rearrange("(g p) d -> p g d", p=128), co[:].rearrange("p (g d) -> p g d", g=2),
                          R=[co.b], sb=co.b)
            P.barrier()
        if stop_after == "A2":
            break

        class PView:
            def __init__(self, tn):
                self.ap = tn[:].bitcast(F32)
                self.b = tn.b

            def __getitem__(self, k):
                return self.ap[k]

        psc2 = Pool(psf.items[0:2])
        psc3 = Pool(psf.items[0:2] + [PView(psb.items[0])])
        psc = psc3
        PIPE = [2]
        accp = Pool(psf.items[2:6])

        class Job:
            def __init__(self, scores, exp, pv, post=None, pre=None):
                self.scores, self.exp, self.pv, self.post, self.pre = scores, exp, pv, post, pre

        def run_jobs(jobs):
            pend = []

            def retire(n):
                while len(pend) > n:
                    p = pend.pop(0)
                    p.pv()
                    if p.post is not None:
                        p.post()

            for j in jobs:
                if j.pre is not None:
                    retire(0)
                    j.pre()
                j.scores()
                j.exp()
                pend.append(j)
                retire(PIPE[0])
            retire(0)

        def head_jobs(ptp, score_fn, mask_fn, v_fn, acc, nv, kts, scale, post=None, pre=None):
            kts = list(kts)
            chs = [kts[i:i + 4] for i in range(0, len(kts), 4)]
            for ci, ch in enumerate(chs):
                st = {}

                def scores(ch=ch, st=st):
                    sbk = psc.next()
                    st["sb"] = sbk
                    for j, kt in enumerate(ch):
                        terms = list(score_fn(kt))
                        mks = mask_fn(kt)
                        terms.extend(mks)
                        for i, (lt, rh, bufs) in enumerate(terms):
                            P.mm(sbk[:, j * 128:(j + 1) * 128], lt, rh, i == 0, i == len(terms) - 1, bufs, [sbk.b])

                def exp(ch=ch, st=st):
                    pt = ptp.next()
                    st["pt"] = pt
                    n = len(ch) * 128
                    P.act(pt[:, :n], st["sb"][:, :n], AF.Exp, [st["sb"].b], [pt.b], scale=scale)

                def pv(ch=ch, st=st, ci=ci):
                    for j, kt in enumerate(ch):
                        va, vb = v_fn(kt)
                        P.mm(acc[:, :nv], st["pt"][:, j * 128:(j + 1) * 128], va, ci == 0 and j == 0,
                             ci == len(chs) - 1 and j == len(ch) - 1, [st["pt"].b] + vb, [acc.b])

                yield Job(scores, exp, pv, post if ci == len(chs) - 1 else None, pre if ci == 0 else None)

        def recip_sum(acc, col, dst_ap, dst_buf):
            P.ts("dve", dst_ap, acc[:, col:col + 1], TINY, None, ALU.add, None, [acc.b], [dst_buf])
            P.op("dve", lambda e: e.reciprocal(out=dst_ap, in_=dst_ap), [dst_buf], [dst_buf])

        def load_vaug(V, src, c0, nvv=129):
            P.dma(V[:, :, 0:128], src[:, c0:c0 + 128].rearrange("(t p) c -> p t c", p=128), W=[V.b], sb=V.b)
            P.memset("pool", V[:, :, 128:129], 1.0, [V.b])

        if "A" in branches:
          with scope() as esb:
            ptp = sbpool(nc, esb, "pt", 3, [128, 512], BF16)
            Kp = sbpool(nc, esb, "K", 2, [128, S], BF16)
            Qp = sbpool(nc, esb, "Qpad", 2, [128, 2, S], BF16)
            for q_ in Qp.items:
                P.memset("pool", q_[64:128, 0, :], 0.0, [q_.b])
                P.memset("pool", q_[0:64, 1, :], 0.0, [q_.b])
            Vp = sbpool(nc, esb, "V", 2, [128, NT, 129], BF16)
            of = sbpool(nc, esb, "of", 3, [128, 128], F32)
            obp = sbpool(nc, esb, "ob", 3, [128, 128], BF16)
            lqs = [load_bcast(esb, nm, I[nm][l:l + 1, :], 64) for nm in ("diff_lq1", "diff_lk1", "diff_lq2", "diff_lk2")]
            lam = sb(nc, esb, "lam", [128, 8], F32)
            ltmp = sb(nc, esb, "ltmp", [128, 64], F32)
            for i in range(2):
                P.tt("dve", ltmp[:], lqs[2 * i][:], lqs[2 * i + 1][:], ALU.mult, [lqs[2 * i].b, lqs[2 * i + 1].b], [ltmp.b])
                P.red(lam[:, i:i + 1], ltmp[:], ALU.add, [ltmp.b], [lam.b])
            P.act(lam[:, 2:4], lam[:, 0:2], AF.Exp, [lam.b], [lam.b])
            P.tt("dve", lam[:, 4:5], lam[:, 3:4], lam[:, 2:3], ALU.subtract, [lam.b], [lam.b])
            P.ts("dve", lam[:, 5:6], lam[:, 4:5], -lam_init, None, ALU.add, None, [lam.b], [lam.b])
            gsub = load_bcast(esb, "gsub", I["diff_subln_g"][l:l + 1, :], 128)
            P.ts("dve", gsub[:], gsub[:], 1.0 - lam_init, None, ALU.mult, None, [gsub.b], [gsub.b])

            def jobsA():
                for h in range(4):
                    K, Q, V = Kp.next(), Qp.next(), Vp.next()
                    P.dma(K[:], Sx["KT_A"][h * 128:(h + 1) * 128, :], W=[K.b], sb=K.b)
                    P.dma(Q[0:64, 0, :], Sx["QT_A"][h * 128:h * 128 + 64, :], W=[Q.b], sb=Q.b)
                    P.dma(Q[64:128, 1, :], Sx["QT_A"][h * 128 + 64:(h + 1) * 128, :], W=[Q.b], sb=Q.b)
                    load_vaug(V, Sx["V_A"], h * 128)
                    for qt in range(NT):
                        accs = [accp.next(), accp.next()]

                        def post(h=h, qt=qt, accs=accs):
                            r = C.small.next()
                            recip_sum(accs[0], 128, r[:, 0:1], r.b)
                            recip_sum(accs[1], 128, r[:, 1:2], r.b)
                            P.tt("dve", r[:, 1:2], r[:, 1:2], lam[:, 5:6], ALU.mult, [r.b, lam.b], [r.b])
                            o = of.next()
                            P.ts("dve", o[:], accs[0][:, 0:128], r[:, 0:1], None, ALU.mult, None, [accs[0].b, r.b], [o.b])
                            P.stt(o[:], accs[1][:, 0:128], r[:, 1:2], o[:], ALU.mult, ALU.add, [accs[1].b, r.b, o.b], [o.b])
                            ss = rms(o[:], 128, [o.b])
                            ob = obp.next()
                            P.stt(ob[:], o[:], ss[:, 1:2], gsub[:], ALU.mult, ALU.mult, [o.b, ss.b, gsub.b], [ob.b])
                            P.dma(Sx["O_A"][qt * 128:(qt + 1) * 128, h * 128:(h + 1) * 128], ob[:], R=[ob.b], sb=ob.b)

                        for m in range(2):
                            def score_fn(kt, m=m, K=K, Q=Q, qt=qt):
                                return [(K[:, kt * 128:(kt + 1) * 128],
                                         Q[:, m, qt * 128:(qt + 1) * 128], [K.b, Q.b])]

                            def mask_fn(kt, qt=qt):
                                return [(causal[:], ident[:], [causal.b, ident.b])] if kt == qt else []

                            def v_fn(kt, V=V):
                                return V[:, kt, :], [V.b]

                            yield from head_jobs(ptp, score_fn, mask_fn, v_fn, accs[m], 129, range(qt + 1),
                                                 64 ** -0.5, post if m == 1 else None)
            run_jobs(jobsA())
            P.barrier()
        if stop_after == "BA":
            break

        if "C" in branches:
          with scope() as esb:
            ptp = sbpool(nc, esb, "pt", 3, [128, 512], BF16)
            Kp = sbpool(nc, esb, "K", 2, [128, S], BF16)
            Qp = sbpool(nc, esb, "Q", 2, [128, S], BF16)
            Qrp = sbpool(nc, esb, "Qr", 2, [128, S], BF16)
            for q_ in Qrp.items:
                P.memset("pool", q_[64:128, :], 0.0, [q_.b])
            Vp = sbpool(nc, esb, "V", 2, [128, NT, 129], BF16)
            kpe = sb(nc, esb, "kpe", [128, S], BF16)
            P.memset("pool", kpe[64:128, :], 0.0, [kpe.b])
            obp = sbpool(nc, esb, "ob", 3, [128, 128], BF16)
            P.dma(kpe[0:64, :], Sx["kpeT"][:, :], W=[kpe.b], sb=kpe.b)

            def jobsC():
                for h in range(4):
                    K, Q, Qr, V = Kp.next(), Qp.next(), Qrp.next(), Vp.next()
                    P.dma(K[:], Sx["KT_Cn"][h * 128:(h + 1) * 128, :], W=[K.b], sb=K.b)
                    P.dma(Q[:], Sx["QT_Cn"][h * 128:(h + 1) * 128, :], W=[Q.b], sb=Q.b)
                    P.dma(Qr[0:64, :], Sx["QT_Cr"][h * 64:(h + 1) * 64, :], W=[Qr.b], sb=Qr.b)
                    load_vaug(V, Sx["V_C"], h * 128)
                    for qt in range(NT):
                        acc = accp.next()

                        def post(h=h, qt=qt, acc=acc):
                            r = C.small.next()
                            recip_sum(acc, 128, r[:, 0:1], r.b)
                            ob = obp.next()
                            P.ts("dve", ob[:], acc[:, 0:128], r[:, 0:1], None, ALU.mult, None, [acc.b, r.b], [ob.b])
                            P.dma(Sx["O_C"][qt * 128:(qt + 1) * 128, h * 128:(h + 1) * 128], ob[:], R=[ob.b], sb=ob.b)

                        def score_fn(kt, K=K, Q=Q, Qr=Qr, qt=qt):
                            return [(K[:, kt * 128:(kt + 1) * 128], Q[:, qt * 128:(qt + 1) * 128], [K.b, Q.b]),
                                    (kpe[:, kt * 128:(kt + 1) * 128], Qr[:, qt * 128:(qt + 1) * 128], [kpe.b, Qr.b])]

                        def mask_fn(kt, qt=qt):
                            return [(causal[:], ident[:], [causal.b, ident.b])] if kt == qt else []

                        def v_fn(kt, V=V):
                            return V[:, kt, :], [V.b]

                        yield from head_jobs(ptp, score_fn, mask_fn, v_fn, acc, 129, range(qt + 1), 192 ** -0.5, post)
            run_jobs(jobsC())
            P.barrier()
        if stop_after == "BC":
            break

        if "D" in branches:
          psc = psc2
          PIPE[0] = 1
          with scope() as esb:
            ptp = sbpool(nc, esb, "pt", 3, [128, 512], BF16)
            iqp = sbpool(nc, esb, "iqt", 4, [128, 8, 128], BF16)
            for q_ in iqp.items:
                v_ = q_[:].rearrange("p (g e) c -> p g e c", e=2)
                P.memset("pool", v_[64:128, :, 0, :], 0.0, [q_.b])
                P.memset("pool", v_[0:64, :, 1, :], 0.0, [q_.b])
            ik2 = sb(nc, esb, "ik2", [128, S], BF16)
            qdp = sbpool(nc, esb, "qd", 3, [128, 4, 128], BF16)
            Kd = [sb(nc, esb, f"Kd{i}", [128, S], BF16) for i in range(4)]
            Vd = [sb(nc, esb, f"Vd{i}", [128, NT, 129], BF16) for i in range(4)]
            iwt = sb(nc, esb, "iwt", [128, NT, 8], F32)
            idxp = sbpool(nc, esb, "idx", 4, [128, S], F32)
            Mkp = sbpool(nc, esb, "Mk", 4, [128, S], BF16)
            rp = sbpool(nc, esb, "relu", 2, [128, 512], F32)
            bjunk = sb(nc, esb, "bjunk", [128, S], BF16)
            negbig = sb(nc, esb, "negbig", [128, 128], F32)
            posbig = sb(nc, esb, "posbig", [128, 128], F32)
            dtmpp = sbpool(nc, esb, "dtmp", 2, [128, 128], F32)
            bs = sbpool(nc, esb, "bs", 6, [128, 32], F32)
            obp = sbpool(nc, esb, "ob", 3, [128, 128], BF16)
            P.dma(negbig[:], I["c_negbig"][:, :], W=[negbig.b], sb=negbig.b)
            P.dma(posbig[:], I["c_posbig"][:, :], W=[posbig.b], sb=posbig.b)
            P.dma(iwt[:], Sx["iw"].rearrange("(t p) h -> p t h", p=128), W=[iwt.b], sb=iwt.b)
            P.dma(ik2[0:64, :], Sx["ikT"][:, :], W=[ik2.b], sb=ik2.b)
            P.dma(ik2[64:128, :], Sx["ikT"][:, :], W=[ik2.b], sb=ik2.b)
            for i in range(4):
                P.dma(Kd[i][:], Sx["KT_D"][i * 128:(i + 1) * 128, :], W=[Kd[i].b], sb=Kd[i].b)
                load_vaug(Vd[i], Sx["V_D"], i * 128)
            NBIS = 16

            def idx_accum(qt, out):
                Lq = (qt + 1) * 128
                idx = idxp.next()
                iq = iqp.next()
                iqv = iq[:].rearrange("p (g e) c -> p g e c", e=2)
                srcv = Sx["iqT"][:, qt * 128:(qt + 1) * 128].rearrange("(g e p) c -> e p g c", e=2, p=64)
                P.dma(iqv[0:64, :, 0, :], srcv[0], W=[iq.b], sb=iq.b)
                P.dma(iqv[64:128, :, 1, :], srcv[1], W=[iq.b], sb=iq.b)
                for c0 in range(0, Lq, 512):
                    w = min(512, Lq - c0)
                    for h in range(8):
                        pbk = psb.next()
                        psv = pbk[:].bitcast(F32)
                        p0 = (h % 2) * 64
                        P.mm(psv[:, :w], iq[:, h, :], ik2[:, c0:c0 + w],
                             True, True, [iq.b, ik2.b], [pbk.b])
                        r = rp.next()
                        P.act(r[:, :w], psv[:, :w], AF.Relu, [pbk.b], [r.b])
                        if h == 0:
                            P.ts("dve", idx[:, c0:c0 + w], r[:, :w], iwt[:, qt, 0:1], None, ALU.mult, None,
                                 [r.b, iwt.b], [idx.b])
                        else:
                            P.stt(idx[:, c0:c0 + w], r[:, :w], iwt[:, qt, h:h + 1], idx[:, c0:c0 + w], ALU.mult, ALU.add,
                                  [r.b, iwt.b, idx.b], [idx.b])
                    yield
                b = bs.next()
                d0 = qt * 128
                if qt >= 2:
                    dtmp = dtmpp.next()
                    P.tt("dve", dtmp[:], idx[:, d0:Lq], posbig[:], ALU.add, [idx.b, posbig.b], [dtmp.b])
                    P.red(b[:, 0:1], dtmp[:], ALU.min, [dtmp.b], [b.b])
                    P.red(b[:, 1:2], idx[:, 0:d0], ALU.min, [idx.b], [b.b])
                    P.tt("dve", b[:, 0:1], b[:, 0:1], b[:, 1:2], ALU.min, [b.b], [b.b])
                P.tt("dve", idx[:, d0:Lq], idx[:, d0:Lq], negbig[:], ALU.add, [idx.b, negbig.b], [idx.b])
                if qt >= 2:
                    P.red(b[:, 1:2], idx[:, 0:Lq], ALU.max, [idx.b], [b.b])
                    P.tt("dve", b[:, 2:3], b[:, 1:2], b[:, 0:1], ALU.subtract, [b.b], [b.b])
                    P.memset("dve", b[:, 8:8 + NBIS], 0.0, [b.b])
                else:
                    P.memset("dve", b[:, 0:1], -1e29, [b.b])
                out.append(dict(qt=qt, Lq=Lq, idx=idx, b=b))

            def bisect(states):
                sts = [x for x in states if x["qt"] >= 2]
                for it in range(NBIS):
                    f = 0.5 ** (it + 1)
                    for x in sts:
                        b = x["b"]
                        P.stt(b[:, 3:4], b[:, 2:3], -f, b[:, 0:1], ALU.mult, ALU.subtract, [b.b], [b.b])
                    for x in sts:
                        b, idx, Lq = x["b"], x["idx"], x["Lq"]
                        P.act(bjunk[:, :Lq], idx[:, :Lq], AF.Sign, [idx.b, b.b], [bjunk.b, b.b], bias=b[:, 3:4],
                              accum=b[:, 8 + it:9 + it])
                    for x in sts:
                        b, Lq = x["b"], x["Lq"]
                        P.ts("dve", b[:, 5:6], b[:, 8 + it:9 + it], 510.5 - Lq, f, ALU.is_ge, ALU.mult, [b.b], [b.b])
                        P.stt(b[:, 0:1], b[:, 5:6], b[:, 2:3], b[:, 0:1], ALU.mult, ALU.add, [b.b], [b.b])
                    yield

            def run_rr(gens):
                gens = list(gens)
                while gens:
                    nxt = []
                    for g_ in gens:
                        try:
                            next(g_)
                            nxt.append(g_)
                        except StopIteration:
                            pass
                    gens = nxt

            def accum_pair(p):
                out = []
                accd[p] = out
                for qt in (2 * p, 2 * p + 1):
                    yield from idx_accum(qt, out)

            def mk_pair(p):
                res = {}
                for x in accd.pop(p):
                    Mk = Mkp.next()
                    P.ts("pool", Mk[:, :x["Lq"]], x["idx"][:, :x["Lq"]], x["b"][:, 0:1], NEG, ALU.is_lt, ALU.mult,
                         [x["idx"].b, x["b"].b], [Mk.b])
                    res[x["qt"]] = Mk
                return res

            accd = {}
            NP = NT // 2

            def jobsD():
                run_rr([accum_pair(0)])
                run_rr([bisect(accd[0])])
                mks = dict(mk_pair(0))
                run_rr([accum_pair(1)])
                for qt in range(NT):
                    st = {"Mk": mks[qt]}
                    qd = qdp.next()
                    P.dma(qd[:], Sx["QT_D"][:, qt * 128:(qt + 1) * 128].rearrange("(h p) c -> p h c", p=128),
                          W=[qd.b], sb=qd.b)

                    def pre(qt=qt, mks=mks):
                        p1 = qt // 2 + 1
                        if p1 < NP:
                            gens = [bisect(accd[p1])]
                            if p1 + 1 < NP:
                                gens.append(accum_pair(p1 + 1))
                            run_rr(gens)
                            mks.update(mk_pair(p1))

                    for h in range(4):
                        acc = accp.next()

                        def post(h=h, qt=qt, acc=acc):
                            r = C.small.next()
                            recip_sum(acc, 128, r[:, 0:1], r.b)
                            ob = obp.next()
                            P.ts("dve", ob[:], acc[:, 0:128], r[:, 0:1], None, ALU.mult, None, [acc.b, r.b], [ob.b])
                            P.dma(Sx["O_D"][qt * 128:(qt + 1) * 128, h * 128:(h + 1) * 128], ob[:], R=[ob.b], sb=ob.b)

                        def score_fn(kt, h=h, qd=qd):
                            return [(Kd[h][:, kt * 128:(kt + 1) * 128], qd[:, h, :], [Kd[h].b, qd.b])]

                        def mask_fn(kt, st=st):
                            Mk = st["Mk"]
                            return [(Mk[:, kt * 128:(kt + 1) * 128], ident[:], [Mk.b, ident.b])]

                        def v_fn(kt, h=h):
                            return Vd[h][:, kt, :], [Vd[h].b]

                        yield from head_jobs(ptp, score_fn, mask_fn, v_fn, acc, 129, range(qt + 1), 128 ** -0.5, post,
                                             pre if (h == 0 and qt % 2 == 0) else None)
            run_jobs(jobsD())
            P.barrier()
        if stop_after == "BD":
            break

        if "B" in branches:
          psc = psc3
          PIPE[0] = 2
          with scope() as esb:
            ptp = sbpool(nc, esb, "pt", 3, [128, 512], BF16)
            Qb = [sb(nc, esb, f"Qb{i}", [128, S], BF16) for i in range(4)]
            ks = sb(nc, esb, "ks", [128, S], BF16)
            kw = sb(nc, esb, "kw", [128, S], BF16)
            vsa = sb(nc, esb, "vsa", [128, NT, 129], BF16)
            vwa = sb(nc, esb, "vwa", [128, NT, 129], BF16)
            kcm = sb(nc, esb, "kcm", [128, 256], BF16)
            vca = sb(nc, esb, "vca", [128, 2, 193], BF16)
            gBt = sb(nc, esb, "gBt", [128, NT, 12], F32)
            selb = sb(nc, esb, "selb", [128, NT, 64], F32)
            cmp_ = sbpool(nc, esb, "cm", 2, [128, 256], BF16)
            Mkp = sbpool(nc, esb, "MkB", 2, [128, S], BF16)
            obf = sbpool(nc, esb, "obf", 2, [128, 4, 128], F32)
            impp = sbpool(nc, esb, "imp", 2, [128, 64], F32)
            scp = sbpool(nc, esb, "sc", 2, [128, 64], F32)
            cmpm = sb(nc, esb, "cmpm", [128, 64, 64], F32)
            rank = sbpool(nc, esb, "rank", 2, [128, 64], F32)
            obp = sbpool(nc, esb, "ob", 3, [128, 128], BF16)
            coefp = sbpool(nc, esb, "coef", 8, [128, 2], F32)
            for i in range(4):
                P.dma(Qb[i][:], Sx["QT_B"][i * 128:(i + 1) * 128, :], W=[Qb[i].b], sb=Qb[i].b)
            P.dma(ks[:], Sx["bkT"][128:256, :], W=[ks.b], sb=ks.b)
            P.dma(kw[:], Sx["bkT"][256:384, :], W=[kw.b], sb=kw.b)
            load_vaug(vsa, Sx["vs"], 0)
            load_vaug(vwa, Sx["vw"], 0)
            P.dma(kcm[:], Sx["kcmpT"][:, :], W=[kcm.b], sb=kcm.b)
            P.dma(vca[:, :, 0:128], Sx["vcmp"].rearrange("(g p) d -> p g d", p=128), W=[vca.b], sb=vca.b)
            P.memset("pool", vca[:, :, 128:129], 1.0, [vca.b])
            P.dma(vca[:, :, 129:193], I["c_overlap"].rearrange("(g p) j -> p g j", p=128), W=[vca.b], sb=vca.b)
            P.dma(gBt[:], Sx["gB"].rearrange("(t p) g -> p t g", p=128), W=[gBt.b], sb=gBt.b)
            P.dma(selb[:], I["c_selb"].rearrange("(t p) j -> p t j", p=128), W=[selb.b], sb=selb.b)

            def jobsB():
                QS = {}

                def setup(qt):
                    Lq = (qt + 1) * 128
                    cm = cmp_.next()
                    P.dma(cm[:], I["c_cmask"][qt * 128:(qt + 1) * 128, :], W=[cm.b], sb=cm.b)
                    of4 = obf.next()
                    imp = impp.next()
                    st = {}

                    def coef(acc, col, gcol, qt=qt):
                        cf = coefp.next()
                        recip_sum(acc, col, cf[:, 0:1], cf.b)
                        P.tt("dve", cf[:, 1:2], cf[:, 0:1], gBt[:, qt, gcol:gcol + 1], ALU.mult, [cf.b, gBt.b], [cf.b])
                        return cf
                    QS[qt] = (Lq, cm, of4, imp, st, coef)

                def cmp_jobs(qt):
                    Lq, cm, of4, imp, st, coef = QS[qt]
                    for h in range(4):
                        acc = accp.next()

                        def post(h=h, acc=acc, of4=of4, imp=imp, coef=coef):
                            cf = coef(acc, 128, 3 * h + 0)
                            P.ts("dve", of4[:, h, :], acc[:, 0:128], cf[:, 1:2], None, ALU.mult, None, [acc.b, cf.b], [of4.b])
                            if h == 0:
                                P.ts("dve", imp[:], acc[:, 129:193], cf[:, 0:1], None, ALU.mult, None, [acc.b, cf.b], [imp.b])
                            else:
                                P.stt(imp[:], acc[:, 129:193], cf[:, 0:1], imp[:], ALU.mult, ALU.add,
                                      [acc.b, cf.b, imp.b], [imp.b])

                        def score_fn(kt, h=h, qt=qt):
                            return [(kcm[:, kt * 128:(kt + 1) * 128], Qb[h][:, qt * 128:(qt + 1) * 128], [kcm.b, Qb[h].b])]

                        def mask_fn(kt, cm=cm):
                            return [(cm[:, kt * 128:(kt + 1) * 128], ident[:], [cm.b, ident.b])]

                        def v_fn(kt):
                            return vca[:, kt, :], [vca.b]

                        yield from head_jobs(ptp, score_fn, mask_fn, v_fn, acc, 193, range(2), 128 ** -0.5, post)


                def make_select(qt):
                    Lq, cm, of4, imp, st, coef = QS[qt]
                    def select(qt=qt, imp=imp, st=st, Lq=Lq):
                        sc = scp.next()
                        P.tt("dve", sc[:], imp[:], selb[:, qt, :], ALU.add, [imp.b, selb.b], [sc.b])
                        P.tt("dve", cmpm[:], sc[:].unsqueeze(1).to_broadcast([128, 64, 64]),
                             sc[:].unsqueeze(2).to_broadcast([128, 64, 64]), ALU.is_gt, [sc.b], [cmpm.b])
                        rk = rank.next()
                        P.red(rk[:], cmpm[:], ALU.add, [cmpm.b], [rk.b])
                        P.ts("dve", rk[:], rk[:], 15.5, NEG, ALU.is_gt, ALU.mult, [rk.b], [rk.b])
                        Mk = Mkp.next()
                        nb = Lq // 64
                        P.copy("pool", Mk[:, :Lq].rearrange("p (j c) -> p j c", c=64),
                               rk[:, :nb].unsqueeze(2).to_broadcast([128, nb, 64]), [rk.b], [Mk.b])
                        P.tt("pool", Mk[:, Lq - 128:Lq], Mk[:, Lq - 128:Lq], causal[:], ALU.add, [Mk.b, causal.b], [Mk.b])
                        st["Mk"] = Mk

                    return select

                def slc_jobs(qt):
                    Lq, cm, of4, imp, st, coef = QS[qt]
                    for h in range(4):
                        acc = accp.next()

                        def post(h=h, acc=acc, of4=of4, coef=coef):
                            cf = coef(acc, 128, 3 * h + 1)
                            P.stt(of4[:, h, :], acc[:, 0:128], cf[:, 1:2], of4[:, h, :], ALU.mult, ALU.add,
                                  [acc.b, cf.b, of4.b], [of4.b])

                        def score_fn(kt, h=h, qt=qt):
                            return [(ks[:, kt * 128:(kt + 1) * 128], Qb[h][:, qt * 128:(qt + 1) * 128], [ks.b, Qb[h].b])]

                        def mask_fn(kt, st=st):
                            Mk = st["Mk"]
                            return [(Mk[:, kt * 128:(kt + 1) * 128], ident[:], [Mk.b, ident.b])]

                        def v_fn(kt):
                            return vsa[:, kt, :], [vsa.b]

                        yield from head_jobs(ptp, score_fn, mask_fn, v_fn, acc, 129, range(qt + 1), 128 ** -0.5, post)


                def win_jobs(qt, pre):
                    Lq, cm, of4, imp, st, coef = QS[qt]
                    for h in range(4):
                        acc = accp.next()

                        def post(h=h, acc=acc, of4=of4, qt=qt, coef=coef):
                            cf = coef(acc, 128, 3 * h + 2)
                            P.stt(of4[:, h, :], acc[:, 0:128], cf[:, 1:2], of4[:, h, :], ALU.mult, ALU.add,
                                  [acc.b, cf.b, of4.b], [of4.b])
                            ob = obp.next()
                            P.copy("pool", ob[:], of4[:, h, :], [of4.b], [ob.b])
                            P.dma(Sx["O_B"][qt * 128:(qt + 1) * 128, h * 128:(h + 1) * 128], ob[:], R=[ob.b], sb=ob.b)

                        def score_fn(kt, h=h, qt=qt):
                            return [(kw[:, kt * 128:(kt + 1) * 128], Qb[h][:, qt * 128:(qt + 1) * 128], [kw.b, Qb[h].b])]

                        def mask_fn(kt, qt=qt):
                            if kt == qt:
                                return [(causal[:], ident[:], [causal.b, ident.b])]
                            if kt == qt - 4:
                                return [(anti[:], ident[:], [anti.b, ident.b])]
                            return []

                        def v_fn(kt):
                            return vwa[:, kt, :], [vwa.b]

                        yield from head_jobs(ptp, score_fn, mask_fn, v_fn, acc, 129, range(max(0, qt - 4), qt + 1),
                                             128 ** -0.5, post, pre if h == 0 else None)

                setup(0)
                yield from cmp_jobs(0)
                sel0 = make_select(0)
                first = True
                for qt in range(NT):
                    if qt + 1 < NT:
                        setup(qt + 1)
                        gen = cmp_jobs(qt + 1)
                        if first:
                            j0 = next(gen)
                            j0.pre = sel0
                            yield j0
                            first = False
                        yield from gen
                    yield from slc_jobs(qt)
                    yield from win_jobs(qt, make_select(qt + 1) if qt + 1 < NT else None)
            run_jobs(jobsB())
            P.barrier()
        if stop_after == "BB":
            break
        with scope() as esc:
            wstage = sbpool(nc, esc, "wstC", 2, [128, 1024], F32)
            Wg = load_w_bf16(esc, "Wg", I["w_in"][l][:, OFF["gate"]:OFF["gate"] + 4096], 8, 4096, wstage)
            Wb = load_w_bf16(esc, "Wb", I["w_branch"][l].rearrange("n w d -> (n w) d"), 16, D, wstage)
            Wo = load_w_bf16(esc, "Wo", I["w_out"][l], 8, D, wstage)
            g1 = load_bcast(esc, "g1c", I["norm1_g"][l:l + 1, :], D)
            xs = sbpool(nc, esc, "xsC", 2, [128, D], F32)
            xop = sbpool(nc, esc, "xoC", 2, [128, D], F32)
            hbp = sbpool(nc, esc, "hbC", 2, [128, D], BF16)
            hTp = sbpool(nc, esc, "hTC", 2, [128, 8, 128], BF16)
            ob4p = sbpool(nc, esc, "ob4", 1, [128, 4, 512], BF16)
            oTp = sbpool(nc, esc, "oT", 1, [128, 16, 128], BF16)
            mrg = sbpool(nc, esc, "mrg", 1, [128, D], F32)
            mbp = sbpool(nc, esc, "mb", 1, [128, D], BF16)
            mTp = sbpool(nc, esc, "mT", 2, [128, 8, 128], BF16)
            sgp = sbpool(nc, esc, "sg", 2, [128, 512], F32)
            tmpp = sbpool(nc, esc, "tmpC", 2, [128, 512], F32)
            for tt in range(NT):
                rows = slice(tt * 128, (tt + 1) * 128)
                xt = xs.next()
                P.dma(xt[:], xsrc[rows, :], W=[xt.b], sb=xt.b)
                ob4 = ob4p.next()
                for n_, nm in enumerate(("O_A", "O_B", "O_C", "O_D")):
                    P.dma(ob4[:, n_, :], Sx[nm][rows, :], W=[ob4.b], sb=ob4.b)
                ss = rms(xt[:], D, [xt.b])
                hb = hbp.next()
                P.stt(hb[:], xt[:], ss[:, 1:2], g1[:], ALU.mult, ALU.mult, [xt.b, ss.b, g1.b], [hb.b])
                pb = psb.next()
                for k in range(8):
                    P.tr(pb[:, k * 128:(k + 1) * 128], hb[:, k * 128:(k + 1) * 128], [hb.b, ident.b], [pb.b])
                hTt = hTp.next()
                P.copy("dve", hTt[:], pb[:].rearrange("p (k c) -> p k c", k=8), [pb.b], [hTt.b])
                oT = oTp.next()
                for g in range(2):
                    pb = psb.next()
                    for k in range(8):
                        kk = g * 8 + k
                        P.tr(pb[:, k * 128:(k + 1) * 128], ob4[:, kk // 4, (kk % 4) * 128:(kk % 4 + 1) * 128],
                             [ob4.b, ident.b], [pb.b])
                    P.copy("dve", oT[:, g * 8:(g + 1) * 8, :], pb[:].rearrange("p (k c) -> p k c", k=8), [pb.b], [oT.b])
                mg = mrg.next()
                for n_ in range(4):
                    for j in range(2):
                        cs = slice(j * 512, (j + 1) * 512)
                        pg = psf.next()
                        for k in range(8):
                            P.mm(pg[:, :], hTt[:, k, :], Wg[:, k, n_ * 1024 + j * 512:n_ * 1024 + (j + 1) * 512],
                                 k == 0, k == 7, [hTt.b, Wg.b], [pg.b])
                        sg = sgp.next()
                        P.act(sg[:], pg[:, :], AF.Sigmoid, [pg.b], [sg.b])
                        pbr = psf.next()
                        for k in range(4):
                            P.mm(pbr[:, :], oT[:, n_ * 4 + k, :], Wb[:, n_ * 4 + k, cs], k == 0, k == 3, [oT.b, Wb.b], [pbr.b])
                        if n_ == 0:
                            P.tt("dve", mg[:, cs], pbr[:, :], sg[:], ALU.mult, [pbr.b, sg.b], [mg.b])
                        else:
                            tm = tmpp.next()
                            P.tt("dve", tm[:], pbr[:, :], sg[:], ALU.mult, [pbr.b, sg.b], [tm.b])
                            P.tt("pool", mg[:, cs], mg[:, cs], tm[:], ALU.add, [mg.b, tm.b], [mg.b])
                mb = mbp.next()
                P.copy("pool", mb[:], mg[:], [mg.b], [mb.b])
                pb = psb.next()
                for k in range(8):
                    P.tr(pb[:, k * 128:(k + 1) * 128], mb[:, k * 128:(k + 1) * 128], [mb.b, ident.b], [pb.b])
                mT = mTp.next()
                P.copy("dve", mT[:], pb[:].rearrange("p (k c) -> p k c", k=8), [pb.b], [mT.b])
                xo = xop.next()
                for j in range(2):
                    cs = slice(j * 512, (j + 1) * 512)
                    po = psf.next()
                    for k in range(8):
                        P.mm(po[:, :], mT[:, k, :], Wo[:, k, cs], k == 0, k == 7, [mT.b, Wo.b], [po.b])
                    P.tt("dve", xo[:, cs], po[:, :], xt[:, cs], ALU.add, [po.b, xt.b], [xo.b])
                P.dma(Sx["xb"][rows, :], xo[:], R=[xo.b], sb=xo.b)
            P.barrier()
        if stop_after == "Ca":
            break

        with scope() as esd:
            wstage = sbpool(nc, esd, "wstD", 2, [128, 1024], F32)
            Wgu = load_w_bf16(esd, "Wgu", I["w_gate_up"][l], 8, 2 * DFF, wstage)
            Wd = load_w_bf16(esd, "Wd", I["w_down"][l], 22, D, wstage)
            g2 = load_bcast(esd, "g2", I["norm2_g"][l:l + 1, :], D)
            last = (li == n_layers - 1)
            if last and final_norm:
                gf = load_bcast(esd, "gf", I["final_norm_g"][0:1, :], D)
            xs = sbpool(nc, esd, "xsD", 2, [128, D], F32)
            xop = sbpool(nc, esd, "xoD", 2, [128, D], F32)
            hbp = sbpool(nc, esd, "hbD", 2, [128, D], BF16)
            hTp = sbpool(nc, esd, "hTD", 2, [128, 8, 128], BF16)
            actp = sbpool(nc, esd, "actb", 1, [128, DFF], BF16)
            aTp = sbpool(nc, esd, "aT", 1, [128, 22, 128], BF16)
            sgp = sbpool(nc, esd, "sgD", 3, [128, 256], F32)
            for tt in range(NT):
                rows = slice(tt * 128, (tt + 1) * 128)
                xt = xs.next()
                P.dma(xt[:], Sx["xb"][rows, :], W=[xt.b], sb=xt.b)
                ss = rms(xt[:], D, [xt.b])
                hb = hbp.next()
                P.stt(hb[:], xt[:], ss[:, 1:2], g2[:], ALU.mult, ALU.mult, [xt.b, ss.b, g2.b], [hb.b])
                pb = psb.next()
                for k in range(8):
                    P.tr(pb[:, k * 128:(k + 1) * 128], hb[:, k * 128:(k + 1) * 128], [hb.b, ident.b], [pb.b])
                hTt = hTp.next()
                P.copy("dve", hTt[:], pb[:].rearrange("p (k c) -> p k c", k=8), [pb.b], [hTt.b])
                ab = actp.next()
                for j in range(11):
                    pg = psf.next()
                    for part in range(2):
                        c0 = part * DFF + j * 256
                        for k in range(8):
                            P.mm(pg[:, part * 256:(part + 1) * 256], hTt[:, k, :], Wgu[:, k, c0:c0 + 256], k == 0, k == 7,
                                 [hTt.b, Wgu.b], [pg.b])
                    sg = sgp.next()
                    P.act(sg[:], pg[:, 0:256], AF.Silu, [pg.b], [sg.b])
                    P.tt("dve", ab[:, j * 256:(j + 1) * 256], pg[:, 256:512], sg[:], ALU.mult, [pg.b, sg.b], [ab.b])
                aT = aTp.next()
                for g0 in range(0, 22, 8):
                    ng = min(8, 22 - g0)
                    pb = psb.next()
                    for k in range(ng):
                        P.tr(pb[:, k * 128:(k + 1) * 128], ab[:, (g0 + k) * 128:(g0 + k + 1) * 128], [ab.b, ident.b], [pb.b])
                    P.copy("dve", aT[:, g0:g0 + ng, :], pb[:, :ng * 128].rearrange("p (k c) -> p k c", k=ng), [pb.b], [aT.b])
                xo = xop.next()
                for j in range(2):
                    cs = slice(j * 512, (j + 1) * 512)
                    pd = psf.next()
                    for k in range(22):
                        P.mm(pd[:, :], aT[:, k, :], Wd[:, k, cs], k == 0, k == 21, [aT.b, Wd.b], [pd.b])
                    P.tt("dve", xo[:, cs], pd[:, :], xt[:, cs], ALU.add, [pd.b, xt.b], [xo.b])
                if last:
                    if final_norm:
                        ss2 = rms(xo[:], D, [xo.b])
                        P.stt(xo[:], xo[:], ss2[:, 1:2], gf[:], ALU.mult, ALU.mult, [xo.b, ss2.b, gf.b], [xo.b])
                    P.dma(out[rows, :], xo[:], R=[xo.b], sb=xo.b)
                else:
                    P.dma(Sx["xa"][rows, :], xo[:], R=[xo.b], sb=xo.b)
            P.barrier()

    P.barrier()
    with nc.Block() as blk:
        @blk.tensor
        def _(e):
            P.replay(e, "pe")

        @blk.scalar
        def _(e):
            P.replay(e, "act")

        @blk.vector
        def _(e):
            P.replay(e, "dve")

        @blk.gpsimd
        def _(e):
            P.replay(e, "pool")

        @blk.sync
        def _(e):
            P.replay(e, "sp")
    es_top.close()
    C.nsem = P.nsem
    C.maxval = P.maxval
    C.counts = dict(P.seq)
    return nc, C


def host_consts():
    bf = ml_dtypes.bfloat16
    c = {}
    c["c_ident"] = np.eye(128, dtype=np.float32).astype(bf)
    t = np.arange(128)[:, None]
    s = np.arange(128)[None, :]
    c["c_causal"] = np.where(s > t, NEG, 0.0).astype(np.float32).astype(bf)
    c["c_anti"] = np.where(s <= t, NEG, 0.0).astype(np.float32).astype(bf)
    tt = np.arange(S)[:, None]
    n = np.arange(256)[None, :]
    c["c_cmask"] = np.where(16 * n + 31 > tt, NEG, 0.0).astype(np.float32).astype(bf)
    j = np.arange(64)[None, :]
    cur = tt // 64
    forced = (j == 0) | (j == cur) | (j == cur - 1)
    visible = (j * 64) <= tt
    c["c_selb"] = np.where(visible, np.where(forced, 1e9, 0.0), -1e30).astype(np.float32)
    c["c_negbig"] = np.where(s > t, -1e30, 0.0).astype(np.float32)
    c["c_posbig"] = np.where(s > t, 1e30, 0.0).astype(np.float32)
    nn = np.arange(256)[:, None]
    cs = nn * 16
    ce = cs + 31
    ss_ = np.arange(64)[None, :] * 64
    ov = ((cs < ss_ + 64) & (ce >= ss_)).astype(np.float32)
    ov[255, :] = 0.0
    c["c_overlap"] = ov.astype(bf)
    for nm, rot in (("da", 16), ("nsa", 32), ("mla", 64), ("dsa", 32), ("idx", 16)):
        inv = np.power(np.float32(500000.0), -np.arange(0, rot, 2, dtype=np.float32) / np.float32(rot)).astype(np.float32)
        ang = (np.arange(S, dtype=np.float32)[:, None] * inv[None, :]).astype(np.float32)
        c["cos_" + nm] = np.cos(ang).astype(np.float32)
        c["sin_" + nm] = np.sin(ang).astype(np.float32)
    return c


_CACHE = {}


def kernel(**inputs):
    x = np.ascontiguousarray(inputs["x"], dtype=np.float32)
    if "prog" not in _CACHE:
        _CACHE["prog"] = build(DEPTH)
    nc, C = _CACHE["prog"]
    consts = host_consts()
    base = {k: np.ascontiguousarray(v, dtype=np.float32) for k, v in inputs.items() if k != "x"}
    base["final_norm_g"] = base["final_norm_g"].reshape(1, D)
    base.update(consts)
    in_maps = []
    cmap = {0: 0, 1: 1, 4: 2, 5: 3}
    zx = np.zeros_like(x[0])
    for c in range(8):
        m = dict(base)
        m["x"] = x[cmap[c]] if c in cmap else zx
        in_maps.append(m)
    res = run_bass_kernel_spmd(nc, in_maps, core_ids=list(range(8)))
    inv = {b: c for c, b in cmap.items()}
    outs = [res.results[inv[b]]["out"] for b in range(4)]
    return np.stack(outs, axis=0).astype(np.float32)
```
